# Optimizing a Trainium2 kernel written in Bass

```python
import jax, jax.numpy as jnp
from jax import lax
import numpy as np

D_MODEL = 1024
BATCH = 8
SEQ = 4096
DEPTH = 1

MEM_LEN = 256
LRU_WIDTH = 512
LRU_BLOCKS = 8
LRU_BLOCK_DIM = LRU_WIDTH // LRU_BLOCKS
CONV_WIDTH = 4
LRU_C = 8.0
FOX_HEADS = 8
FOX_HEAD_DIM = 64
FOX_WIDTH = FOX_HEADS * FOX_HEAD_DIM
Q_BLOCK = 128
MIX_WIDTH = LRU_WIDTH + FOX_WIDTH
IN_COLS = 2 * LRU_WIDTH + 3 * FOX_WIDTH + FOX_HEADS
MEM_HEADS = 4
MEM_HEAD_DIM = 128
MEM_WIDTH = MEM_HEADS * MEM_HEAD_DIM
N_GROUPS = 4
EXPERTS_PER_GROUP = 8
N_EXPERTS = N_GROUPS * EXPERTS_PER_GROUP
TOP_K = 2
D_EXPERT = 512
MOE_BLOCK = 128
EPS = 1e-6

kernel_name = 'hymba_rglru_fox_hmoe_layer'


def rmsnorm(x, g):
    xf = x.astype(jnp.float32)
    y = xf * lax.rsqrt(jnp.mean(xf * xf, axis=-1, keepdims=True) + EPS)
    return (y * g.astype(jnp.float32)).astype(x.dtype)


def rg_lru_group(u, gate_in, conv_w, conv_b, wa, ba, wx, bx, a_param):
    B, S, _ = u.shape
    up = jnp.pad(u, ((0, 0), (CONV_WIDTH - 1, 0), (0, 0)))
    xc = conv_b
    for tap in range(CONV_WIDTH):
        xc = xc + up[:, tap:tap + S] * conv_w[tap]
    xb = xc.reshape(B, S, LRU_BLOCKS, LRU_BLOCK_DIM)
    r = jax.nn.sigmoid(jnp.einsum('bsni,nij->bsnj', xb, wa).reshape(B, S, LRU_WIDTH) + ba)
    i = jax.nn.sigmoid(jnp.einsum('bsni,nij->bsnj', xb, wx).reshape(B, S, LRU_WIDTH) + bx)
    log_a = -LRU_C * r.astype(jnp.float32) * jax.nn.softplus(-a_param.astype(jnp.float32))
    a = jnp.exp(log_a)
    mult = jnp.sqrt(jnp.maximum(-jnp.expm1(2.0 * log_a), 0.0))
    b = mult * (i * xc).astype(jnp.float32)

    def combine(lhs, rhs):
        a1, b1 = lhs
        a2, b2 = rhs
        return a1 * a2, a2 * b1 + b2

    _, h = lax.associative_scan(combine, (a, b), axis=1)
    return (h * jax.nn.gelu(gate_in.astype(jnp.float32))).astype(u.dtype)


def forgetting_attention(q, k, v, log_f):
    B, S, H, Dh = q.shape
    c = jnp.cumsum(log_f, axis=1).transpose(0, 2, 1)
    scale = Dh ** -0.5
    outs = []
    for blk in range(S // Q_BLOCK):
        q0 = blk * Q_BLOCK
        q1 = q0 + Q_BLOCK
        s = jnp.einsum('bqhd,bkhd->bhqk', q[:, q0:q1], k[:, :q1]).astype(jnp.float32) * scale
        s = s + c[:, :, q0:q1, None] - c[:, :, None, :q1]
        qpos = q0 + jnp.arange(Q_BLOCK)[:, None]
        kpos = jnp.arange(q1)[None, :]
        s = jnp.where(kpos <= qpos, s, -jnp.inf)
        p = jax.nn.softmax(s, axis=-1)
        outs.append(jnp.einsum('bhqk,bkhd->bqhd', p.astype(v.dtype), v[:, :q1]))
    return jnp.concatenate(outs, axis=1)


def memory_cross_attention(xn, memn, wq, wkv, q_g, k_g, wo):
    B, S, _ = xn.shape
    M = memn.shape[1]
    q = (xn @ wq).reshape(B, S, MEM_HEADS, MEM_HEAD_DIM)
    kv = memn @ wkv
    k = kv[..., :MEM_WIDTH].reshape(B, M, MEM_HEADS, MEM_HEAD_DIM)
    v = kv[..., MEM_WIDTH:].reshape(B, M, MEM_HEADS, MEM_HEAD_DIM)
    q = rmsnorm(q, q_g)
    k = rmsnorm(k, k_g)
    s = jnp.einsum('bqhd,bkhd->bhqk', q, k).astype(jnp.float32) * (MEM_HEAD_DIM ** -0.5)
    p = jax.nn.softmax(s, axis=-1)
    o = jnp.einsum('bhqk,bkhd->bqhd', p.astype(v.dtype), v).reshape(B, S, MEM_WIDTH)
    return o @ wo


def hierarchical_moe(xn, wg, bg, we, be, w_gate, w_up, w_down):
    B, S, D = xn.shape
    T = B * S
    xt = xn.reshape(T, D)
    g_logits = (xt @ wg).astype(jnp.float32) + bg
    g_prob = jax.nn.softmax(g_logits, axis=-1)
    g_idx = jnp.argmax(g_logits, axis=-1)
    g_w = jnp.take_along_axis(g_prob, g_idx[:, None], axis=1)[:, 0]
    e_logits = ((xt @ we).astype(jnp.float32) + be).reshape(T, N_GROUPS, EXPERTS_PER_GROUP)
    e_logits = jnp.take_along_axis(e_logits, g_idx[:, None, None], axis=1)[:, 0]
    e_prob = jax.nn.softmax(e_logits, axis=-1)
    top_p, top_i = lax.top_k(e_prob, TOP_K)
    top_p = top_p / jnp.sum(top_p, axis=-1, keepdims=True)
    gate = g_w[:, None] * top_p
    expert = g_idx[:, None] * EXPERTS_PER_GROUP + top_i
    A = T * TOP_K
    e_flat = expert.reshape(A).astype(jnp.int32)
    tok_flat = jnp.repeat(jnp.arange(T, dtype=jnp.int32), TOP_K)
    w_flat = gate.reshape(A)
    order = jnp.argsort(e_flat)
    e_s = e_flat[order]
    tok_s = tok_flat[order]
    w_s = w_flat[order]
    counts = jnp.zeros((N_EXPERTS,), jnp.int32).at[e_flat].add(1)
    starts = jnp.cumsum(counts) - counts
    padded = (counts + MOE_BLOCK - 1) // MOE_BLOCK * MOE_BLOCK
    pstarts = jnp.cumsum(padded) - padded
    pends = pstarts + padded
    dest = pstarts[e_s] + (jnp.arange(A, dtype=jnp.int32) - starts[e_s])
    P = A + N_EXPERTS * MOE_BLOCK
    n_blocks = P // MOE_BLOCK
    tok_buf = jnp.zeros((P,), jnp.int32).at[dest].set(tok_s)
    w_buf = jnp.zeros((P,), jnp.float32).at[dest].set(w_s)
    blk_start = jnp.arange(n_blocks, dtype=jnp.int32) * MOE_BLOCK
    blk_expert = jnp.minimum(jnp.sum(blk_start[:, None] >= pends[None, :], axis=1), N_EXPERTS - 1)
    x_buf = xt[tok_buf].reshape(n_blocks, MOE_BLOCK, D)

    def run_block(args):
        xb, e = args
        hdn = jax.nn.silu(xb @ w_gate[e]) * (xb @ w_up[e])
        return hdn @ w_down[e]

    y_buf = lax.map(run_block, (x_buf, blk_expert)).reshape(P, D)
    y = jax.ops.segment_sum(y_buf * w_buf[:, None].astype(y_buf.dtype), tok_buf, num_segments=T)
    return y.reshape(B, S, D)


def setup_inputs(seed: int = 0) -> dict:
    key = jax.random.key(seed)
    ks = jax.random.split(key, 32)
    f32 = jnp.float32
    L = DEPTH

    def nrm(k, shape, scale):
        return jax.random.normal(k, shape, f32) * scale

    def gain(k, shape):
        return 1.0 + 0.01 * jax.random.normal(k, shape, f32)

    a_c = jax.random.uniform(ks[10], (L, LRU_WIDTH), f32, minval=0.9, maxval=0.999)
    sig = a_c ** (1.0 / LRU_C)
    a_param = jnp.log(sig) - jnp.log1p(-sig)
    return {
        'x': jax.random.normal(ks[0], (BATCH, SEQ, D_MODEL), f32),
        'mem': jax.random.normal(ks[1], (BATCH, MEM_LEN, D_MODEL), f32),
        'norm_mix_g': gain(ks[2], (L, D_MODEL)),
        'w_in': nrm(ks[3], (L, D_MODEL, IN_COLS), D_MODEL ** -0.5),
        'b_forget': jax.random.uniform(ks[4], (L, FOX_HEADS), f32, minval=1.0, maxval=5.0),
        'conv_w': nrm(ks[5], (L, CONV_WIDTH, LRU_WIDTH), CONV_WIDTH ** -0.5),
        'conv_b': nrm(ks[6], (L, LRU_WIDTH), 0.01),
        'lru_wa': nrm(ks[7], (L, LRU_BLOCKS, LRU_BLOCK_DIM, LRU_BLOCK_DIM), LRU_BLOCK_DIM ** -0.5),
        'lru_ba': nrm(ks[8], (L, LRU_WIDTH), 0.01),
        'lru_wx': nrm(ks[9], (L, LRU_BLOCKS, LRU_BLOCK_DIM, LRU_BLOCK_DIM), LRU_BLOCK_DIM ** -0.5),
        'lru_bx': nrm(ks[11], (L, LRU_WIDTH), 0.01),
        'lru_a_param': a_param,
        'fox_q_g': gain(ks[12], (L, FOX_HEAD_DIM)),
        'fox_k_g': gain(ks[13], (L, FOX_HEAD_DIM)),
        'lru_out_g': gain(ks[14], (L, LRU_WIDTH)),
        'fox_out_g': gain(ks[15], (L, FOX_WIDTH)),
        'w_out': nrm(ks[16], (L, MIX_WIDTH, D_MODEL), MIX_WIDTH ** -0.5),
        'norm_mem_x_g': gain(ks[17], (L, D_MODEL)),
        'norm_mem_g': gain(ks[18], (L, D_MODEL)),
        'mem_wq': nrm(ks[19], (L, D_MODEL, MEM_WIDTH), D_MODEL ** -0.5),
        'mem_wkv': nrm(ks[20], (L, D_MODEL, 2 * MEM_WIDTH), D_MODEL ** -0.5),
        'mem_q_g': gain(ks[21], (L, MEM_HEAD_DIM)),
        'mem_k_g': gain(ks[22], (L, MEM_HEAD_DIM)),
        'mem_wo': nrm(ks[23], (L, MEM_WIDTH, D_MODEL), MEM_WIDTH ** -0.5),
        'norm_ffn_g': gain(ks[24], (L, D_MODEL)),
        'router_group_w': nrm(ks[25], (L, D_MODEL, N_GROUPS), D_MODEL ** -0.5),
        'router_group_b': nrm(ks[26], (L, N_GROUPS), 0.01),
        'router_expert_w': nrm(ks[27], (L, D_MODEL, N_EXPERTS), D_MODEL ** -0.5),
        'router_expert_b': nrm(ks[28], (L, N_EXPERTS), 0.01),
        'exp_w_gate': nrm(ks[29], (L, N_EXPERTS, D_MODEL, D_EXPERT), D_MODEL ** -0.5),
        'exp_w_up': nrm(ks[30], (L, N_EXPERTS, D_MODEL, D_EXPERT), D_MODEL ** -0.5),
        'exp_w_down': nrm(ks[31], (L, N_EXPERTS, D_EXPERT, D_MODEL), D_EXPERT ** -0.5),
    }


def reference(x, mem, norm_mix_g, w_in, b_forget, conv_w, conv_b, lru_wa, lru_ba, lru_wx, lru_bx,
              lru_a_param, fox_q_g, fox_k_g, lru_out_g, fox_out_g, w_out, norm_mem_x_g, norm_mem_g,
              mem_wq, mem_wkv, mem_q_g, mem_k_g, mem_wo, norm_ffn_g, router_group_w, router_group_b,
              router_expert_w, router_expert_b, exp_w_gate, exp_w_up, exp_w_down):
    B, S, _ = x.shape
    cuts = [LRU_WIDTH, 2 * LRU_WIDTH, 2 * LRU_WIDTH + FOX_WIDTH,
            2 * LRU_WIDTH + 2 * FOX_WIDTH, 2 * LRU_WIDTH + 3 * FOX_WIDTH]
    for l in range(DEPTH):
        h = rmsnorm(x, norm_mix_g[l])
        proj = h @ w_in[l]
        u_lru, g_lru, q, k, v, f_logit = jnp.split(proj, cuts, axis=-1)
        y_lru = rg_lru_group(u_lru, g_lru, conv_w[l], conv_b[l], lru_wa[l], lru_ba[l],
                             lru_wx[l], lru_bx[l], lru_a_param[l])
        q = rmsnorm(q.reshape(B, S, FOX_HEADS, FOX_HEAD_DIM), fox_q_g[l])
        k = rmsnorm(k.reshape(B, S, FOX_HEADS, FOX_HEAD_DIM), fox_k_g[l])
        v = v.reshape(B, S, FOX_HEADS, FOX_HEAD_DIM)
        log_f = jax.nn.log_sigmoid(f_logit.astype(jnp.float32) + b_forget[l])
        y_fox = forgetting_attention(q, k, v, log_f).reshape(B, S, FOX_WIDTH)
        mixed = jnp.concatenate([rmsnorm(y_lru, lru_out_g[l]), rmsnorm(y_fox, fox_out_g[l])], axis=-1)
        x = x + mixed @ w_out[l]
        memn = rmsnorm(mem, norm_mem_g[l])
        x = x + memory_cross_attention(rmsnorm(x, norm_mem_x_g[l]), memn, mem_wq[l], mem_wkv[l],
                                       mem_q_g[l], mem_k_g[l], mem_wo[l])
        x = x + hierarchical_moe(rmsnorm(x, norm_ffn_g[l]), router_group_w[l], router_group_b[l],
                                 router_expert_w[l], router_expert_b[l], exp_w_gate[l],
                                 exp_w_up[l], exp_w_down[l])
    return x
```

```python
from contextlib import ExitStack
import numpy as np
import ml_dtypes
import concourse.bass as bass
import concourse.mybir as mybir
from concourse.bass_utils import run_bass_kernel_spmd

F32 = mybir.dt.float32
BF16 = mybir.dt.bfloat16
I32 = mybir.dt.int32
AF = mybir.ActivationFunctionType
ALU = mybir.AluOpType
AX = mybir.AxisListType

ENGS = ["tensor", "vector", "scalar", "gpsimd", "sync"]

S = 4096
D = 1024
NT = 32
NB = 8
NE = 32
CAP = 512
NSB = CAP // 128
EPS = 1e-6
IN_COLS = 2568


class Res:
    __slots__ = ("name", "w", "r", "dsem")

    def __init__(self, name):
        self.name = name
        self.w = None
        self.r = []
        self.dsem = {}


class Prog:
    def __init__(self, nc, stack):
        self.nc = nc
        self.stack = stack
        self.q = {e: [] for e in ENGS}
        self.sems = {}
        self.cnt = {}
        self.waited = {e: {} for e in ENGS}
        self.res = {}
        self.nsem = 0
        self.free_dsems = {"sw": [], "hw": []}
        self.bgsem = {}
        self.bgkeys = set()
        for e in ENGS:
            self._mksem("E_" + e)

    def _mksem(self, key):
        s = self.stack.enter_context(self.nc.semaphore("s%d" % self.nsem))
        self.nsem += 1
        self.sems[key] = s
        self.cnt[key] = 0
        return s

    def R(self, name):
        r = self.res.get(name)
        if r is None:
            r = Res(name)
            self.res[name] = r
        return r

    def _deps(self, eng, reads, writes):
        need = {}

        def add(ev):
            if ev is None:
                return
            k, v = ev
            if need.get(k, 0) < v:
                need[k] = v
        for r in reads:
            if r.startswith("bg:"):
                k_ = self.bgsem[r[3:]]
                add((k_, self.cnt[k_]))
            else:
                add(self.R(r).w)
        for w in writes:
            rw = self.R(w)
            add(rw.w)
            for ev in rw.r:
                add(ev)
        out = []
        own = "E_" + eng
        for k, v in need.items():
            if k == own and v > self.cnt[own]:
                continue
            if self.waited[eng].get(k, 0) >= v:
                continue
            self.waited[eng][k] = v
            out.append((k, v))
        return out

    def _commit(self, ev, reads, writes):
        for w in writes:
            rw = self.R(w)
            rw.w = ev
            rw.r = []
        for r in reads:
            if r in writes or r.startswith("bg:"):
                continue
            rr = self.R(r)
            rr.r = [e for e in rr.r if e[0] != ev[0]]
            rr.r.append(ev)

    def op(self, eng, fn, reads=(), writes=(), inc=True):
        reads = list(reads)
        writes = list(writes)
        waits = self._deps(eng, reads, writes)
        key = "E_" + eng
        if inc:
            self.cnt[key] += 1
            ev = (key, self.cnt[key])
        else:
            ev = (key, self.cnt[key] + 1)
        sems = self.sems
        sem = sems[key]

        def run(h, waits=waits, fn=fn, inc=inc, sem=sem):
            for k, v in waits:
                h.wait_ge(sems[k], v)
            ins = fn(h)
            if inc:
                ins.then_inc(sem, 1)
        run.waits = waits
        self.q[eng].append(run)
        self._commit(ev, reads, writes)
        return ev

    def dma(self, eng, fn, reads=(), writes=(), semres=None):
        reads = list(reads)
        writes = list(writes)
        waits = self._deps(eng, reads, writes)
        if semres is None:
            semres = writes[0] if writes else reads[0]
        rr = self.R(semres)
        cls = "sw" if eng == "gpsimd" else "hw"
        if cls not in rr.dsem:
            if self.free_dsems[cls]:
                rr.dsem[cls] = self.free_dsems[cls].pop()
            else:
                rr.dsem[cls] = "D%s%d" % (cls, self.nsem)
                self._mksem(rr.dsem[cls])
        key = rr.dsem[cls]
        self.cnt[key] += 16
        ev = (key, self.cnt[key])
        sems = self.sems
        sem = sems[key]

        def run(h, waits=waits, fn=fn, sem=sem):
            for k, v in waits:
                h.wait_ge(sems[k], v)
            fn(h).then_inc(sem, 16)
        self.q[eng].append(run)
        self._commit(ev, reads, writes)
        return ev

    def dma_bg(self, eng, fn, name, reads=()):
        key = self.bgsem.get(name)
        if key is None:
            key = "B_" + name
            self._mksem(key)
            self.bgsem[name] = key
            self.bgkeys.add(key)
        waits = self._deps(eng, list(reads), [])
        self.cnt[key] += 16
        sem = self.sems[key]
        sems = self.sems

        def run(h, fn=fn, sem=sem, waits=waits):
            for k, v in waits:
                h.wait_ge(sems[k], v)
            fn(h).then_inc(sem, 16)
        self.q[eng].append(run)

    def begin_region(self, eng):
        self._rg = (eng, len(self.q[eng]), dict(self.waited[eng]), self.cnt["E_" + eng])

    def end_region(self, wrap):
        eng, start, waited, c0 = self._rg
        clos = self.q[eng][start:]
        del self.q[eng][start:]
        nincs = self.cnt["E_" + eng] - c0
        self.q[eng].append(lambda h, clos=clos, nincs=nincs: wrap(h, clos, nincs))
        self.waited[eng] = waited

    def barrier(self):
        for e in ENGS:
            waits = []
            for k, v in self.cnt.items():
                if v == 0 or k in self.bgkeys or self.waited[e].get(k, 0) >= v:
                    continue
                self.waited[e][k] = v
                waits.append((k, v))
            sems = self.sems

            def run(h, waits=waits):
                for k, v in waits:
                    h.wait_ge(sems[k], v)
            self.q[e].append(run)
        for r in self.res.values():
            for cls, k in r.dsem.items():
                self.free_dsems[cls].append(k)
        for cls in self.free_dsems:
            self.free_dsems[cls] = sorted(set(self.free_dsems[cls]))
        self.res = {}

    def emit(self):
        nc = self.nc
        with nc.Block() as block:
            for e in ENGS:
                lst = self.q[e]
                if not lst:
                    continue

                def body(h, lst=lst):
                    for f in lst:
                        f(h)
                getattr(block, e)(body)


def run_pipeline(units):
    n = len(units)
    ns = max(len(u) for u in units)
    for t in range(n + ns - 1):
        gens = []

        def flush():
            while gens:
                for g_ in list(gens):
                    try:
                        next(g_)
                    except StopIteration:
                        gens.remove(g_)
        for s_ in reversed(range(ns)):
            u = t - s_
            if 0 <= u < n and s_ < len(units[u]):
                r = units[u][s_]()
                if r is not None:
                    gens.append(r)
                else:
                    pass
            if gens and (s_ == 0 or not _is_gen_next(units, t, s_ - 1, n)):
                flush()
        flush()


def _is_gen_next(units, t, s_, n):
    u = t - s_
    if not (0 <= u < n and s_ < len(units[u])):
        return True
    import inspect
    return inspect.isgeneratorfunction(units[u][s_])


PV = {}
_c = 0
for _n, _w in [("gmix", 8), ("gout", 8), ("gmemx", 8), ("gmem", 8), ("convw", 16), ("convb", 4), ("ba", 4),
               ("bx", 4), ("apar", 4), ("gq2", 1), ("gk2", 1), ("mqg", 1), ("mkg", 1), ("bf", 1), ("gfx", 8)]:
    PV[_n] = _c
    _c += _w
NPV = _c
CB_ID, CB_BO, CB_ONE, CB_MASK = 0, 128, 256, 384
NCB = 384 + 2048
CF_ID, CF_US, CF_ONE, CF_EB = 0, 128, 256, 384
NCF = 384 + 32
NBV = 1024 + 36 + 3 * 1024
BV_GMIX, BV_GMEMX, BV_GMEM = 1060, 2084, 3108


class Builder:
    def __init__(self, debug=None):
        self.debug = debug
        self.nc = bass.Bass("TRN2", target_bir_lowering=False)
        nc = self.nc
        di = lambda n, s, d=F32: nc.dram_tensor(n, s, d, kind="ExternalInput").ap()
        self.x = di("x", [S, D])
        self.mem = di("mem", [256, D])
        self.w_in = di("w_in", [D, IN_COLS])
        self.w_out = di("w_out", [D, D])
        self.mem_wq = di("mem_wq", [D, 512])
        self.mem_wkv = di("mem_wkv", [D, 1024])
        self.mem_wo = di("mem_wo", [512, D])
        self.wr = di("wr", [D, 36])
        self.wgate = di("wgate", [NE, D, 512])
        self.wup = di("wup", [NE, D, 512])
        self.wdown = di("wdown", [NE, 512, D])
        self.lruw = di("lruw", [128, 4 * 2 * 128])
        self.pvec = di("pvec", [128, NPV])
        self.bvec = di("bvec", [1, NBV])
        self.cbf = di("cbf", [128, NCB], BF16)
        self.cf32 = di("cf32", [128, NCF])
        self.out = nc.dram_tensor("out", [S, D], F32, kind="ExternalOutput").ap()
        dt = lambda n, s, d: nc.dram_tensor(n, s, d).ap()
        self.winb = dt("winb", [D, IN_COLS], BF16)
        self.woutb = dt("woutb", [D, D], BF16)
        self.wqb = dt("wqb", [D, 512], BF16)
        self.wkvb = dt("wkvb", [D, 1024], BF16)
        self.wob = dt("wob", [512, D], BF16)
        self.ymix = dt("ymix", [D, S], BF16)
        self.cscr = dt("cscr", [8, 12, S], BF16)
        self.x2buf = dt("x2buf", [S, D], F32)
        self.xbuf = dt("xbuf", [NE * CAP, D], BF16)
        self.ybuf = dt("ybuf", [NE * CAP, D], BF16)
        self.wg16 = dt("wg16", [NE, D, 512], BF16)
        self.wu16 = dt("wu16", [NE, D, 512], BF16)
        self.wd16 = dt("wd16", [NE, 512, D], BF16)
        if debug:
            self.dbg = {}

    def dbg_out(self, name, shape, dtype=F32):
        t = self.nc.dram_tensor("dbg_" + name, shape, dtype, kind="ExternalOutput").ap()
        self.dbg[name] = t
        return t

    def build(self, upto=99):
        nc = self.nc
        with ExitStack() as top:
            P = Prog(nc, top)
            self.P = P
            sbt = lambda st, name, shape, dt: st.enter_context(nc.sbuf_tensor(name, shape, dt))
            pst = lambda st, name, shape, dt=F32: st.enter_context(nc.psum_tensor(name, shape, dt))
            pv = sbt(top, "pv", [128, NPV], F32)
            pv2 = sbt(top, "pv2", [128, 24], F32)
            cb = sbt(top, "cb", [128, NCB], BF16)
            cf = sbt(top, "cf", [128, NCF], F32)
            nhalf = sbt(top, "nhalf", [128, 512], F32)
            phalf = sbt(top, "phalf", [128, 512], F32)
            self.pv, self.pv2, self.cb, self.cf, self.nhalf, self.phalf = pv, pv2, cb, cf, nhalf, phalf
            P.dma("sync", lambda h: h.dma_start(out=pv[:], in_=self.pvec[:, :]), writes=["pv"])
            P.dma("sync", lambda h: h.dma_start(out=cb[:], in_=self.cbf[:, :]), writes=["cb"])
            P.dma("sync", lambda h: h.dma_start(out=cf[:], in_=self.cf32[:, :]), writes=["cf"])
            self.epsc = sbt(top, "epsc", [128, 1], F32)
            P.op("gpsimd", lambda h: h.memset(self.epsc[:], EPS), writes=["epsc"])
            P.op("gpsimd", lambda h: h.memset(nhalf[:], -0.5), writes=["nhalf"])
            zt = sbt(top, "zt", [128, 2, D], BF16)
            P.op("gpsimd", lambda h: h.memset(zt[:], 0.0), writes=["zt"])
            self.zt = zt
            P.op("gpsimd", lambda h: h.memset(phalf[:], 0.5), writes=["phalf"])
            c = PV
            P.op("vector", lambda h: h.tensor_scalar(pv2[:, 0:4], pv[:, c["ba"]:c["ba"] + 4], 0.5, None, op0=ALU.mult), reads=["pv"], writes=["pv2a"])
            P.op("vector", lambda h: h.tensor_scalar(pv2[:, 4:8], pv[:, c["bx"]:c["bx"] + 4], 0.5, None, op0=ALU.mult), reads=["pv"], writes=["pv2b"])
            P.op("scalar", lambda h: h.activation(out=pv2[:, 8:12], in_=pv[:, c["apar"]:c["apar"] + 4], func=AF.Exp, scale=-1.0), reads=["pv"], writes=["pv2c"])
            P.op("scalar", lambda h: h.activation(out=pv2[:, 8:12], in_=pv2[:, 8:12], func=AF.Ln, bias=1.0), reads=["pv2c"], writes=["pv2c"])
            P.op("vector", lambda h: h.tensor_scalar(pv2[:, 8:12], pv2[:, 8:12], -4.0, None, op0=ALU.mult), reads=["pv2c"], writes=["pv2c"])
            P.op("vector", lambda h: h.tensor_scalar(pv2[:, 12:13], pv[:, c["gq2"]:c["gq2"] + 1], 0.125, None, op0=ALU.mult), reads=["pv"], writes=["pv2d"])
            P.op("vector", lambda h: h.tensor_scalar(pv2[:, 13:14], pv[:, c["mqg"]:c["mqg"] + 1], 128.0 ** -0.5, None, op0=ALU.mult), reads=["pv"], writes=["pv2e"])
            P.op("vector", lambda h: h.tensor_scalar(pv2[:, 14:15], pv[:, c["bf"]:c["bf"] + 1], -1.0, None, op0=ALU.mult), reads=["pv"], writes=["pv2f"])
            self.PVR = ["pv", "pv2a", "pv2b", "pv2c", "pv2d", "pv2e", "pv2f", "cb", "cf", "nhalf", "phalf"]

            self.knT = sbt(top, "knT", [128, 4, 256], BF16)
            self.vmem = sbt(top, "vmem", [128, 2, 512], BF16)
            self.cnti = sbt(top, "cnti", [1, 32], I32)
            self.preg = top.enter_context(nc.tensor.register("cntreg"))
            self.gates = sbt(top, "gates", [128, NT, 2], F32)
            self.idxs = sbt(top, "idxs", [128, NT, 2], I32)
            self.gsq = sbt(top, "gsq", [128, 8], BF16)
            gtmp = sbt(top, "gtmp", [128, 8], F32)
            P.op("vector", lambda h: h.tensor_scalar(pv2[:, 16:20], pv[:, c["gout"]:c["gout"] + 4], 0.5, None, op0=ALU.mult), reads=["pv"], writes=["pv2g"])
            P.op("vector", lambda h: h.tensor_tensor(gtmp[:], pv[:, c["gout"]:c["gout"] + 8], pv[:, c["gout"]:c["gout"] + 8], op=ALU.mult), reads=["pv"], writes=["gtmp"])
            P.op("vector", lambda h: h.reciprocal(gtmp[:], gtmp[:]), reads=["gtmp"], writes=["gtmp"])
            P.op("vector", lambda h: h.tensor_copy(self.gsq[:], gtmp[:]), reads=["gtmp"], writes=["gsq"])
            P.barrier()
            if upto >= 1:
                with ExitStack() as stA:
                    hT = sbt(stA, "hT", [128, 8, S], BF16)
                    self.hT = hT
                    self.phaseA1(stA)
                    P.barrier()
                    if upto >= 2:
                        self.phaseA2()
                        P.barrier()
                    if upto >= 3:
                        self.phaseA3()
                        P.barrier()
            if upto >= 4:
                self.phaseB()
                P.barrier()
            if upto >= 5:
                self.phaseC()
            P.barrier()
            if self.debug in ("A2", "A3"):
                o = self.dbg_out("ymix", [D, S], BF16)
                for r_ in range(8):
                    P.dma("sync", (lambda r_=r_: (lambda h: h.dma_start(out=o[r_ * 128:(r_ + 1) * 128, :], in_=self.ymix[r_ * 128:(r_ + 1) * 128, :])))(), writes=["dbg%d" % r_])
                P.barrier()
            P.emit()
        return nc

    def precast(self, e0, e1):
        P = self.P
        for e in range(e0, e1):
            P.dma_bg("gpsimd", (lambda e=e: (lambda h: h.dma_start(out=self.wg16[e], in_=self.wgate[e])))(), "w16_%d" % e)
            P.dma_bg("gpsimd", (lambda e=e: (lambda h: h.dma_start(out=self.wu16[e], in_=self.wup[e])))(), "w16_%d" % e)
            P.dma_bg("gpsimd", (lambda e=e: (lambda h: h.dma_start(out=self.wd16[e], in_=self.wdown[e])))(), "w16_%d" % e)

    def prep_weight(self, st, tag, src, dst, K, N, gcol):
        nc, P, pv = self.nc, self.P, self.pv
        kcs = K // 128
        srcv = src.rearrange("(kc p) n -> p kc n", p=128)
        dstv = dst.rearrange("(kc p) n -> p kc n", p=128)
        stg = [st.enter_context(nc.sbuf_tensor("stg%s%d" % (tag, i), [128, 8, 512], F32)) for i in range(2)]
        ob = [st.enter_context(nc.sbuf_tensor("ob%s%d" % (tag, i), [128, 8, 512], BF16)) for i in range(2)]
        ci = 0
        for c0 in range(0, N, 512):
            w = min(512, N - c0)
            b = ci % 2
            sn, on = "stg%s%d" % (tag, b), "ob%s%d" % (tag, b)
            P.dma("sync", (lambda b=b, c0=c0, w=w: (lambda h: h.dma_start(out=stg[b][:, 0:kcs, 0:w], in_=srcv[:, :, c0:c0 + w])))(), writes=[sn])
            for kc in range(kcs):
                if gcol is None:
                    if kc % 2 == 0:
                        P.op("vector", (lambda b=b, kc=kc, w=w: (lambda h: h.tensor_copy(ob[b][:, kc, 0:w], stg[b][:, kc, 0:w])))(), reads=[sn], writes=[on])
                    else:
                        P.op("scalar", (lambda b=b, kc=kc, w=w: (lambda h: h.activation(out=ob[b][:, kc, 0:w], in_=stg[b][:, kc, 0:w], func=AF.Copy)))(), reads=[sn], writes=[on])
                elif kc % 2 == 0:
                    P.op("vector", (lambda b=b, kc=kc, w=w: (lambda h: h.tensor_scalar(ob[b][:, kc, 0:w], stg[b][:, kc, 0:w], pv[:, gcol + kc:gcol + kc + 1], None, op0=ALU.mult)))(),
                         reads=[sn, "pv"], writes=[on])
                else:
                    P.op("scalar", (lambda b=b, kc=kc, w=w: (lambda h: h.activation(out=ob[b][:, kc, 0:w], in_=stg[b][:, kc, 0:w], func=AF.Copy, scale=pv[:, gcol + kc:gcol + kc + 1])))(),
                         reads=[sn, "pv"], writes=[on])
            P.dma("scalar", (lambda b=b, c0=c0, w=w: (lambda h: h.dma_start(out=dstv[:, :, c0:c0 + w], in_=ob[b][:, 0:kcs, 0:w])))(), reads=[on], writes=["dram_" + tag], semres=on)
            ci += 1

    def phase0(self):
        nc, P = self.nc, self.P
        with ExitStack() as st:
            zt = st.enter_context(nc.sbuf_tensor("zt", [128, 8, D], BF16))
            P.op("gpsimd", lambda h: h.memset(zt[:], 0.0), writes=["zt"])
            for r in range(NE * CAP // 1024):
                P.dma("scalar", (lambda r=r: (lambda h: h.dma_start(out=self.xbuf[r * 1024:(r + 1) * 1024, :].rearrange("(p a) n -> p a n", a=8), in_=zt[:])))(), reads=["zt"], writes=["xz%d" % r], semres="zt")
            P.barrier()
        with ExitStack() as st:
            self.prep_weight(st, "win", self.w_in, self.winb, D, IN_COLS, PV["gmix"])
        self.P.barrier()
        with ExitStack() as st:
            self.prep_weight(st, "wout", self.w_out, self.woutb, D, D, PV["gout"])
            self.prep_weight(st, "wq", self.mem_wq, self.wqb, D, 512, PV["gmemx"])
        self.P.barrier()
        with ExitStack() as st:
            self.prep_weight(st, "wkv", self.mem_wkv, self.wkvb, D, 1024, PV["gmem"])
            self.prep_weight(st, "wo", self.mem_wo, self.wob, 512, D, None)

    def norm_transpose(self, st, tag, src_fn, dstT, ntiles, pre=None):
        raise NotImplementedError

    def emit_kv(self, st2):
        nc, P = self.nc, self.P
        cb, pv, nhalf, knT, vmem = self.cb, self.pv, self.nhalf, self.knT, self.vmem
        sb2 = lambda name, shape, dt: st2.enter_context(nc.sbuf_tensor(name, shape, dt))
        pT_kv = st2.enter_context(nc.psum_tensor("pTB", [128, D], BF16))
        psQ_kv = st2.enter_context(nc.psum_tensor("psQB", [128, 512], F32))
        psN_kv = st2.enter_context(nc.psum_tensor("psNB", [128, 512], F32))
        wkvb = sb2("wkvb_s", [128, 8, D], BF16)
        mt_ = [sb2("memt%d" % i, [128, D], F32) for i in range(2)]
        mb_ = [sb2("memb%d" % i, [128, D], BF16) for i in range(2)]
        memT = sb2("memT", [128, 8, 256], BF16)
        junk_kv = sb2("junkM", [128, D], BF16)
        mstat = sb2("mstat", [128, 6], F32)
        sqm = sb2("sqm", [128, 256], BF16)
        vrm = sb2("vrm", [128, 256], F32)
        rsm = sb2("rsm", [128, 256], F32)
        P.dma("gpsimd", lambda h: h.dma_start(out=wkvb[:], in_=self.mem_wkv.rearrange("(kc p) n -> p kc n", p=128)), writes=["wkvb"])
        gmb = sb2("gmb", [128, D], F32)
        P.dma("sync", lambda h: h.dma_start(out=gmb[:], in_=self.bvec[:, BV_GMEM:BV_GMEM + D].partition_broadcast(128)), writes=["gmb"])
        for i in range(2):
            P.dma("sync", (lambda i=i: (lambda h: h.dma_start(out=mt_[i][:], in_=self.mem[i * 128:(i + 1) * 128, :])))(), writes=["memt%d" % i])
            P.op("scalar", (lambda i=i: (lambda h: h.activation(out=junk_kv[:], in_=mt_[i][:], func=AF.Square, accum_out=mstat[:, i:i + 1])))(), reads=["memt%d" % i], writes=["junkM", "ms%d" % i])
            P.op("vector", (lambda i=i: (lambda h: h.tensor_scalar(mstat[:, 2 + i:3 + i], mstat[:, i:i + 1], 1.0 / D, EPS, op0=ALU.mult, op1=ALU.add)))(), reads=["ms%d" % i], writes=["mv%d" % i])
            P.op("gpsimd", (lambda i=i: (lambda h: h.tensor_tensor(mstat[:, 4 + i:5 + i], mstat[:, 2 + i:3 + i], nhalf[:, 0:1], op=ALU.pow)))(), reads=["mv%d" % i, "nhalf"], writes=["mr%d" % i])
            P.op("vector", (lambda i=i: (lambda h: h.scalar_tensor_tensor(out=mb_[i][:], in0=mt_[i][:], scalar=mstat[:, 4 + i:5 + i], in1=gmb[:], op0=ALU.mult, op1=ALU.mult)))(), reads=["memt%d" % i, "mr%d" % i, "gmb"], writes=["memb%d" % i])
            for kc in range(8):
                P.op("tensor", (lambda kc=kc, i=i: (lambda h: h.transpose(pT_kv[:, kc * 128:(kc + 1) * 128], mb_[i][:, kc * 128:(kc + 1) * 128], cb[:, CB_ID:CB_ID + 128])))(),
                     reads=["memb%d" % i, "cb"], writes=["pTB"], inc=(kc == 7))
            P.op("vector", (lambda i=i: (lambda h: h.tensor_copy(memT[:, :, i * 128:(i + 1) * 128], pT_kv[:].rearrange("p (k t) -> p k t", k=8))))(), reads=["pTB"], writes=["memT%d" % i])
        mres = ["memT0", "memT1"]
        for hd in range(4):
            for kc in range(8):
                P.op("tensor", (lambda kc=kc, hd=hd: (lambda h: h.matmul(psQ_kv[:, 0:256], wkvb[:, kc, hd * 128:(hd + 1) * 128], memT[:, kc, :], start=(kc == 0), stop=(kc == 7))))(),
                     reads=["wkvb"] + mres, writes=["psQB"], inc=(kc == 7))
            P.op("scalar", lambda h: h.activation(out=sqm[:], in_=psQ_kv[:, 0:256], func=AF.Square), reads=["psQB"], writes=["sqm"])
            P.op("tensor", lambda h: h.matmul(psN_kv[:, 0:256], cb[:, CB_ONE:CB_ONE + 128], sqm[:], start=True, stop=True), reads=["cb", "sqm"], writes=["psNB"])
            P.op("scalar", lambda h: h.activation(out=vrm[:], in_=psN_kv[:, 0:256], func=AF.Sqrt, scale=1.0 / 128, bias=self.epsc[:, 0:1]), reads=["psNB", "epsc"], writes=["vrm"])
            P.op("vector", lambda h: h.reciprocal(rsm[:], vrm[:]), reads=["vrm"], writes=["rsm"])
            P.op("vector", (lambda hd=hd: (lambda h: h.scalar_tensor_tensor(out=knT[:, hd, :], in0=psQ_kv[:, 0:256], scalar=pv[:, PV["mkg"]:PV["mkg"] + 1], in1=rsm[:], op0=ALU.mult, op1=ALU.mult)))(),
                 reads=["psQB", "rsm", "pv"], writes=["knT"])
        for kt in range(2):
            for kc in range(8):
                P.op("tensor", (lambda kc=kc, kt=kt: (lambda h: h.matmul(psQ_kv[:], memT[:, kc, kt * 128:(kt + 1) * 128], wkvb[:, kc, 512:1024], start=(kc == 0), stop=(kc == 7))))(),
                     reads=["wkvb"] + mres, writes=["psQB"], inc=(kc == 7))
            P.op("vector", (lambda kt=kt: (lambda h: h.tensor_copy(vmem[:, kt, :], psQ_kv[:])))(), reads=["psQB"], writes=["vmem"])


    def emit_forget(self, st2):
        nc, P = self.nc, self.P
        hT, pv2 = self.hT, self.pv2
        winv = self.w_in.rearrange("(kc p) n -> p kc n", p=128)
        wf = st2.enter_context(nc.sbuf_tensor("wf", [128, 8, 8], BF16))
        psQ = st2.enter_context(nc.psum_tensor("psQf", [128, 512], F32))
        P.dma("gpsimd", lambda h: h.dma_start(out=wf[:], in_=winv[:, :, 2560:2568]), writes=["wf"])
        sb2 = lambda name, shape, dt: st2.enter_context(nc.sbuf_tensor(name, shape, dt))
        HW = 2048
        fe = sb2("fe", [8, HW], F32)
        fm0 = sb2("fm0", [8, HW], F32)
        fm = [fm0, fm0]
        fcar = sb2("fcar", [8, 1], F32)
        fr = fe
        pcs = [sb2("pc%d" % i, [8, HW], BF16) for i in range(7)]
        P.op("gpsimd", lambda h: h.memset(pcs[6][:], 1.0), writes=["pc6"])

        def block(tb):
            if True:
                tq = tb % 4
                t0 = tb * 512
                for kc in range(8):
                    P.op("tensor", (lambda kc=kc, t0=t0: (lambda h: h.matmul(psQ[0:8, :], wf[:, kc, :], hT[:, kc, t0:t0 + 512], start=(kc == 0), stop=(kc == 7))))(),
                         reads=["wf"] + ["hT%d" % (tb * 4 + j) for j in range(4)], writes=["psQf"], inc=(kc == 7))
                P.op("scalar", (lambda tq=tq: (lambda h: h.activation(out=fe[:, tq * 512:(tq + 1) * 512], in_=psQ[0:8, :], func=AF.Exp, scale=-1.0, bias=pv2[0:8, 14:15])))(),
                     reads=["psQf", "pv2f"], writes=["fe"])
        def half(hf):
            c0 = hf * HW
            P.op("scalar", lambda h: h.activation(out=fe[:], in_=fe[:], func=AF.Ln, bias=1.0), reads=["fe"], writes=["fe"])
            if hf == 0:
                P.op("vector", lambda h: h.tensor_tensor_scan(out=fm[0][:], data0=fe[:], data1=fe[:], initial=0.0, op0=ALU.add, op1=ALU.max), reads=["fe"], writes=["fm0"])
            else:
                P.op("vector", lambda h: h.tensor_copy(fcar[:], fm0[:, HW - 1:HW]), reads=["fm0"], writes=["fcar"])
                P.op("vector", lambda h: h.tensor_tensor_scan(out=fm0[:], data0=fe[:], data1=fe[:], initial=fcar[:, 0:1], op0=ALU.add, op1=ALU.max), reads=["fe", "fcar"], writes=["fm0"])
            fmh = fm[hf]
            fmn = "fm0"
            P.op("scalar", (lambda fmh=fmh: (lambda h: h.activation(out=pcs[0][:], in_=fmh[:], func=AF.Copy)))(), reads=[fmn], writes=["pc0"])
            P.op("vector", (lambda fmh=fmh: (lambda h: h.tensor_tensor(fr[:], fmh[:], pcs[0][:], op=ALU.subtract)))(), reads=[fmn, "pc0", "fe"], writes=["fe"])
            P.op("scalar", lambda h: h.activation(out=pcs[1][:], in_=fr[:], func=AF.Copy), reads=["fe"], writes=["pc1"])
            P.op("vector", lambda h: h.tensor_tensor(fr[:], fr[:], pcs[1][:], op=ALU.subtract), reads=["fe", "pc1"], writes=["fe"])
            P.op("scalar", lambda h: h.activation(out=pcs[2][:], in_=fr[:], func=AF.Copy), reads=["fe"], writes=["pc2"])
            for j in range(3):
                P.op("scalar", (lambda j=j: (lambda h: h.activation(out=pcs[3 + j][:], in_=pcs[j][:], func=AF.Copy, scale=-1.0)))(), reads=["pc%d" % j], writes=["pc%d" % (3 + j)])
            rowsrc = [3, 4, 5, 6, 6, 6, 6, 6, 6, 0, 1, 2]
            for row in range(12):
                src = rowsrc[row]
                P.dma("scalar", (lambda row=row, src=src, c0=c0: (lambda h: h.dma_start(out=self.cscr[:, row, c0:c0 + HW], in_=pcs[src][:])))(),
                      reads=["pc%d" % src], writes=["cscr_%d_%d" % (row, hf)], semres="pc%d" % src)
            if self.debug == "A3f":
                o = self.dbg_out("fm%d" % hf, [8, HW])
                P.dma("sync", (lambda o=o, fmh=fmh: (lambda h: h.dma_start(out=o[:, :], in_=fmh[:])))(), reads=[fmn], writes=["dbgf%d" % hf])
        return block, half


    def phaseA1(self, stA):
        nc, P = self.nc, self.P
        hT, cb, nhalf = self.hT, self.cb, self.nhalf
        with ExitStack() as st:
            xt = [st.enter_context(nc.sbuf_tensor("xt%d" % i, [128, D], F32)) for i in range(3)]
            hb = [st.enter_context(nc.sbuf_tensor("hb%d" % i, [128, D], BF16)) for i in range(2)]
            junk = st.enter_context(nc.sbuf_tensor("junkA", [128, D], BF16))
            stat = st.enter_context(nc.sbuf_tensor("statA", [128, 3 * NT], F32))
            gmixb = st.enter_context(nc.sbuf_tensor("gmixb", [128, D], F32))
            P.dma("sync", lambda h: h.dma_start(out=gmixb[:], in_=self.bvec[:, BV_GMIX:BV_GMIX + D].partition_broadcast(128)), writes=["gmixb"])
            pT = [st.enter_context(nc.psum_tensor("pTA%d" % i, [128, D], BF16)) for i in range(2)]
            fblock, fhalf = self.emit_forget(st)
            units = []
            for i in range(NT):
                def mk(i=i):
                    xb, b2 = i % 3, i % 2
                    xn, hn, pn = "xt%d" % xb, "hb%d" % b2, "pTA%d" % b2

                    def s0():
                        P.dma("sync", lambda h: h.dma_start(out=xt[xb][:], in_=self.x[i * 128:(i + 1) * 128, :]), writes=[xn])

                    def s1():
                        P.op("scalar", lambda h: h.activation(out=junk[:], in_=xt[xb][:], func=AF.Square, accum_out=stat[:, i:i + 1]), reads=[xn], writes=["junkA", "ssq%d" % i])
                        P.op("vector", lambda h: h.tensor_scalar(stat[:, NT + i:NT + i + 1], stat[:, i:i + 1], 1.0 / D, EPS, op0=ALU.mult, op1=ALU.add), reads=["ssq%d" % i], writes=["var%d" % i])
                        P.op("gpsimd", lambda h: h.tensor_tensor(stat[:, 2 * NT + i:2 * NT + i + 1], stat[:, NT + i:NT + i + 1], nhalf[:, 0:1], op=ALU.pow), reads=["var%d" % i, "nhalf"], writes=["rstd%d" % i])

                    def s2():
                        P.op("vector", lambda h: h.scalar_tensor_tensor(out=hb[b2][:], in0=xt[xb][:], scalar=stat[:, 2 * NT + i:2 * NT + i + 1], in1=gmixb[:], op0=ALU.mult, op1=ALU.mult),
                             reads=[xn, "rstd%d" % i, "gmixb"], writes=[hn])

                    def s3():
                        for kc in range(8):
                            P.op("tensor", (lambda kc=kc: (lambda h: h.transpose(pT[b2][:, kc * 128:(kc + 1) * 128], hb[b2][:, kc * 128:(kc + 1) * 128], cb[:, CB_ID:CB_ID + 128])))(),
                                 reads=[hn, "cb"], writes=[pn], inc=(kc == 7))

                    def s4():
                        if i % 2 == 0:
                            P.op("scalar", lambda h: h.activation(out=hT[:, :, i * 128:(i + 1) * 128], in_=pT[b2][:].rearrange("p (k t) -> p k t", k=8), func=AF.Copy), reads=[pn], writes=["hT%d" % i])
                        else:
                            P.op("vector", lambda h: h.tensor_copy(hT[:, :, i * 128:(i + 1) * 128], pT[b2][:].rearrange("p (k t) -> p k t", k=8)), reads=[pn], writes=["hT%d" % i])
                        if i == 5:
                            self.emit_kv(st)
                        if i % 4 == 3:
                            fblock(i // 4)
                        if i % 16 == 15:
                            fhalf(i // 16)
                    return [s0, s1, s2, s3, s4]
                units.append(mk())
            run_pipeline(units)
            if self.debug == "A1":
                o = self.dbg_out("hT", [128, 8 * S], BF16)
                P.dma("sync", lambda h: h.dma_start(out=o[:, :], in_=hT[:].rearrange("p k t -> p (k t)")), reads=["hT%d" % i for i in range(NT)], writes=["dbg"])

    def phaseA2(self):
        nc, P = self.nc, self.P
        hT, cb, pv, pv2 = self.hT, self.cb, self.pv, self.pv2
        winv = self.w_in.rearrange("(kc p) n -> p kc n", p=128)
        with ExitStack() as st:
            sb = lambda name, shape, dt: st.enter_context(nc.sbuf_tensor(name, shape, dt))
            lw = sb("lw", [128, 1024], F32)
            lwb = sb("lwb", [128, 1024], BF16)
            wu = [sb("wu%d" % i, [128, 8, 128], BF16) for i in range(2)]
            wg = [sb("wg%d" % i, [128, 8, 128], BF16) for i in range(2)]
            ub = [sb("ub%d" % i, [128, 515], F32) for i in range(2)]
            yb = sb("yb0", [128, S], BF16)
            aaF = sb("aaF", [128, S], F32)
            t1F = sb("t1F", [128, S], F32)
            omF = sb("omF", [128, S], F32)
            geF = sb("geF", [128, S], F32)
            names = ["xc", "tr", "ti", "gs", "g2"]
            T = {n: [sb(n + "%d" % i, [128, 512], F32) for i in range(2)] for n in names}
            xcb = [sb("xcb%d" % i, [128, 512], BF16) for i in range(2)]
            psU = [st.enter_context(nc.psum_tensor("psU%d" % i, [128, 512], F32)) for i in range(2)]
            psG = [st.enter_context(nc.psum_tensor("psG%d" % i, [128, 512], F32)) for i in range(2)]
            psR = [st.enter_context(nc.psum_tensor("psR%d" % i, [128, 512], F32)) for i in range(2)]
            psI = [st.enter_context(nc.psum_tensor("psI%d" % i, [128, 512], F32)) for i in range(2)]
            P.dma("sync", lambda h: h.dma_start(out=lw[:], in_=self.lruw[:, :]), writes=["lw"])
            P.op("vector", lambda h: h.tensor_copy(lwb[:], lw[:]), reads=["lw"], writes=["lwb"])
            def load_ct(ct):
                cp = ct % 2
                P.dma("gpsimd", (lambda ct=ct, cp=cp: (lambda h: h.dma_start(out=wu[cp][:], in_=winv[:, :, ct * 128:(ct + 1) * 128])))(), writes=["wu%d" % cp])
                P.dma("gpsimd", (lambda ct=ct, cp=cp: (lambda h: h.dma_start(out=wg[cp][:], in_=winv[:, :, 512 + ct * 128:512 + (ct + 1) * 128])))(), writes=["wg%d" % cp])
            load_ct(0)
            load_ct(1)
            allb = lambda n: [n + "%d" % t for t in range(NB)]
            units = []
            for ct in range(4):
                for tb in range(NB):
                    def mk(ct=ct, tb=tb):
                        it = ct * NB + tb
                        b = it % 2
                        cp = ct % 2
                        cwc = PV["convw"] + ct * 4
                        t0 = tb * 512
                        blk = slice(t0, t0 + 512)
                        hres = ["hT%d" % (tb * 4 + j) for j in range(4)]
                        R_ = lambda n: n + "%d" % b
                        xc, tr, ti, gs, g2 = (T[n][b] for n in ["xc", "tr", "ti", "gs", "g2"])
                        ur = [R_("ubm"), R_("ubh")]

                        def s0():
                            if tb == 0 and ct >= 1 and ct + 1 < 4:
                                load_ct(ct + 1)
                            if tb % 2 == 0:
                                self.precast(4 * ct + tb // 2, 4 * ct + tb // 2 + 1)
                            for kc in range(8):
                                P.op("tensor", (lambda kc=kc: (lambda h: h.matmul(psU[b][:], wu[cp][:, kc, :], hT[:, kc, t0:t0 + 512], start=(kc == 0), stop=(kc == 7))))(),
                                     reads=["wu%d" % cp] + hres, writes=[R_("psU")], inc=(kc == 7))
                            for kc in range(8):
                                P.op("tensor", (lambda kc=kc: (lambda h: h.matmul(psG[b][:], wg[cp][:, kc, :], hT[:, kc, t0:t0 + 512], start=(kc == 0), stop=(kc == 7))))(),
                                     reads=["wg%d" % cp] + hres, writes=[R_("psG")], inc=(kc == 7))

                        def s1():
                            P.op("scalar", lambda h: h.activation(out=ub[b][:, 3:515], in_=psU[b][:], func=AF.Copy), reads=[R_("psU")], writes=[R_("ubm")])
                            yield
                            if tb == 0:
                                P.op("gpsimd", lambda h: h.memset(ub[b][:, 0:3], 0.0), writes=[R_("ubh")])
                            else:
                                P.op("gpsimd", lambda h: h.tensor_copy(ub[b][:, 0:3], ub[1 - b][:, 512:515]), reads=["ubm%d" % (1 - b)], writes=[R_("ubh")])
                            P.op("scalar", lambda h: h.activation(out=gs[:], in_=psG[b][:], func=AF.Copy), reads=[R_("psG")], writes=[R_("gs")])
                            yield
                            P.op("scalar", lambda h: h.activation(out=g2[:], in_=psG[b][:], func=AF.Square), reads=[R_("psG")], writes=[R_("g2")])
                            yield
                            P.op("gpsimd", lambda h: h.tensor_scalar(g2[:], g2[:], 0.044715, 1.0, op0=ALU.mult, op1=ALU.add), reads=[R_("g2")], writes=[R_("g2")])
                            yield
                            P.op("vector", lambda h: h.tensor_scalar(xc[:], ub[b][:, 0:512], pv[:, cwc:cwc + 1], pv[:, PV["convb"] + ct:PV["convb"] + ct + 1], op0=ALU.mult, op1=ALU.add),
                                 reads=ur + ["pv"], writes=[R_("xc")])
                            for tap in range(1, 4):
                                P.op("vector", (lambda tap=tap: (lambda h: h.scalar_tensor_tensor(out=xc[:], in0=ub[b][:, tap:tap + 512], scalar=pv[:, cwc + tap:cwc + tap + 1], in1=xc[:], op0=ALU.mult, op1=ALU.add)))(),
                                     reads=ur + ["pv", R_("xc")], writes=[R_("xc")])
                            P.op("scalar", lambda h: h.activation(out=xcb[b][:], in_=xc[:], func=AF.Copy), reads=[R_("xc")], writes=[R_("xcb")])
                            yield
                            P.op("vector", lambda h: h.tensor_tensor(g2[:], g2[:], gs[:], op=ALU.mult), reads=[R_("g2"), R_("gs")], writes=[R_("g2")])
                            yield
                            P.op("scalar", lambda h: h.activation(out=g2[:], in_=g2[:], func=AF.Tanh, scale=0.7978845608028654), reads=[R_("g2")], writes=[R_("g2")])
                            yield
                            P.op("vector", lambda h: h.scalar_tensor_tensor(out=geF[:, blk], in0=g2[:], scalar=1.0, in1=gs[:], op0=ALU.add, op1=ALU.mult), reads=[R_("g2"), R_("gs")], writes=["geF%d" % tb])
                            yield

                        def s2():
                            P.op("tensor", lambda h: h.matmul(psR[b][:], lwb[:, ct * 256:ct * 256 + 128], xcb[b][:], start=True, stop=True), reads=["lwb", R_("xcb")], writes=[R_("psR")])
                            yield
                            P.op("tensor", lambda h: h.matmul(psI[b][:], lwb[:, ct * 256 + 128:ct * 256 + 256], xcb[b][:], start=True, stop=True), reads=["lwb", R_("xcb")], writes=[R_("psI")])
                            yield
                            P.op("scalar", lambda h: h.activation(out=tr[:], in_=psR[b][:], func=AF.Tanh, scale=0.5, bias=pv2[:, ct:ct + 1]), reads=[R_("psR"), "pv2a"], writes=[R_("tr")])
                            yield
                            P.op("scalar", lambda h: h.activation(out=ti[:], in_=psI[b][:], func=AF.Tanh, scale=0.5, bias=pv2[:, 4 + ct:5 + ct]), reads=[R_("psI"), "pv2b"], writes=[R_("ti")])
                            yield
                            P.op("scalar", lambda h: h.activation(out=aaF[:, blk], in_=tr[:], func=AF.Exp, scale=pv2[:, 8 + ct:9 + ct], bias=pv2[:, 8 + ct:9 + ct]), reads=[R_("tr"), "pv2c"], writes=["aaF%d" % tb])
                            yield
                            P.op("scalar", lambda h: h.activation(out=omF[:, blk], in_=aaF[:, blk], func=AF.Square), reads=["aaF%d" % tb], writes=["omF%d" % tb])
                            yield
                            P.op("gpsimd", lambda h: h.tensor_scalar(omF[:, blk], omF[:, blk], -1.0, 1.0, op0=ALU.mult, op1=ALU.add), reads=["omF%d" % tb], writes=["omF%d" % tb])
                            yield
                            P.op("vector", lambda h: h.scalar_tensor_tensor(out=t1F[:, blk], in0=ti[:], scalar=1.0, in1=xc[:], op0=ALU.add, op1=ALU.mult), reads=[R_("ti"), R_("xc")], writes=["t1F%d" % tb])
                            yield
                            if tb == NB - 1:
                                P.op("scalar", lambda h: h.activation(out=omF[:], in_=omF[:], func=AF.Sqrt), reads=allb("omF"), writes=allb("omF"))
                                P.op("vector", lambda h: h.scalar_tensor_tensor(out=omF[:], in0=t1F[:], scalar=0.5, in1=omF[:], op0=ALU.mult, op1=ALU.mult), reads=allb("t1F") + allb("omF"), writes=allb("omF"))
                                P.op("vector", lambda h: h.tensor_tensor_scan(out=t1F[:], data0=aaF[:], data1=omF[:], initial=0.0, op0=ALU.mult, op1=ALU.add), reads=allb("aaF") + allb("omF"), writes=allb("t1F"))
                                P.op("vector", lambda h: h.scalar_tensor_tensor(out=yb[:], in0=geF[:], scalar=pv2[:, 16 + ct:17 + ct], in1=t1F[:], op0=ALU.mult, op1=ALU.mult), reads=allb("geF") + allb("t1F") + ["pv2g"], writes=["yb0"])
                                P.dma("scalar", lambda h: h.dma_start(out=self.ymix[ct * 128:(ct + 1) * 128, :], in_=yb[:]), reads=["yb0"], writes=["ymix_l%d" % ct], semres="yb0")
                        def s2_full():
                            for _ in s2():
                                pass
                        return [s0, s1, s2_full if tb == NB - 1 else s2]
                    units.append(mk())
            run_pipeline(units)

    def phaseA3(self):
        nc, P = self.nc, self.P
        hT, cb, pv, pv2, nhalf = self.hT, self.cb, self.pv, self.pv2, self.nhalf
        winv = self.w_in.rearrange("(kc p) n -> p kc n", p=128)
        QC, KC, VC, FC = 1024, 1536, 2048, 2560
        with ExitStack() as st:
            sb = lambda name, shape, dt: st.enter_context(nc.sbuf_tensor(name, shape, dt))
            ps = lambda name, shape, dt=F32: st.enter_context(nc.psum_tensor(name, shape, dt))
            vtok = sb("vtok", [128, NT, 512], BF16)
            wv = sb("wv", [128, 8, 512], BF16)
            psS = [ps("psS%d" % i, [128, 2, 512]) for i in range(2)]
            psO = [ps("psO%d" % i, [128, 512]) for i in range(2)]
            psQ = ps("psQ", [128, 512])
            psN = ps("psN", [128, 512])
            P.dma("gpsimd", lambda h: h.dma_start(out=wv[:], in_=winv[:, :, VC:VC + 512]), writes=["wv"])
            for i in range(NT):
                pq = psS[i % 2]
                pn = "psS%d_0" % (i % 2)
                for kc in range(8):
                    P.op("tensor", (lambda kc=kc, i=i, pq=pq: (lambda h: h.matmul(pq[:, 0, :], hT[:, kc, i * 128:(i + 1) * 128], wv[:, kc, :], start=(kc == 0), stop=(kc == 7))))(),
                         reads=["wv", "hT%d" % i], writes=[pn], inc=(kc == 7))
                eng = "scalar" if i % 2 == 0 else "vector"
                if eng == "scalar":
                    P.op("scalar", (lambda i=i, pq=pq: (lambda h: h.activation(out=vtok[:, i, :], in_=pq[:, 0, :], func=AF.Copy)))(), reads=[pn], writes=["vtok%d" % i])
                else:
                    P.op("vector", (lambda i=i, pq=pq: (lambda h: h.tensor_copy(vtok[:, i, :], pq[:, 0, :])))(), reads=[pn], writes=["vtok%d" % i])
            sb = lambda name, shape, dt: st.enter_context(nc.sbuf_tensor(name, shape, dt))
            wqk = [sb("wqk%d" % i, [128, 8, 256], BF16) for i in range(2)]
            qa = [sb("qa%d" % i, [128, S], BF16) for i in range(2)]
            ka = [sb("ka%d" % i, [128, S], BF16) for i in range(2)]
            va = [sb("va%d" % i, [128, NT, 128], BF16) for i in range(2)]
            yf = [sb("yf0", [128, S], BF16)] * 2
            sq = [sb("sq%d" % i, [128, 512], BF16) for i in range(2)]
            vr = [sb("vr%d" % i, [128, 512], F32) for i in range(2)]
            rs = [sb("rs%d" % i, [128, 512], F32) for i in range(2)]
            pt = [sb("pt%d" % i, [128, 2, 512], BF16) for i in range(3)]
            rl = [sb("rl%d" % i, [64, 512], F32) for i in range(2)]
            for i in range(2):
                P.op("gpsimd", (lambda i=i: (lambda h: h.memset(va[i][:, :, 64:128], 1.0)))(), writes=["va1_%d" % i])
            nrm = 0
            pti = 0
            oi = 0
            for p in range(4):
                pp = p % 2
                P.dma("gpsimd", (lambda p=p, pp=pp: (lambda h: h.dma_start(out=wqk[pp][:, :, 0:128], in_=winv[:, :, QC + p * 128:QC + (p + 1) * 128])))(), writes=["wqk%d" % pp])
                P.dma("gpsimd", (lambda p=p, pp=pp: (lambda h: h.dma_start(out=wqk[pp][:, :, 128:256], in_=winv[:, :, KC + p * 128:KC + (p + 1) * 128])))(), writes=["wqk%d" % pp])
                for hp in range(2):
                    hd = 2 * p + hp
                    P.dma("sync", (lambda hp=hp, hd=hd: (lambda h: h.dma_start(out=qa[hp][64:70, :], in_=self.cscr[hd, 0:6, :])))(), writes=["qa%d_aug" % hp])
                    P.dma("sync", (lambda hp=hp, hd=hd: (lambda h: h.dma_start(out=ka[hp][64:70, :], in_=self.cscr[hd, 6:12, :])))(), writes=["ka%d_aug" % hp])
                    P.op("gpsimd", (lambda hp=hp, hd=hd: (lambda h: h.tensor_copy(va[hp][:, :, 0:64], vtok[:, :, hd * 64:(hd + 1) * 64])))(),
                         reads=["vtok%d" % i for i in range(NT)], writes=["va0_%d" % hp])
                if p == 1:
                    for r in range(NE * CAP // 256):
                        P.dma_bg("gpsimd", (lambda r=r: (lambda h: h.dma_start(out=self.xbuf[r * 256:(r + 1) * 256, :].rearrange("(p a) n -> p a n", a=2), in_=self.zt[:])))(), "xz")
                PQ4 = [(psQ[:], "psQ"), (psS[0][:, 0, :], "psS0_0"), (psS[0][:, 1, :], "psS0_1"), (psS[1][:, 0, :], "psS1_0")]
                PN2 = [(psN[:], "psN"), (psS[1][:, 1, :], "psS1_1")]
                units = []
                for tb in range(NB):
                    for which in range(2):
                        def mk(tb=tb, which=which, nrm=nrm, pp=pp):
                            t0 = tb * 512
                            hres = ["hT%d" % (tb * 4 + j) for j in range(4)]
                            nb = nrm % 2
                            pq, pqn = PQ4[nrm % 4]
                            pn_, pnn = PN2[nrm % 2]
                            dst = qa if which == 0 else ka
                            gcol = pv2[:, 12:13] if which == 0 else pv[:, PV["gk2"]:PV["gk2"] + 1]
                            gres = "pv2d" if which == 0 else "pv"

                            def s0():
                                for kc in range(8):
                                    P.op("tensor", (lambda kc=kc: (lambda h: h.matmul(pq, wqk[pp][:, kc, which * 128:(which + 1) * 128], hT[:, kc, t0:t0 + 512], start=(kc == 0), stop=(kc == 7))))(),
                                         reads=["wqk%d" % pp] + hres, writes=[pqn], inc=(kc == 7))

                            def s1():
                                P.op("scalar", lambda h: h.activation(out=sq[nb][:], in_=pq, func=AF.Square), reads=[pqn], writes=["sq%d" % nb])

                            def s1b():
                                P.op("tensor", lambda h: h.matmul(pn_, cb[:, CB_BO:CB_BO + 128], sq[nb][:], start=True, stop=True), reads=["cb", "sq%d" % nb], writes=[pnn])

                            def s2():
                                P.op("scalar", lambda h: h.activation(out=vr[nb][:], in_=pn_, func=AF.Ln, scale=1.0 / 64, bias=self.epsc[:, 0:1]), reads=[pnn, "epsc"], writes=["vr%d" % nb])
                                P.op("scalar", lambda h: h.activation(out=rs[nb][:], in_=vr[nb][:], func=AF.Exp, scale=-0.5), reads=["vr%d" % nb], writes=["rs%d" % nb])
                                for hp in range(2):
                                    P.op("vector", (lambda hp=hp: (lambda h: h.scalar_tensor_tensor(out=dst[hp][0:64, t0:t0 + 512], in0=pq[hp * 64:(hp + 1) * 64, :], scalar=gcol[hp * 64:(hp + 1) * 64, :], in1=rs[nb][hp * 64:(hp + 1) * 64, :], op0=ALU.mult, op1=ALU.mult)))(),
                                         reads=[pqn, "rs%d" % nb, gres], writes=[("qa%d_%d" if which == 0 else "ka%d_%d") % (hp, tb)])
                            return [s0, s1, s1b, s2]
                        units.append(mk())
                        nrm += 1
                run_pipeline(units)
                if self.debug == "A3q" and p == 0:
                    o = self.dbg_out("qa", [128, S], BF16)
                    o2 = self.dbg_out("ka", [128, S], BF16)
                    P.dma("sync", lambda h: h.dma_start(out=o[:, :], in_=qa[0][:]), reads=["qa0_%d" % t for t in range(NB)] + ["qa0_aug"], writes=["dbg"])
                    P.dma("sync", lambda h: h.dma_start(out=o2[:, :], in_=ka[0][:]), reads=["ka0_%d" % t for t in range(NB)] + ["ka0_aug"], writes=["dbg2"])
                units = []
                for hp in range(2):
                    for j in range(NB):
                        ob = oi % 2
                        oi += 1
                        nkt = 4 * j + 4
                        for g in range(0, nkt, 2):
                            def mk(hp=hp, j=j, g=g, ob=ob, nkt=nkt, pti=pti, pp=pp, p=p, gi=len(units)):
                                q0 = j * 512
                                sbi = pti % 2
                                ptb = pti % 3
                                qres = ["qa%d_%d" % (hp, j), "qa%d_aug" % hp]

                                cst = [max(0, 128 * (g + u - 4 * j)) for u in range(2)]
                                ce = min(cst)

                                def s0():
                                    if gi % 36 == 0:
                                        self.precast(16 + 4 * p + gi // 36, 16 + 4 * p + gi // 36 + 1)
                                    for u in range(2):
                                        i = g + u
                                        m = i - 4 * j
                                        c0 = cst[u]
                                        kres = ["ka%d_%d" % (hp, i // 4), "ka%d_aug" % hp]
                                        last = (m < 0)
                                        P.op("tensor", (lambda u=u, i=i, last=last, c0=c0: (lambda h: h.matmul(psS[sbi][:, u, c0:512], ka[hp][0:70, i * 128:(i + 1) * 128], qa[hp][0:70, q0 + c0:q0 + 512], start=True, stop=last)))(),
                                             reads=kres + qres, writes=["psS%d_%d" % (sbi, u)], inc=last)
                                        if m >= 0:
                                            P.op("tensor", (lambda u=u, c0=c0: (lambda h: h.matmul(psS[sbi][:, u, c0:c0 + 128], cb[:, CB_ID:CB_ID + 128], cb[:, CB_MASK:CB_MASK + 128], start=False, stop=True)))(),
                                                 reads=["cb"], writes=["psS%d_%d" % (sbi, u)], inc=True)

                                def s1():
                                    P.op("scalar", lambda h: h.activation(out=pt[ptb][:, :, ce:512], in_=psS[sbi][:, :, ce:512], func=AF.Exp), reads=["psS%d_0" % sbi, "psS%d_1" % sbi], writes=["pt%d" % ptb])

                                def s2():
                                    for u in range(2):
                                        i = g + u
                                        c0 = cst[u]
                                        P.op("tensor", (lambda u=u, i=i, c0=c0: (lambda h: h.matmul(psO[ob][:, c0:512], va[hp][:, i, :], pt[ptb][:, u, c0:512], start=(i == 0), stop=(i == nkt - 1))))(),
                                             reads=["va0_%d" % hp, "va1_%d" % hp, "pt%d" % ptb], writes=["psO%d" % ob], inc=(u == 1))
                                    if g + 2 >= nkt:
                                        P.op("vector", lambda h: h.reciprocal(rl[ob][:], psO[ob][64:128, :]), reads=["psO%d" % ob], writes=["rl%d" % ob])
                                        P.op("vector", lambda h: h.scalar_tensor_tensor(out=yf[pp][hp * 64:(hp + 1) * 64, q0:q0 + 512], in0=psO[ob][0:64, :], scalar=pv[0:64, PV["gfx"] + 2 * p + hp:PV["gfx"] + 2 * p + hp + 1], in1=rl[ob][:], op0=ALU.mult, op1=ALU.mult),
                                             reads=["psO%d" % ob, "rl%d" % ob, "pv"], writes=["yf0"])
                                return [s0, s1, s2]
                            units.append(mk())
                            pti += 1
                run_pipeline(units)
                P.dma("scalar", (lambda p=p, pp=pp: (lambda h: h.dma_start(out=self.ymix[512 + p * 128:512 + (p + 1) * 128, :], in_=yf[pp][:])))(), reads=["yf0"], writes=["ymix_f%d" % p], semres="yf0")


    def phaseB(self):
        nc, P = self.nc, self.P
        cb, cf, pv, pv2, nhalf = self.cb, self.cf, self.pv, self.pv2, self.nhalf
        gates, idxs = self.gates, self.idxs
        with ExitStack() as st:
            sb = lambda name, shape, dt: st.enter_context(nc.sbuf_tensor(name, shape, dt))
            ps = lambda name, shape, dt=F32: st.enter_context(nc.psum_tensor(name, shape, dt))
            knT, vmem = self.knT, self.vmem
            gbc = sb("gbc", [128, 1060], F32)
            wr32 = sb("wr32", [128, 8, 36], F32)
            cntrow = sb("cntrow", [1, 32], F32)
            P.dma("sync", lambda h: h.dma_start(out=gbc[:], in_=self.bvec[:, 0:1060].partition_broadcast(128)), writes=["gbc"])
            P.dma("sync", lambda h: h.dma_start(out=wr32[:], in_=self.wr.rearrange("(kc p) n -> p kc n", p=128)), writes=["wr32"])
            P.op("gpsimd", lambda h: h.memset(cntrow[:], 0.0), writes=["cntrow"])
            self.x1buf = nc.dram_tensor("x1buf", [S, D], F32).ap()
            sb = lambda name, shape, dt: st.enter_context(nc.sbuf_tensor(name, shape, dt))
            qnT = sb("qnT_all", [128, 4, S], BF16)
            statB = sb("statB", [128, NT, 8], F32)
            sX = ExitStack()
            xnT = sX.enter_context(nc.sbuf_tensor("xnT_all", [128, 8, S], BF16))
            ymv = self.ymix.rearrange("(cc p) t -> p cc t", p=128)
            with ExitStack() as s1:
                sb1 = lambda name, shape, dt: s1.enter_context(nc.sbuf_tensor(name, shape, dt))
                ps1 = lambda name, shape, dt=F32: s1.enter_context(nc.psum_tensor(name, shape, dt))
                woutb = sb1("woutb_s", [128, 8, D], BF16)
                yt = [sb1("yt%d" % i, [128, 8, 512], BF16) for i in range(2)]
                ysq = sb1("ysq", [128, 8, 512], BF16)
                xt = [sb1("xtB%d" % i, [128, D], F32) for i in range(3)]
                x1t = [sb1("x1t%d" % i, [128, D], F32) for i in range(4)]
                xnb = [sb1("xnb%d" % i, [128, D], BF16) for i in range(2)]
                junk_b1 = sb1("junkB1", [128, D], BF16)
                bkS = ps1("bkS", [128, 512])
                pW_b1 = [[ps1("pW%d%d" % (a_, b_), [128, 512]) for b_ in range(2)] for a_ in range(2)]
                pT_b1 = [ps1("pTB%d" % i, [128, D], BF16) for i in range(2)]
                gmxb = sb1("gmxb", [128, D], F32)
                P.dma("sync", lambda h: h.dma_start(out=gmxb[:], in_=self.bvec[:, BV_GMEMX:BV_GMEMX + D].partition_broadcast(128)), writes=["gmxb"])
                P.dma("gpsimd", lambda h: h.dma_start(out=woutb[:], in_=self.w_out.rearrange("(kc p) n -> p kc n", p=128)), writes=["woutb"])
                units = []
                for i in range(NT):
                    def mk(i=i):
                        tb, tt = i // 4, i % 4
                        yb = tb % 2
                        x3 = i % 3
                        b2 = i % 2
                        ts = slice(tt * 128, (tt + 1) * 128)
                        c0 = (i % 8) * 2
                        ytn, xn_, x1n = "yt%d" % yb, "xtB%d" % x3, "x1t%d" % (i % 4)

                        def s0():
                            if tt == 0:
                                P.dma("sync", lambda h: h.dma_start(out=yt[yb][:], in_=ymv[:, :, tb * 512:(tb + 1) * 512]), writes=[ytn])
                                P.op("vector", lambda h: h.tensor_tensor(ysq[:], yt[yb][:], yt[yb][:], op=ALU.mult), reads=[ytn], writes=["ysq"])
                            P.dma("sync", lambda h: h.dma_start(out=xt[x3][:], in_=self.x[i * 128:(i + 1) * 128, :]), writes=[xn_])

                        def s1_():
                            for grp in range(2):
                                for c4 in range(4):
                                    cc = grp * 4 + c4
                                    P.op("tensor", (lambda cc=cc, grp=grp, c4=c4: (lambda h: h.matmul(bkS[:, c0 + grp:c0 + grp + 1], ysq[:, cc, ts], self.gsq[:, cc:cc + 1], start=(c4 == 0), stop=(c4 == 3))))(),
                                         reads=["ysq", "gsq"], writes=["bkS"], inc=(c4 == 3))
                            for half in range(2):
                                hs_ = slice(half * 512, (half + 1) * 512)
                                for grp in range(2):
                                    for c4 in range(4):
                                        cc = grp * 4 + c4
                                        P.op("tensor", (lambda cc=cc, grp=grp, c4=c4, half=half, hs_=hs_: (lambda h: h.matmul(pW_b1[half][grp][:], yt[yb][:, cc, ts], woutb[:, cc, hs_], start=(c4 == 0), stop=(c4 == 3))))(),
                                             reads=[ytn, "woutb"], writes=["pW%d%d" % (half, grp)], inc=(c4 == 3))

                        def s2():
                            P.op("vector", lambda h: h.tensor_scalar(statB[:, i, 0:2], bkS[:, c0:c0 + 2], 1.0 / 512, EPS, op0=ALU.mult, op1=ALU.add), reads=["bkS"], writes=["sB01_%d" % i])
                            yield
                            P.op("gpsimd", lambda h: h.tensor_tensor(statB[:, i, 2:4], statB[:, i, 0:2], nhalf[:, 0:2], op=ALU.pow), reads=["sB01_%d" % i, "nhalf"], writes=["sB23_%d" % i])
                            yield
                            for half in range(2):
                                hs_ = slice(half * 512, (half + 1) * 512)
                                P.op("vector", (lambda half=half, hs_=hs_: (lambda h: h.scalar_tensor_tensor(out=x1t[i % 4][:, hs_], in0=pW_b1[half][0][:], scalar=statB[:, i, 2:3], in1=xt[x3][:, hs_], op0=ALU.mult, op1=ALU.add)))(),
                                     reads=["pW%d0" % half, "sB23_%d" % i, xn_], writes=[x1n])
                                P.op("vector", (lambda half=half, hs_=hs_: (lambda h: h.scalar_tensor_tensor(out=x1t[i % 4][:, hs_], in0=pW_b1[half][1][:], scalar=statB[:, i, 3:4], in1=x1t[i % 4][:, hs_], op0=ALU.mult, op1=ALU.add)))(),
                                     reads=["pW%d1" % half, "sB23_%d" % i, x1n], writes=[x1n])
                            P.dma("gpsimd", lambda h: h.dma_start(out=self.x1buf[i * 128:(i + 1) * 128, :], in_=x1t[i % 4][:]), reads=[x1n], writes=["x1buf%d" % i], semres=x1n)
                            P.op("scalar", lambda h: h.activation(out=junk_b1[:], in_=x1t[i % 4][:], func=AF.Square, accum_out=statB[:, i, 4:5]), reads=[x1n], writes=["junkB1", "sB4_%d" % i])
                            yield

                        def s2b():
                            P.op("vector", lambda h: h.tensor_scalar(statB[:, i, 5:6], statB[:, i, 4:5], 1.0 / D, EPS, op0=ALU.mult, op1=ALU.add), reads=["sB4_%d" % i], writes=["sB5_%d" % i])
                            yield
                            P.op("gpsimd", lambda h: h.tensor_tensor(statB[:, i, 6:7], statB[:, i, 5:6], nhalf[:, 0:1], op=ALU.pow), reads=["sB5_%d" % i, "nhalf"], writes=["sB6_%d" % i])
                            yield
                            P.op("vector", lambda h: h.scalar_tensor_tensor(out=xnb[b2][:], in0=x1t[i % 4][:], scalar=statB[:, i, 6:7], in1=gmxb[:], op0=ALU.mult, op1=ALU.mult), reads=[x1n, "sB6_%d" % i, "gmxb"], writes=["xnb%d" % b2])
                            yield

                        def s3():
                            for kc in range(8):
                                P.op("tensor", (lambda kc=kc: (lambda h: h.transpose(pT_b1[b2][:, kc * 128:(kc + 1) * 128], xnb[b2][:, kc * 128:(kc + 1) * 128], cb[:, CB_ID:CB_ID + 128])))(),
                                     reads=["xnb%d" % b2, "cb"], writes=["pTB%d" % b2], inc=(kc == 7))
                            if i % 2 == 0:
                                P.op("scalar", lambda h: h.activation(out=xnT[:, :, i * 128:(i + 1) * 128], in_=pT_b1[b2][:].rearrange("p (k t) -> p k t", k=8), func=AF.Copy), reads=["pTB%d" % b2], writes=["xnT%d" % i])
                            else:
                                P.op("vector", lambda h: h.tensor_copy(xnT[:, :, i * 128:(i + 1) * 128], pT_b1[b2][:].rearrange("p (k t) -> p k t", k=8)), reads=["pTB%d" % b2], writes=["xnT%d" % i])
                        return [s0, s1_, s2, s2b, s3]
                    units.append(mk())
                run_pipeline(units)
                if self.debug == "B":
                    self._o1 = self.dbg_out("x1", [S, D])
                    self._o2 = self.dbg_out("x2", [S, D])
                P.barrier()
            with ExitStack() as s2a:
                sb2 = lambda name, shape, dt: s2a.enter_context(nc.sbuf_tensor(name, shape, dt))
                ps2 = lambda name, shape, dt=F32: s2a.enter_context(nc.psum_tensor(name, shape, dt))
                wqb = sb2("wqb_s", [128, 8, 512], BF16)
                sq_2a = [sb2("sqB%d" % i, [128, 512], BF16) for i in range(2)]
                vr_2a = [sb2("vrB%d" % i, [128, 512], F32) for i in range(2)]
                rs_2a = [sb2("rsB%d" % i, [128, 512], F32) for i in range(2)]
                PQ_2a = [ps2("psQB%d" % i, [128, 512]) for i in range(4)]
                PN_2a = [ps2("psNB%d" % i, [128, 512]) for i in range(2)]
                P.dma("gpsimd", lambda h: h.dma_start(out=wqb[:], in_=self.mem_wq.rearrange("(kc p) n -> p kc n", p=128)), writes=["wqb"])

                units = []
                u = 0
                for tb in range(NB):
                    for hd in range(4):
                        def mk(tb=tb, hd=hd, u=u):
                            t0 = tb * 512
                            nb = u % 2
                            pq, pn_ = PQ_2a[u % 4], PN_2a[u % 2]
                            pqn, pnn = "psQB%d" % (u % 4), "psNB%d" % (u % 2)
                            xres = ["xnT%d" % (tb * 4 + t) for t in range(4)]

                            def s0():
                                for kc in range(8):
                                    P.op("tensor", (lambda kc=kc: (lambda h: h.matmul(pq[:], wqb[:, kc, hd * 128:(hd + 1) * 128], xnT[:, kc, t0:t0 + 512], start=(kc == 0), stop=(kc == 7))))(),
                                         reads=["wqb"] + xres, writes=[pqn], inc=(kc == 7))

                            def s1_():
                                P.op("scalar", lambda h: h.activation(out=sq_2a[nb][:], in_=pq[:], func=AF.Square), reads=[pqn], writes=["sqB%d" % nb])

                            def s1b():
                                P.op("tensor", lambda h: h.matmul(pn_[:], cb[:, CB_ONE:CB_ONE + 128], sq_2a[nb][:], start=True, stop=True), reads=["cb", "sqB%d" % nb], writes=[pnn])

                            def s2():
                                P.op("scalar", lambda h: h.activation(out=vr_2a[nb][:], in_=pn_[:], func=AF.Ln, scale=1.0 / 128, bias=self.epsc[:, 0:1]), reads=[pnn, "epsc"], writes=["vrB%d" % nb])
                                P.op("scalar", lambda h: h.activation(out=rs_2a[nb][:], in_=vr_2a[nb][:], func=AF.Exp, scale=-0.5), reads=["vrB%d" % nb], writes=["rsB%d" % nb])
                                P.op("vector", lambda h: h.scalar_tensor_tensor(out=qnT[:, hd, t0:t0 + 512], in0=pq[:], scalar=pv2[:, 13:14], in1=rs_2a[nb][:], op0=ALU.mult, op1=ALU.mult),
                                     reads=[pqn, "rsB%d" % nb, "pv2e"], writes=["qnT%d_%d" % (tb, hd)])
                            return [s0, s1_, s1b, s2]
                        units.append(mk())
                        u += 1
                run_pipeline(units)
                P.barrier()
            sX.close()
            onT = sb("onT_all", [128, 4, S], BF16)
            with ExitStack() as s2b:
                sb2 = lambda name, shape, dt: s2b.enter_context(nc.sbuf_tensor(name, shape, dt))
                ps2 = lambda name, shape, dt=F32: s2b.enter_context(nc.psum_tensor(name, shape, dt))
                pt = [sb2("ptB%d" % i, [128, 2, 512], BF16) for i in range(3)]
                rl = [sb2("rlB%d" % i, [128, 512], F32) for i in range(2)]
                psS = [ps2("psSB%d" % i, [128, 2, 512]) for i in range(2)]
                psO = [ps2("psOB%d" % i, [128, 512]) for i in range(2)]
                psL = [ps2("psLB%d" % i, [128, 512]) for i in range(2)]
                units = []
                u = 0
                for tb in range(NB):
                    for hd in range(4):
                        def mk(tb=tb, hd=hd, u=u):
                            t0 = tb * 512
                            b2, b3 = u % 2, u % 3

                            def s0():
                                for kt in range(2):
                                    P.op("tensor", (lambda kt=kt: (lambda h: h.matmul(psS[b2][:, kt, :], knT[:, hd, kt * 128:(kt + 1) * 128], qnT[:, hd, t0:t0 + 512], start=True, stop=True)))(),
                                         reads=["knT", "qnT%d_%d" % (tb, hd)], writes=["psSB%d" % b2], inc=(kt == 1))

                            def s1_():
                                P.op("scalar", lambda h: h.activation(out=pt[b3][:], in_=psS[b2][:], func=AF.Exp), reads=["psSB%d" % b2], writes=["ptB%d" % b3])

                            def s2():
                                for kt in range(2):
                                    P.op("tensor", (lambda kt=kt: (lambda h: h.matmul(psO[b2][:], vmem[:, kt, hd * 128:(hd + 1) * 128], pt[b3][:, kt, :], start=(kt == 0), stop=(kt == 1))))(),
                                         reads=["vmem", "ptB%d" % b3], writes=["psOB%d" % b2], inc=(kt == 1))
                                for kt in range(2):
                                    P.op("tensor", (lambda kt=kt: (lambda h: h.matmul(psL[b2][:], cb[:, CB_ONE:CB_ONE + 128], pt[b3][:, kt, :], start=(kt == 0), stop=(kt == 1))))(),
                                         reads=["cb", "ptB%d" % b3], writes=["psLB%d" % b2], inc=(kt == 1))

                            def s3():
                                P.op("scalar", lambda h: h.activation(out=rl[b2][:], in_=psL[b2][:], func=AF.Ln), reads=["psLB%d" % b2], writes=["rlB%d" % b2])
                                P.op("scalar", lambda h: h.activation(out=rl[b2][:], in_=rl[b2][:], func=AF.Exp, scale=-1.0), reads=["rlB%d" % b2], writes=["rlB%d" % b2])
                                P.op("vector", lambda h: h.tensor_tensor(onT[:, hd, t0:t0 + 512], psO[b2][:], rl[b2][:], op=ALU.mult), reads=["psOB%d" % b2, "rlB%d" % b2], writes=["onT%d_%d" % (tb, hd)])
                            return [s0, s1_, s2, s3]
                        units.append(mk())
                        u += 1
                run_pipeline(units)
                P.barrier()
            with ExitStack() as s3_:
                sb3 = lambda name, shape, dt: s3_.enter_context(nc.sbuf_tensor(name, shape, dt))
                ps3 = lambda name, shape, dt=F32: s3_.enter_context(nc.psum_tensor(name, shape, dt))
                wob = sb3("wob_s", [128, 4, D], BF16)
                x1r = [sb3("x1r%d" % i, [128, D], F32) for i in range(3)]
                x2t = [sb3("x2t%d" % i, [128, D], F32) for i in range(4)]
                xn2 = [sb3("xn2_%d" % i, [128, D], F32) for i in range(2)]
                xn2b = [sb3("xn2b%d" % i, [128, D], BF16) for i in range(4)]
                xn2T = [sb3("xn2T%d" % i, [128, 8, 128], F32) for i in range(2)]
                junk_b3 = sb3("junkB3", [128, D], BF16)
                SM = sb3("smB3", [128, 2, 32], F32)
                LG = sb3("lgB3", [128, 2, 36], F32)
                GOH = sb3("gohB3", [128, 2, 4], F32)
                ESEL = sb3("eselB3", [128, 2, 8], F32)
                MX8 = sb3("mx8B3", [128, 2, 8], F32)
                MK = sb3("mkB3", [128, 2, 8], F32)
                M2T = sb3("m2tB3", [128, 2, 8], F32)
                GEJ = sb3("gejB3", [128, 2, 4], F32)
                A1_ = sb3("A1B3", [128, 2, 32], F32)
                A2_ = sb3("A2B3", [128, 2, 32], F32)
                AA_ = sb3("AAB3", [128, 2, 32], F32)
                POS = sb3("posB3", [128, 2, 32], F32)
                J32 = sb3("j32B3", [128, 2, 32], F32)
                pW_b3 = [[ps3("pWo%d%d" % (a_, b_), [128, 512]) for b_ in range(2)] for a_ in range(2)]
                pX = ps3("pXB", [128, 2, 512])
                bk0 = ps3("bk0", [128, 512])
                bk1 = ps3("bk1", [128, 512])
                P.dma("gpsimd", lambda h: h.dma_start(out=wob[:], in_=self.mem_wo.rearrange("(kc p) n -> p kc n", p=128)), writes=["wob"])

                units = []
                for i in range(NT):
                    def mk(i=i):
                        tb, tt = i // 4, i % 4
                        x3, b2 = i % 3, i % 2
                        ts = slice(tb * 512 + tt * 128, tb * 512 + (tt + 1) * 128)
                        x1n, x2n = "x1r%d" % x3, "x2t%d" % (i % 4)
                        sm, lg, goh, esel, mx8, mk_, m2t, gej = SM[:, b2, :], LG[:, b2, :], GOH[:, b2, :], ESEL[:, b2, :], MX8[:, b2, :], MK[:, b2, :], M2T[:, b2, :], GEJ[:, b2, :]
                        A1, A2, AA, pos, j32 = A1_[:, b2, :], A2_[:, b2, :], AA_[:, b2, :], POS[:, b2, :], J32[:, b2, :]
                        N = lambda n: "%s_%d" % (n, b2)
                        V = lambda fn, reads, writes: P.op("vector", fn, reads=reads, writes=writes)

                        def s0():
                            P.dma("sync", lambda h: h.dma_start(out=x1r[x3][:], in_=self.x1buf[i * 128:(i + 1) * 128, :]), writes=[x1n])

                        def s1_():
                            for half in range(2):
                                hs_ = slice(half * 512, (half + 1) * 512)
                                for hd in range(4):
                                    P.op("tensor", (lambda hd=hd, half=half, hs_=hs_: (lambda h: h.matmul(pW_b3[b2][half][:], onT[:, hd, ts], wob[:, hd, hs_], start=(hd == 0), stop=(hd == 3))))(),
                                         reads=["onT%d_%d" % (tb, hd_) for hd_ in range(4)] + ["wob"], writes=["pWo%d%d" % (b2, half)], inc=(hd == 3))

                        def s2():
                            for half in range(2):
                                hs_ = slice(half * 512, (half + 1) * 512)
                                V((lambda half=half, hs_=hs_: (lambda h: h.tensor_tensor(x2t[i % 4][:, hs_], pW_b3[b2][half][:], x1r[x3][:, hs_], op=ALU.add)))(), ["pWo%d%d" % (b2, half), x1n], [x2n])
                            P.dma("scalar", lambda h: h.dma_start(out=self.x2buf[i * 128:(i + 1) * 128, :], in_=x2t[i % 4][:]), reads=[x2n], writes=["x2buf%d" % i], semres=x2n)
                            if self.debug == "B":
                                P.dma("sync", lambda h: h.dma_start(out=self._o2[i * 128:(i + 1) * 128, :], in_=x2t[i % 4][:]), reads=[x2n], writes=["dbg2"], semres=x2n)
                                P.dma("sync", lambda h: h.dma_start(out=self._o1[i * 128:(i + 1) * 128, :], in_=x1r[x3][:]), reads=[x1n], writes=["dbg1"], semres=x2n)
                            P.op("scalar", lambda h: h.activation(out=junk_b3[:], in_=x2t[i % 4][:], func=AF.Square, accum_out=sm[:, 8:9]), reads=[x2n], writes=["junkB3", N("sm8")])
                            V(lambda h: h.tensor_scalar(sm[:, 9:10], sm[:, 8:9], 1.0 / D, EPS, op0=ALU.mult, op1=ALU.add), [N("sm8")], [N("sm9")])
                            P.op("gpsimd", lambda h: h.tensor_tensor(sm[:, 10:11], sm[:, 9:10], nhalf[:, 0:1], op=ALU.pow), reads=[N("sm9"), "nhalf"], writes=[N("sm10")])
                            V(lambda h: h.scalar_tensor_tensor(out=xn2[b2][:], in0=x2t[i % 4][:], scalar=sm[:, 10:11], in1=gbc[:, 0:D], op0=ALU.mult, op1=ALU.mult), [x2n, N("sm10"), "gbc"], [N("xn2")])
                            P.op("scalar", lambda h: h.activation(out=xn2b[i % 4][:], in_=xn2[b2][:], func=AF.Copy), reads=[N("xn2")], writes=["xn2b%d" % (i % 4)])

                        def s3():
                            pXf = pX[:].rearrange("p a b -> p (a b)")
                            for kc in range(8):
                                P.op("tensor", (lambda kc=kc: (lambda h: h.transpose(pXf[:, kc * 128:(kc + 1) * 128], xn2[b2][:, kc * 128:(kc + 1) * 128], cf[:, CF_ID:CF_ID + 128])))(),
                                     reads=[N("xn2"), "cf"], writes=["pXB"], inc=(kc == 7))
                            P.op("scalar", lambda h: h.activation(out=xn2T[b2][:], in_=pXf.rearrange("p (k t) -> p k t", k=8), func=AF.Copy), reads=["pXB"], writes=[N("xn2T")])

                        def s4():
                            for kc in range(8):
                                P.op("tensor", (lambda kc=kc: (lambda h: h.matmul(bk0[:, 0:36], xn2T[b2][:, kc, :], wr32[:, kc, :], start=(kc == 0), stop=(kc == 7))))(),
                                     reads=[N("xn2T"), "wr32"], writes=["bk0"], inc=(kc == 7))
                            V(lambda h: h.tensor_tensor(lg, bk0[:, 0:36], gbc[:, D:D + 36], op=ALU.add), ["bk0", "gbc"], [N("lg")])
                            yield
                            V(lambda h: h.reduce_max(out=sm[:, 16:17], in_=lg[:, 0:4], axis=AX.X), [N("lg")], [N("sm16")])
                            yield
                            V(lambda h: h.tensor_scalar(goh, lg[:, 0:4], sm[:, 16:17], None, op0=ALU.is_equal), [N("lg"), N("sm16")], [N("goh")])
                            yield
                            V(lambda h: h.tensor_scalar(sm[:, 17:18], sm[:, 16:17], -1.0, None, op0=ALU.mult), [N("sm16")], [N("sm17")])
                            yield
                            P.op("scalar", lambda h: h.activation(out=gej, in_=lg[:, 0:4], func=AF.Exp, bias=sm[:, 17:18], accum_out=sm[:, 18:19]), reads=[N("lg"), N("sm17")], writes=[N("gej"), N("sm18")])
                            yield
                            V(lambda h: h.reciprocal(sm[:, 19:20], sm[:, 18:19]), [N("sm18")], [N("sm19")])
                            yield
                            V(lambda h: h.tensor_tensor(A1.rearrange("p (g e) -> p g e", g=4), lg[:, 4:36].rearrange("p (g e) -> p g e", g=4), goh.unsqueeze(2).to_broadcast([128, 4, 8]), op=ALU.mult), [N("lg"), N("goh")], [N("A1")])
                            yield
                            V(lambda h: h.tensor_reduce(out=esel, in_=A1.rearrange("p (g e) -> p e g", g=4), axis=AX.X, op=ALU.add), [N("A1")], [N("esel")])
                            yield
                            V(lambda h: h.max(out=mx8, in_=esel), [N("esel")], [N("mx8")])
                            yield
                            V(lambda h: h.tensor_scalar(mk_, esel, mx8[:, 0:1], None, op0=ALU.is_equal), [N("esel"), N("mx8")], [N("mk0")])
                            yield
                            V(lambda h: h.tensor_scalar(m2t, esel, mx8[:, 1:2], None, op0=ALU.is_equal), [N("esel"), N("mx8")], [N("mk1")])
                            yield
                            V(lambda h: h.tensor_tensor(sm[:, 20:21], mx8[:, 1:2], mx8[:, 0:1], op=ALU.subtract), [N("mx8")], [N("sm20")])
                            yield
                            P.op("scalar", lambda h: h.activation(out=sm[:, 21:22], in_=sm[:, 20:21], func=AF.Exp), reads=[N("sm20")], writes=[N("sm21")])
                            yield
                            V(lambda h: h.tensor_scalar(sm[:, 22:23], sm[:, 21:22], 1.0, None, op0=ALU.add), [N("sm21")], [N("sm22")])
                            yield
                            V(lambda h: h.reciprocal(sm[:, 23:24], sm[:, 22:23]), [N("sm22")], [N("sm23")])
                            yield
                            V(lambda h: h.tensor_tensor(gates[:, i, 0:1], sm[:, 19:20], sm[:, 23:24], op=ALU.mult), [N("sm19"), N("sm23")], ["gate%d" % i])
                            yield
                            V(lambda h: h.tensor_tensor(gates[:, i, 1:2], gates[:, i, 0:1], sm[:, 21:22], op=ALU.mult), ["gate%d" % i, N("sm21")], ["gate%d" % i])
                            yield
                            V(lambda h: h.tensor_tensor(A1.rearrange("p (g e) -> p g e", g=4), goh.unsqueeze(2).to_broadcast([128, 4, 8]), mk_.unsqueeze(1).to_broadcast([128, 4, 8]), op=ALU.mult), [N("mk0"), N("goh"), N("esel")], [N("A1")])
                            yield
                            V(lambda h: h.tensor_tensor(A2.rearrange("p (g e) -> p g e", g=4), goh.unsqueeze(2).to_broadcast([128, 4, 8]), m2t.unsqueeze(1).to_broadcast([128, 4, 8]), op=ALU.mult), [N("mk1"), N("goh")], [N("A2")])
                            yield
                            V(lambda h: h.tensor_tensor(AA, A1, A2, op=ALU.add), [N("A1"), N("A2")], [N("AA")])
                            yield

                        def s5():
                            P.op("tensor", lambda h: h.matmul(bk1[:, 64:96], cf[:, CF_US:CF_US + 128], AA, start=True, stop=False), reads=["cf", N("AA")], writes=["bk1"], inc=False)
                            yield
                            P.op("tensor", lambda h: h.matmul(bk1[:, 64:96], cf[0:1, CF_ONE:CF_ONE + 128], cntrow[0:1, :], start=False, stop=True), reads=["cf", "cntrow"], writes=["bk1"])
                            yield
                            V(lambda h: h.tensor_tensor(pos, bk1[:, 64:96], cf[:, CF_EB:CF_EB + 32], op=ALU.add), ["bk1", "cf"], [N("pos")])
                            yield
                            P.op("tensor", lambda h: h.matmul(bk1[0:1, 128:160], cf[:, CF_ONE:CF_ONE + 1], AA, start=True, stop=True), reads=["cf", N("AA")], writes=["bk1"])
                            yield
                            V(lambda h: h.tensor_tensor(cntrow[:], cntrow[:], bk1[0:1, 128:160], op=ALU.add), ["bk1", "cntrow"], ["cntrow"])
                            yield
                            V(lambda h: h.scalar_tensor_tensor(out=j32, in0=pos, scalar=1.0, in1=A1, op0=ALU.mult, op1=ALU.mult, accum_out=sm[:, 24:25]), [N("pos"), N("A1")], [N("j32"), N("sm24")])
                            yield
                            V(lambda h: h.scalar_tensor_tensor(out=j32, in0=pos, scalar=1.0, in1=A2, op0=ALU.mult, op1=ALU.mult, accum_out=sm[:, 25:26]), [N("pos"), N("A2"), N("j32")], [N("j32"), N("sm25")])
                            yield
                            V(lambda h: h.tensor_copy(idxs[:, i, 0:2], sm[:, 24:26]), [N("sm24"), N("sm25")], ["idx%d" % i])
                            yield
                            for k2 in range(2):
                                P.dma("gpsimd", (lambda k2=k2: (lambda h: h.indirect_dma_start(out=self.xbuf, out_offset=bass.IndirectOffsetOnAxis(ap=idxs[:, i, k2:k2 + 1], axis=0), in_=xn2b[i % 4][:], in_offset=None)))(),
                                      reads=["xn2b%d" % (i % 4), "idx%d" % i, "bg:xz"], writes=["xbuf_s%d_%d" % (i, k2)], semres="xn2b%d" % (i % 4))
                        return [s0, s1_, s2, s3, s4, s5]
                    units.append(mk())
                run_pipeline(units)
                P.op("vector", lambda h: h.tensor_copy(self.cnti[:], cntrow[:]), reads=["cntrow"], writes=["cnti"])
                if self.debug == "B":
                    og = self.dbg_out("gates", [128, NT * 2])
                    oi = self.dbg_out("idxs", [128, NT * 2], I32)
                    P.dma("sync", lambda h: h.dma_start(out=og[:, :], in_=gates[:].rearrange("p a b -> p (a b)")), reads=["gate%d" % i for i in range(NT)], writes=["dbg3"])
                    P.dma("sync", lambda h: h.dma_start(out=oi[:, :], in_=idxs[:].rearrange("p a b -> p (a b)")), reads=["idx%d" % i for i in range(NT)], writes=["dbg4"])
                P.barrier()

    def phaseC(self):
        nc, P = self.nc, self.P
        cb = self.cb
        with ExitStack() as st:
            sb = lambda name, shape, dt: st.enter_context(nc.sbuf_tensor(name, shape, dt))
            ps = lambda name, shape, dt=F32: st.enter_context(nc.psum_tensor(name, shape, dt))
            NW, ND = 3, 4
            wgb = [sb("wgb%d" % i, [128, 8, 512], BF16) for i in range(NW)]
            wub = [sb("wub%d" % i, [128, 8, 512], BF16) for i in range(NW)]
            wdb = [sb("wdb%d" % i, [128, 4, D], BF16) for i in range(ND)]
            xrow = [sb("xrow%d" % i, [128, NSB, D], BF16) for i in range(2)]
            XT = [sb("XT%d" % i, [128, 8, CAP], BF16) for i in range(2)]
            th = [sb("thC%d" % i, [128, CAP], F32) for i in range(2)]
            t1 = [sb("t1C%d" % i, [128, CAP], F32) for i in range(2)]
            HT = [sb("HT%d" % i, [128, 4, CAP], BF16) for i in range(2)]
            yo = [sb("yo%d" % i, [128, D], BF16) for i in range(8)]
            pT = [ps("pTC%d" % i, [128, D], BF16) for i in range(2)]
            psG = [ps("psGC%d" % i, [128, 512]) for i in range(2)]
            psU = [ps("psUC%d" % i, [128, 512]) for i in range(2)]
            psY = [ps("psYC%d" % i, [128, 512]) for i in range(2)]
            preg, cnti = self.preg, self.cnti
            semT = P.sems["E_tensor"]
            Ncell = [CAP]

            def skip_wrap(e, thr):
                def wrap(h, clos, nincs):
                    h.reg_load(preg, cnti[0:1, e:e + 1])
                    with h.If_lt(preg, thr + 1):
                        for c_ in clos:
                            for k_, v_ in getattr(c_, "waits", ()):
                                h.wait_ge(P.sems[k_], v_)
                        h.matmul(psY[0][:, 0:1], cb[:, CB_ID:CB_ID + 128], cb[:, CB_ONE:CB_ONE + 1], start=True, stop=True).then_inc(semT, nincs)
                    with h.Else():
                        for c_ in clos:
                            c_(h)
                return wrap

            def nvar_wrap(e):
                def wrap(h, clos, nincs):
                    h.reg_load(preg, cnti[0:1, e:e + 1])
                    with h.If_lt(preg, 257):
                        Ncell[0] = 256
                        for c_ in clos:
                            c_(h)
                    with h.Else():
                        with h.If_lt(preg, 385):
                            Ncell[0] = 384
                            for c_ in clos:
                                c_(h)
                        with h.Else():
                            Ncell[0] = CAP
                            for c_ in clos:
                                c_(h)
                    Ncell[0] = CAP
                return wrap
            units = []
            for e in range(NE):
                def mk(e=e):
                    b = e % 2
                    w3 = e % NW
                    w4 = e % ND

                    def s0():
                        P.dma("sync", lambda h: h.dma_start(out=xrow[b][:], in_=self.xbuf[e * CAP:(e + 1) * CAP, :].rearrange("(s p) n -> p s n", p=128)), writes=["xrow%d" % b])
                        P.dma("gpsimd", lambda h: h.dma_start(out=wgb[w3][:], in_=self.wg16[e].rearrange("(kc p) n -> p kc n", p=128)), reads=["bg:w16_%d" % e], writes=["wgb%d" % w3])
                        P.dma("gpsimd", lambda h: h.dma_start(out=wub[w3][:], in_=self.wu16[e].rearrange("(kc p) n -> p kc n", p=128)), reads=["bg:w16_%d" % e], writes=["wub%d" % w3])
                        P.dma("gpsimd", lambda h: h.dma_start(out=wdb[w4][:], in_=self.wd16[e].rearrange("(kc p) n -> p kc n", p=128)), reads=["bg:w16_%d" % e], writes=["wdb%d" % w4])

                    def s1():
                        for sbk in range(NSB):
                            xb = sbk % 2
                            for kc in range(8):
                                P.op("tensor", (lambda kc=kc, sbk=sbk, xb=xb: (lambda h: h.transpose(pT[xb][:, kc * 128:(kc + 1) * 128], xrow[b][:, sbk, kc * 128:(kc + 1) * 128], cb[:, CB_ID:CB_ID + 128])))(),
                                     reads=["xrow%d" % b, "cb"], writes=["pTC%d" % xb], inc=(kc == 7))
                            if sbk % 2 == 0:
                                P.op("scalar", (lambda sbk=sbk, xb=xb: (lambda h: h.activation(out=XT[b][:, :, sbk * 128:(sbk + 1) * 128], in_=pT[xb][:].rearrange("p (k t) -> p k t", k=8), func=AF.Copy)))(),
                                     reads=["pTC%d" % xb], writes=["XT%d_%d" % (b, sbk)])
                            else:
                                P.op("vector", (lambda sbk=sbk, xb=xb: (lambda h: h.tensor_copy(XT[b][:, :, sbk * 128:(sbk + 1) * 128], pT[xb][:].rearrange("p (k t) -> p k t", k=8))))(),
                                     reads=["pTC%d" % xb], writes=["XT%d_%d" % (b, sbk)])

                    def s2():
                        xres = ["XT%d_%d" % (b, k) for k in range(NSB)]
                        P.begin_region("tensor")
                        for mt in range(4):
                            mb = mt % 2
                            for kc in range(8):
                                P.op("tensor", (lambda kc=kc, mt=mt, mb=mb: (lambda h: h.matmul(psG[mb][:, 0:Ncell[0]], wgb[w3][:, kc, mt * 128:(mt + 1) * 128], XT[b][:, kc, 0:Ncell[0]], start=(kc == 0), stop=(kc == 7))))(),
                                     reads=["wgb%d" % w3, "cnti"] + xres, writes=["psGC%d" % mb], inc=(kc == 7))
                            for kc in range(8):
                                P.op("tensor", (lambda kc=kc, mt=mt: (lambda h: h.matmul(psU[mt % 2][:, 0:Ncell[0]], wub[w3][:, kc, mt * 128:(mt + 1) * 128], XT[b][:, kc, 0:Ncell[0]], start=(kc == 0), stop=(kc == 7))))(),
                                     reads=["wub%d" % w3, "cnti"] + xres, writes=["psUC%d" % (mt % 2)], inc=(kc == 7))
                            P.op("scalar", (lambda mb=mb: (lambda h: h.activation(out=th[mb][:], in_=psG[mb][:, 0:CAP], func=AF.Tanh, scale=0.5)))(), reads=["psGC%d" % mb], writes=["thC%d" % mb])
                            P.op("vector", (lambda mb=mb: (lambda h: h.scalar_tensor_tensor(out=t1[mb][:], in0=th[mb][:], scalar=1.0, in1=psG[mb][:, 0:CAP], op0=ALU.add, op1=ALU.mult)))(), reads=["thC%d" % mb, "psGC%d" % mb], writes=["t1C%d" % mb])
                            P.op("vector", (lambda mb=mb, mt=mt: (lambda h: h.scalar_tensor_tensor(out=HT[b][:, mt, :], in0=t1[mb][:], scalar=0.5, in1=psU[mb][:, 0:CAP], op0=ALU.mult, op1=ALU.mult)))(), reads=["t1C%d" % mb, "psUC%d" % mb], writes=["HT%d_%d" % (b, mt)])
                        P.end_region(nvar_wrap(e))

                    def s3():
                        hres = ["HT%d_%d" % (b, k) for k in range(4)]
                        for sbk in range(NSB):
                            ob = (e * NSB + sbk) % 8
                            r0 = e * CAP + sbk * 128
                            if sbk >= 2:
                                P.begin_region("tensor")
                            for half in range(2):
                                for mt in range(4):
                                    P.op("tensor", (lambda mt=mt, sbk=sbk, half=half: (lambda h: h.matmul(psY[half][:], HT[b][:, mt, sbk * 128:(sbk + 1) * 128], wdb[w4][:, mt, half * 512:(half + 1) * 512], start=(mt == 0), stop=(mt == 3))))(),
                                         reads=hres + ["wdb%d" % w4, "cnti"], writes=["psYC%d" % half], inc=(mt == 3))
                            if sbk >= 2:
                                P.end_region(skip_wrap(e, 128 * sbk))
                            for half in range(2):
                                if half == 0:
                                    P.op("scalar", (lambda ob=ob: (lambda h: h.activation(out=yo[ob][:, 0:512], in_=psY[0][:], func=AF.Copy)))(), reads=["psYC0"], writes=["yo%d" % ob])
                                else:
                                    P.op("vector", (lambda ob=ob: (lambda h: h.tensor_copy(yo[ob][:, 512:1024], psY[1][:])))(), reads=["psYC1"], writes=["yo%d" % ob])
                            P.dma("scalar", (lambda ob=ob, r0=r0: (lambda h: h.dma_start(out=self.ybuf[r0:r0 + 128, :], in_=yo[ob][:])))(), reads=["yo%d" % ob], writes=["ybuf%d" % (r0 // 128)], semres="yo%d" % ob)
                    return [s0, s1, s2, s3]
                units.append(mk())
            run_pipeline(units)
            self.phaseD(st)

    def phaseD(self, st):
        nc, P = self.nc, self.P
        gates, idxs = self.gates, self.idxs
        if True:
            sb = lambda name, shape, dt: st.enter_context(nc.sbuf_tensor(name, shape, dt))
            yall = ["ybuf%d" % k for k in range(NE * NSB)]
            y1 = [sb("y1D%d" % i, [128, D], BF16) for i in range(3)]
            y2 = [sb("y2D%d" % i, [128, D], BF16) for i in range(3)]
            x2 = [sb("x2D%d" % i, [128, D], F32) for i in range(3)]
            oo = [sb("ooD%d" % i, [128, D], F32) for i in range(3)]
            for i in range(NT):
                b = i % 3
                P.dma("gpsimd", (lambda i=i, b=b: (lambda h: h.indirect_dma_start(out=y1[b][:], out_offset=None, in_=self.ybuf, in_offset=bass.IndirectOffsetOnAxis(ap=idxs[:, i, 0:1], axis=0))))(), reads=yall, writes=["y1D%d" % b])
                P.dma("gpsimd", (lambda i=i, b=b: (lambda h: h.indirect_dma_start(out=y2[b][:], out_offset=None, in_=self.ybuf, in_offset=bass.IndirectOffsetOnAxis(ap=idxs[:, i, 1:2], axis=0))))(), reads=yall, writes=["y2D%d" % b])
                P.dma("sync", (lambda i=i, b=b: (lambda h: h.dma_start(out=x2[b][:], in_=self.x2buf[i * 128:(i + 1) * 128, :])))(), writes=["x2D%d" % b])
                P.op("vector", (lambda i=i, b=b: (lambda h: h.scalar_tensor_tensor(out=oo[b][:], in0=y1[b][:], scalar=gates[:, i, 0:1], in1=x2[b][:], op0=ALU.mult, op1=ALU.add)))(), reads=["y1D%d" % b, "x2D%d" % b], writes=["ooD%d" % b])
                P.op("vector", (lambda i=i, b=b: (lambda h: h.scalar_tensor_tensor(out=oo[b][:], in0=y2[b][:], scalar=gates[:, i, 1:2], in1=oo[b][:], op0=ALU.mult, op1=ALU.add)))(), reads=["y2D%d" % b, "ooD%d" % b], writes=["ooD%d" % b])
                P.dma("scalar", (lambda i=i, b=b: (lambda h: h.dma_start(out=self.out[i * 128:(i + 1) * 128, :], in_=oo[b][:])))(), reads=["ooD%d" % b], writes=["out%d" % i], semres="ooD%d" % b)


def host_shared(inputs):
    f = lambda k: np.ascontiguousarray(np.asarray(inputs[k], dtype=np.float32)[0])
    pv = np.zeros((128, NPV), np.float32)
    t8 = lambda v: v.reshape(8, 128).T
    pv[:, PV["gmix"]:PV["gmix"] + 8] = t8(f("norm_mix_g"))
    pv[:, PV["gout"]:PV["gout"] + 8] = t8(np.concatenate([f("lru_out_g"), f("fox_out_g")]))
    pv[:, PV["gmemx"]:PV["gmemx"] + 8] = t8(f("norm_mem_x_g"))
    pv[:, PV["gmem"]:PV["gmem"] + 8] = t8(f("norm_mem_g"))
    pv[:, PV["convw"]:PV["convw"] + 16] = f("conv_w").reshape(4, 4, 128).transpose(2, 1, 0).reshape(128, 16)
    for n, k in [("convb", "conv_b"), ("ba", "lru_ba"), ("bx", "lru_bx"), ("apar", "lru_a_param")]:
        pv[:, PV[n]:PV[n] + 4] = f(k).reshape(4, 128).T
    pv[:, PV["gq2"]] = np.tile(f("fox_q_g"), 2)
    pv[:, PV["gk2"]] = np.tile(f("fox_k_g"), 2)
    pv[:, PV["mqg"]] = f("mem_q_g")
    pv[:, PV["mkg"]] = f("mem_k_g")
    pv[0:8, PV["bf"]] = f("b_forget")
    pv[0:64, PV["gfx"]:PV["gfx"] + 8] = f("fox_out_g").reshape(8, 64).T
    bvec = np.concatenate([f("norm_ffn_g"), f("router_group_b"), f("router_expert_b"), f("norm_mix_g"), f("norm_mem_x_g"), f("norm_mem_g")])[None, :]
    wr = np.concatenate([f("router_group_w"), f("router_expert_w")], axis=1)
    wa, wx = f("lru_wa"), f("lru_wx")
    lruw = np.zeros((128, 4, 2, 128), np.float32)
    for ct in range(4):
        for hb in range(2):
            lruw[hb * 64:(hb + 1) * 64, ct, 0, hb * 64:(hb + 1) * 64] = wa[2 * ct + hb]
            lruw[hb * 64:(hb + 1) * 64, ct, 1, hb * 64:(hb + 1) * 64] = wx[2 * ct + hb]
    cbf = np.zeros((128, NCB), np.float32)
    cbf[:, CB_ID:CB_ID + 128] = np.eye(128)
    cbf[0:64, CB_BO:CB_BO + 64] = 1.0
    cbf[64:128, CB_BO + 64:CB_BO + 128] = 1.0
    cbf[:, CB_ONE:CB_ONE + 128] = 1.0
    sp = np.arange(128)[:, None]
    tq = np.arange(512)[None, :]
    for m in range(4):
        cbf[:, CB_MASK + m * 512:CB_MASK + (m + 1) * 512] = np.where(m * 128 + sp > tq, -30000.0, 0.0)
    cf = np.zeros((128, NCF), np.float32)
    cf[:, CF_ID:CF_ID + 128] = np.eye(128)
    cf[:, CF_US:CF_US + 128] = (np.arange(128)[:, None] < np.arange(128)[None, :]).astype(np.float32)
    cf[:, CF_ONE:CF_ONE + 128] = 1.0
    cf[:, CF_EB:CF_EB + 32] = (np.arange(32) * CAP)[None, :]
    return {
        "w_in": f("w_in"), "w_out": f("w_out"), "mem_wq": f("mem_wq"), "mem_wkv": f("mem_wkv"), "mem_wo": f("mem_wo"),
        "wr": np.ascontiguousarray(wr), "wgate": f("exp_w_gate"), "wup": f("exp_w_up"), "wdown": f("exp_w_down"),
        "lruw": lruw.reshape(128, 1024), "pvec": pv, "bvec": np.ascontiguousarray(bvec),
        "cbf": cbf.astype(ml_dtypes.bfloat16), "cf32": cf,
    }


def kernel(**inputs):
    nc = Builder().build()
    shared = host_shared(inputs)
    x = np.asarray(inputs["x"], dtype=np.float32)
    mem = np.asarray(inputs["mem"], dtype=np.float32)
    in_maps = []
    for c in range(8):
        m = dict(shared)
        m["x"] = np.ascontiguousarray(x[c])
        m["mem"] = np.ascontiguousarray(mem[c])
        in_maps.append(m)
    res = run_bass_kernel_spmd(nc, in_maps, core_ids=list(range(8)))
    return np.stack([np.asarray(r["out"], dtype=np.float32) for r in res.results], axis=0)
```

```python
from contextlib import ExitStack
import numpy as np
import ml_dtypes
import concourse.bass as bass
import concourse.mybir as mybir
from concourse.bass_utils import run_bass_kernel_spmd

F32 = mybir.dt.float32
BF16 = mybir.dt.bfloat16
I32 = mybir.dt.int32
AF = mybir.ActivationFunctionType
ALU = mybir.AluOpType
AX = mybir.AxisListType

ENGS = ["tensor", "vector", "scalar", "gpsimd", "sync"]

S = 4096
D = 1024
NT = 32
NB = 8
NE = 32
CAP = 512
NSB = CAP // 128
EPS = 1e-6
IN_COLS = 2568


class Res:
    __slots__ = ("name", "w", "r", "dsem")

    def __init__(self, name):
        self.name = name
        self.w = None
        self.r = []
        self.dsem = {}


class Prog:
    def __init__(self, nc, stack):
        self.nc = nc
        self.stack = stack
        self.q = {e: [] for e in ENGS}
        self.sems = {}
        self.cnt = {}
        self.waited = {e: {} for e in ENGS}
        self.res = {}
        self.nsem = 0
        self.free_dsems = {"sw": [], "hw": []}
        self.bgsem = {}
        self.bgkeys = set()
        for e in ENGS:
            self._mksem("E_" + e)

    def _mksem(self, key):
        s = self.stack.enter_context(self.nc.semaphore("s%d" % self.nsem))
        self.nsem += 1
        self.sems[key] = s
        self.cnt[key] = 0
        return s

    def R(self, name):
        r = self.res.get(name)
        if r is None:
            r = Res(name)
            self.res[name] = r
        return r

    def _deps(self, eng, reads, writes):
        need = {}

        def add(ev):
            if ev is None:
                return
            k, v = ev
            if need.get(k, 0) < v:
                need[k] = v
        for r in reads:
            if r.startswith("bg:"):
                k_ = self.bgsem[r[3:]]
                add((k_, self.cnt[k_]))
            else:
                add(self.R(r).w)
        for w in writes:
            rw = self.R(w)
            add(rw.w)
            for ev in rw.r:
                add(ev)
        out = []
        own = "E_" + eng
        for k, v in need.items():
            if k == own and v > self.cnt[own]:
                continue
            if self.waited[eng].get(k, 0) >= v:
                continue
            self.waited[eng][k] = v
            out.append((k, v))
        return out

    def _commit(self, ev, reads, writes):
        for w in writes:
            rw = self.R(w)
            rw.w = ev
            rw.r = []
        for r in reads:
            if r in writes or r.startswith("bg:"):
                continue
            rr = self.R(r)
            rr.r = [e for e in rr.r if e[0] != ev[0]]
            rr.r.append(ev)

    def op(self, eng, fn, reads=(), writes=(), inc=True):
        reads = list(reads)
        writes = list(writes)
        waits = self._deps(eng, reads, writes)
        key = "E_" + eng
        if inc:
            self.cnt[key] += 1
            ev = (key, self.cnt[key])
        else:
            ev = (key, self.cnt[key] + 1)
        sems = self.sems
        sem = sems[key]

        def run(h, waits=waits, fn=fn, inc=inc, sem=sem):
            for k, v in waits:
                h.wait_ge(sems[k], v)
            ins = fn(h)
            if inc:
                ins.then_inc(sem, 1)
        run.waits = waits
        self.q[eng].append(run)
        self._commit(ev, reads, writes)
        return ev

    def dma(self, eng, fn, reads=(), writes=(), semres=None):
        reads = list(reads)
        writes = list(writes)
        waits = self._deps(eng, reads, writes)
        if semres is None:
            semres = writes[0] if writes else reads[0]
        rr = self.R(semres)
        cls = "sw" if eng == "gpsimd" else "hw"
        if cls not in rr.dsem:
            if self.free_dsems[cls]:
                rr.dsem[cls] = self.free_dsems[cls].pop()
            else:
                rr.dsem[cls] = "D%s%d" % (cls, self.nsem)
                self._mksem(rr.dsem[cls])
        key = rr.dsem[cls]
        self.cnt[key] += 16
        ev = (key, self.cnt[key])
        sems = self.sems
        sem = sems[key]

        def run(h, waits=waits, fn=fn, sem=sem):
            for k, v in waits:
                h.wait_ge(sems[k], v)
            fn(h).then_inc(sem, 16)
        self.q[eng].append(run)
        self._commit(ev, reads, writes)
        return ev

    def dma_bg(self, eng, fn, name, reads=()):
        key = self.bgsem.get(name)
        if key is None:
            key = "B_" + name
            self._mksem(key)
            self.bgsem[name] = key
            self.bgkeys.add(key)
        waits = self._deps(eng, list(reads), [])
        self.cnt[key] += 16
        sem = self.sems[key]
        sems = self.sems

        def run(h, fn=fn, sem=sem, waits=waits):
            for k, v in waits:
                h.wait_ge(sems[k], v)
            fn(h).then_inc(sem, 16)
        self.q[eng].append(run)

    def begin_region(self, eng):
        self._rg = (eng, len(self.q[eng]), dict(self.waited[eng]), self.cnt["E_" + eng])

    def end_region(self, wrap):
        eng, start, waited, c0 = self._rg
        clos = self.q[eng][start:]
        del self.q[eng][start:]
        nincs = self.cnt["E_" + eng] - c0
        self.q[eng].append(lambda h, clos=clos, nincs=nincs: wrap(h, clos, nincs))
        self.waited[eng] = waited

    def barrier(self):
        for e in ENGS:
            waits = []
            for k, v in self.cnt.items():
                if v == 0 or k in self.bgkeys or self.waited[e].get(k, 0) >= v:
                    continue
                self.waited[e][k] = v
                waits.append((k, v))
            sems = self.sems

            def run(h, waits=waits):
                for k, v in waits:
                    h.wait_ge(sems[k], v)
            self.q[e].append(run)
        for r in self.res.values():
            for cls, k in r.dsem.items():
                self.free_dsems[cls].append(k)
        for cls in self.free_dsems:
            self.free_dsems[cls] = sorted(set(self.free_dsems[cls]))
        self.res = {}

    def emit(self):
        nc = self.nc
        with nc.Block() as block:
            for e in ENGS:
                lst = self.q[e]
                if not lst:
                    continue

                def body(h, lst=lst):
                    for f in lst:
                        f(h)
                getattr(block, e)(body)


def run_pipeline(units):
    n = len(units)
    ns = max(len(u) for u in units)
    for t in range(n + ns - 1):
        gens = []

        def flush():
            while gens:
                for g_ in list(gens):
                    try:
                        next(g_)
                    except StopIteration:
                        gens.remove(g_)
        for s_ in reversed(range(ns)):
            u = t - s_
            if 0 <= u < n and s_ < len(units[u]):
                r = units[u][s_]()
                if r is not None:
                    gens.append(r)
                else:
                    pass
            if gens and (s_ == 0 or not _is_gen_next(units, t, s_ - 1, n)):
                flush()
        flush()


def _is_gen_next(units, t, s_, n):
    u = t - s_
    if not (0 <= u < n and s_ < len(units[u])):
        return True
    import inspect
    return inspect.isgeneratorfunction(units[u][s_])


PV = {}
_c = 0
for _n, _w in [("gmix", 8), ("gout", 8), ("gmemx", 8), ("gmem", 8), ("convw", 16), ("convb", 4), ("ba", 4),
               ("bx", 4), ("apar", 4), ("gq2", 1), ("gk2", 1), ("mqg", 1), ("mkg", 1), ("bf", 1), ("gfx", 8)]:
    PV[_n] = _c
    _c += _w
NPV = _c
CB_ID, CB_BO, CB_ONE, CB_MASK = 0, 128, 256, 384
NCB = 384 + 2048
CF_ID, CF_US, CF_ONE, CF_EB = 0, 128, 256, 384
NCF = 384 + 32
NBV = 1024 + 36 + 3 * 1024
BV_GMIX, BV_GMEMX, BV_GMEM = 1060, 2084, 3108


class Builder:
    def __init__(self, debug=None):
        self.debug = debug
        self.nc = bass.Bass("TRN2", target_bir_lowering=False)
        nc = self.nc
        di = lambda n, s, d=F32: nc.dram_tensor(n, s, d, kind="ExternalInput").ap()
        self.x = di("x", [S, D])
        self.mem = di("mem", [256, D])
        self.w_in = di("w_in", [D, IN_COLS])
        self.w_out = di("w_out", [D, D])
        self.mem_wq = di("mem_wq", [D, 512])
        self.mem_wkv = di("mem_wkv", [D, 1024])
        self.mem_wo = di("mem_wo", [512, D])
        self.wr = di("wr", [D, 36])
        self.wgate = di("wgate", [NE, D, 512])
        self.wup = di("wup", [NE, D, 512])
        self.wdown = di("wdown", [NE, 512, D])
        self.lruw = di("lruw", [128, 4 * 2 * 128])
        self.pvec = di("pvec", [128, NPV])
        self.bvec = di("bvec", [1, NBV])
        self.cbf = di("cbf", [128, NCB], BF16)
        self.cf32 = di("cf32", [128, NCF])
        self.out = nc.dram_tensor("out", [S, D], F32, kind="ExternalOutput").ap()
        dt = lambda n, s, d: nc.dram_tensor(n, s, d).ap()
        self.winb = dt("winb", [D, IN_COLS], BF16)
        self.woutb = dt("woutb", [D, D], BF16)
        self.wqb = dt("wqb", [D, 512], BF16)
        self.wkvb = dt("wkvb", [D, 1024], BF16)
        self.wob = dt("wob", [512, D], BF16)
        self.ymix = dt("ymix", [D, S], BF16)
        self.cscr = dt("cscr", [8, 12, S], BF16)
        self.x2buf = dt("x2buf", [S, D], F32)
        self.xbuf = dt("xbuf", [NE * CAP, D], BF16)
        self.ybuf = dt("ybuf", [NE * CAP, D], BF16)
        self.wg16 = dt("wg16", [NE, D, 512], BF16)
        self.wu16 = dt("wu16", [NE, D, 512], BF16)
        self.wd16 = dt("wd16", [NE, 512, D], BF16)
        if debug:
            self.dbg = {}

    def dbg_out(self, name, shape, dtype=F32):
        t = self.nc.dram_tensor("dbg_" + name, shape, dtype, kind="ExternalOutput").ap()
        self.dbg[name] = t
        return t

    def build(self, upto=99):
        nc = self.nc
        with ExitStack() as top:
            P = Prog(nc, top)
            self.P = P
            sbt = lambda st, name, shape, dt: st.enter_context(nc.sbuf_tensor(name, shape, dt))
            pst = lambda st, name, shape, dt=F32: st.enter_context(nc.psum_tensor(name, shape, dt))
            pv = sbt(top, "pv", [128, NPV], F32)
            pv2 = sbt(top, "pv2", [128, 24], F32)
            cb = sbt(top, "cb", [128, NCB], BF16)
            cf = sbt(top, "cf", [128, NCF], F32)
            nhalf = sbt(top, "nhalf", [128, 512], F32)
            phalf = sbt(top, "phalf", [128, 512], F32)
            self.pv, self.pv2, self.cb, self.cf, self.nhalf, self.phalf = pv, pv2, cb, cf, nhalf, phalf
            P.dma("sync", lambda h: h.dma_start(out=pv[:], in_=self.pvec[:, :]), writes=["pv"])
            P.dma("sync", lambda h: h.dma_start(out=cb[:], in_=self.cbf[:, :]), writes=["cb"])
            P.dma("sync", lambda h: h.dma_start(out=cf[:], in_=self.cf32[:, :]), writes=["cf"])
            self.epsc = sbt(top, "epsc", [128, 1], F32)
            P.op("gpsimd", lambda h: h.memset(self.epsc[:], EPS), writes=["epsc"])
            P.op("gpsimd", lambda h: h.memset(nhalf[:], -0.5), writes=["nhalf"])
            zt = sbt(top, "zt", [128, 2, D], BF16)
            P.op("gpsimd", lambda h: h.memset(zt[:], 0.0), writes=["zt"])
            self.zt = zt
            P.op("gpsimd", lambda h: h.memset(phalf[:], 0.5), writes=["phalf"])
            c = PV
            P.op("vector", lambda h: h.tensor_scalar(pv2[:, 0:4], pv[:, c["ba"]:c["ba"] + 4], 0.5, None, op0=ALU.mult), reads=["pv"], writes=["pv2a"])
            P.op("vector", lambda h: h.tensor_scalar(pv2[:, 4:8], pv[:, c["bx"]:c["bx"] + 4], 0.5, None, op0=ALU.mult), reads=["pv"], writes=["pv2b"])
            P.op("scalar", lambda h: h.activation(out=pv2[:, 8:12], in_=pv[:, c["apar"]:c["apar"] + 4], func=AF.Exp, scale=-1.0), reads=["pv"], writes=["pv2c"])
            P.op("scalar", lambda h: h.activation(out=pv2[:, 8:12], in_=pv2[:, 8:12], func=AF.Ln, bias=1.0), reads=["pv2c"], writes=["pv2c"])
            P.op("vector", lambda h: h.tensor_scalar(pv2[:, 8:12], pv2[:, 8:12], -4.0, None, op0=ALU.mult), reads=["pv2c"], writes=["pv2c"])
            P.op("vector", lambda h: h.tensor_scalar(pv2[:, 12:13], pv[:, c["gq2"]:c["gq2"] + 1], 0.125, None, op0=ALU.mult), reads=["pv"], writes=["pv2d"])
            P.op("vector", lambda h: h.tensor_scalar(pv2[:, 13:14], pv[:, c["mqg"]:c["mqg"] + 1], 128.0 ** -0.5, None, op0=ALU.mult), reads=["pv"], writes=["pv2e"])
            P.op("vector", lambda h: h.tensor_scalar(pv2[:, 14:15], pv[:, c["bf"]:c["bf"] + 1], -1.0, None, op0=ALU.mult), reads=["pv"], writes=["pv2f"])
            self.PVR = ["pv", "pv2a", "pv2b", "pv2c", "pv2d", "pv2e", "pv2f", "cb", "cf", "nhalf", "phalf"]

            self.knT = sbt(top, "knT", [128, 4, 256], BF16)
            self.vmem = sbt(top, "vmem", [128, 2, 512], BF16)
            self.cnti = sbt(top, "cnti", [1, 32], I32)
            self.preg = top.enter_context(nc.tensor.register("cntreg"))
            self.gates = sbt(top, "gates", [128, NT, 2], F32)
            self.idxs = sbt(top, "idxs", [128, NT, 2], I32)
            self.gsq = sbt(top, "gsq", [128, 8], BF16)
            gtmp = sbt(top, "gtmp", [128, 8], F32)
            P.op("vector", lambda h: h.tensor_scalar(pv2[:, 16:20], pv[:, c["gout"]:c["gout"] + 4], 0.5, None, op0=ALU.mult), reads=["pv"], writes=["pv2g"])
            P.op("vector", lambda h: h.tensor_tensor(gtmp[:], pv[:, c["gout"]:c["gout"] + 8], pv[:, c["gout"]:c["gout"] + 8], op=ALU.mult), reads=["pv"], writes=["gtmp"])
            P.op("vector", lambda h: h.reciprocal(gtmp[:], gtmp[:]), reads=["gtmp"], writes=["gtmp"])
            P.op("vector", lambda h: h.tensor_copy(self.gsq[:], gtmp[:]), reads=["gtmp"], writes=["gsq"])
            P.barrier()
            if upto >= 1:
                with ExitStack() as stA:
                    hT = sbt(stA, "hT", [128, 8, S], BF16)
                    self.hT = hT
                    self.phaseA1(stA)
                    P.barrier()
                    if upto >= 2:
                        self.phaseA2()
                        P.barrier()
                    if upto >= 3:
                        self.phaseA3()
                        P.barrier()
            if upto >= 4:
                self.phaseB()
                P.barrier()
            if upto >= 5:
                self.phaseC()
            P.barrier()
            if self.debug in ("A2", "A3"):
                o = self.dbg_out("ymix", [D, S], BF16)
                for r_ in range(8):
                    P.dma("sync", (lambda r_=r_: (lambda h: h.dma_start(out=o[r_ * 128:(r_ + 1) * 128, :], in_=self.ymix[r_ * 128:(r_ + 1) * 128, :])))(), writes=["dbg%d" % r_])
                P.barrier()
            P.emit()
        return nc

    def precast(self, e0, e1):
        P = self.P
        for e in range(e0, e1):
            P.dma_bg("gpsimd", (lambda e=e: (lambda h: h.dma_start(out=self.wg16[e], in_=self.wgate[e])))(), "w16_%d" % e)
            P.dma_bg("gpsimd", (lambda e=e: (lambda h: h.dma_start(out=self.wu16[e], in_=self.wup[e])))(), "w16_%d" % e)
            P.dma_bg("gpsimd", (lambda e=e: (lambda h: h.dma_start(out=self.wd16[e], in_=self.wdown[e])))(), "w16_%d" % e)

    def prep_weight(self, st, tag, src, dst, K, N, gcol):
        nc, P, pv = self.nc, self.P, self.pv
        kcs = K // 128
        srcv = src.rearrange("(kc p) n -> p kc n", p=128)
        dstv = dst.rearrange("(kc p) n -> p kc n", p=128)
        stg = [st.enter_context(nc.sbuf_tensor("stg%s%d" % (tag, i), [128, 8, 512], F32)) for i in range(2)]
        ob = [st.enter_context(nc.sbuf_tensor("ob%s%d" % (tag, i), [128, 8, 512], BF16)) for i in range(2)]
        ci = 0
        for c0 in range(0, N, 512):
            w = min(512, N - c0)
            b = ci % 2
            sn, on = "stg%s%d" % (tag, b), "ob%s%d" % (tag, b)
            P.dma("sync", (lambda b=b, c0=c0, w=w: (lambda h: h.dma_start(out=stg[b][:, 0:kcs, 0:w], in_=srcv[:, :, c0:c0 + w])))(), writes=[sn])
            for kc in range(kcs):
                if gcol is None:
                    if kc % 2 == 0:
                        P.op("vector", (lambda b=b, kc=kc, w=w: (lambda h: h.tensor_copy(ob[b][:, kc, 0:w], stg[b][:, kc, 0:w])))(), reads=[sn], writes=[on])
                    else:
                        P.op("scalar", (lambda b=b, kc=kc, w=w: (lambda h: h.activation(out=ob[b][:, kc, 0:w], in_=stg[b][:, kc, 0:w], func=AF.Copy)))(), reads=[sn], writes=[on])
                elif kc % 2 == 0:
                    P.op("vector", (lambda b=b, kc=kc, w=w: (lambda h: h.tensor_scalar(ob[b][:, kc, 0:w], stg[b][:, kc, 0:w], pv[:, gcol + kc:gcol + kc + 1], None, op0=ALU.mult)))(),
                         reads=[sn, "pv"], writes=[on])
                else:
                    P.op("scalar", (lambda b=b, kc=kc, w=w: (lambda h: h.activation(out=ob[b][:, kc, 0:w], in_=stg[b][:, kc, 0:w], func=AF.Copy, scale=pv[:, gcol + kc:gcol + kc + 1])))(),
                         reads=[sn, "pv"], writes=[on])
            P.dma("scalar", (lambda b=b, c0=c0, w=w: (lambda h: h.dma_start(out=dstv[:, :, c0:c0 + w], in_=ob[b][:, 0:kcs, 0:w])))(), reads=[on], writes=["dram_" + tag], semres=on)
            ci += 1

    def phase0(self):
        nc, P = self.nc, self.P
        with ExitStack() as st:
            zt = st.enter_context(nc.sbuf_tensor("zt", [128, 8, D], BF16))
            P.op("gpsimd", lambda h: h.memset(zt[:], 0.0), writes=["zt"])
            for r in range(NE * CAP // 1024):
                P.dma("scalar", (lambda r=r: (lambda h: h.dma_start(out=self.xbuf[r * 1024:(r + 1) * 1024, :].rearrange("(p a) n -> p a n", a=8), in_=zt[:])))(), reads=["zt"], writes=["xz%d" % r], semres="zt")
            P.barrier()
        with ExitStack() as st:
            self.prep_weight(st, "win", self.w_in, self.winb, D, IN_COLS, PV["gmix"])
        self.P.barrier()
        with ExitStack() as st:
            self.prep_weight(st, "wout", self.w_out, self.woutb, D, D, PV["gout"])
            self.prep_weight(st, "wq", self.mem_wq, self.wqb, D, 512, PV["gmemx"])
        self.P.barrier()
        with ExitStack() as st:
            self.prep_weight(st, "wkv", self.mem_wkv, self.wkvb, D, 1024, PV["gmem"])
            self.prep_weight(st, "wo", self.mem_wo, self.wob, 512, D, None)

    def norm_transpose(self, st, tag, src_fn, dstT, ntiles, pre=None):
        raise NotImplementedError

    def emit_kv(self, st2):
        nc, P = self.nc, self.P
        cb, pv, nhalf, knT, vmem = self.cb, self.pv, self.nhalf, self.knT, self.vmem
        sb2 = lambda name, shape, dt: st2.enter_context(nc.sbuf_tensor(name, shape, dt))
        pT_kv = st2.enter_context(nc.psum_tensor("pTB", [128, D], BF16))
        psQ_kv = st2.enter_context(nc.psum_tensor("psQB", [128, 512], F32))
        psN_kv = st2.enter_context(nc.psum_tensor("psNB", [128, 512], F32))
        wkvb = sb2("wkvb_s", [128, 8, D], BF16)
        mt_ = [sb2("memt%d" % i, [128, D], F32) for i in range(2)]
        mb_ = [sb2("memb%d" % i, [128, D], BF16) for i in range(2)]
        memT = sb2("memT", [128, 8, 256], BF16)
        junk_kv = sb2("junkM", [128, D], BF16)
        mstat = sb2("mstat", [128, 6], F32)
        sqm = sb2("sqm", [128, 256], BF16)
        vrm = sb2("vrm", [128, 256], F32)
        rsm = sb2("rsm", [128, 256], F32)
        P.dma("gpsimd", lambda h: h.dma_start(out=wkvb[:], in_=self.mem_wkv.rearrange("(kc p) n -> p kc n", p=128)), writes=["wkvb"])
        gmb = sb2("gmb", [128, D], F32)
        P.dma("sync", lambda h: h.dma_start(out=gmb[:], in_=self.bvec[:, BV_GMEM:BV_GMEM + D].partition_broadcast(128)), writes=["gmb"])
        for i in range(2):
            P.dma("sync", (lambda i=i: (lambda h: h.dma_start(out=mt_[i][:], in_=self.mem[i * 128:(i + 1) * 128, :])))(), writes=["memt%d" % i])
            P.op("scalar", (lambda i=i: (lambda h: h.activation(out=junk_kv[:], in_=mt_[i][:], func=AF.Square, accum_out=mstat[:, i:i + 1])))(), reads=["memt%d" % i], writes=["junkM", "ms%d" % i])
            P.op("vector", (lambda i=i: (lambda h: h.tensor_scalar(mstat[:, 2 + i:3 + i], mstat[:, i:i + 1], 1.0 / D, EPS, op0=ALU.mult, op1=ALU.add)))(), reads=["ms%d" % i], writes=["mv%d" % i])
            P.op("gpsimd", (lambda i=i: (lambda h: h.tensor_tensor(mstat[:, 4 + i:5 + i], mstat[:, 2 + i:3 + i], nhalf[:, 0:1], op=ALU.pow)))(), reads=["mv%d" % i, "nhalf"], writes=["mr%d" % i])
            P.op("vector", (lambda i=i: (lambda h: h.scalar_tensor_tensor(out=mb_[i][:], in0=mt_[i][:], scalar=mstat[:, 4 + i:5 + i], in1=gmb[:], op0=ALU.mult, op1=ALU.mult)))(), reads=["memt%d" % i, "mr%d" % i, "gmb"], writes=["memb%d" % i])
            for kc in range(8):
                P.op("tensor", (lambda kc=kc, i=i: (lambda h: h.transpose(pT_kv[:, kc * 128:(kc + 1) * 128], mb_[i][:, kc * 128:(kc + 1) * 128], cb[:, CB_ID:CB_ID + 128])))(),
                     reads=["memb%d" % i, "cb"], writes=["pTB"], inc=(kc == 7))
            P.op("vector", (lambda i=i: (lambda h: h.tensor_copy(memT[:, :, i * 128:(i + 1) * 128], pT_kv[:].rearrange("p (k t) -> p k t", k=8))))(), reads=["pTB"], writes=["memT%d" % i])
        mres = ["memT0", "memT1"]
        for hd in range(4):
            for kc in range(8):
                P.op("tensor", (lambda kc=kc, hd=hd: (lambda h: h.matmul(psQ_kv[:, 0:256], wkvb[:, kc, hd * 128:(hd + 1) * 128], memT[:, kc, :], start=(kc == 0), stop=(kc == 7))))(),
                     reads=["wkvb"] + mres, writes=["psQB"], inc=(kc == 7))
            P.op("scalar", lambda h: h.activation(out=sqm[:], in_=psQ_kv[:, 0:256], func=AF.Square), reads=["psQB"], writes=["sqm"])
            P.op("tensor", lambda h: h.matmul(psN_kv[:, 0:256], cb[:, CB_ONE:CB_ONE + 128], sqm[:], start=True, stop=True), reads=["cb", "sqm"], writes=["psNB"])
            P.op("scalar", lambda h: h.activation(out=vrm[:], in_=psN_kv[:, 0:256], func=AF.Sqrt, scale=1.0 / 128, bias=self.epsc[:, 0:1]), reads=["psNB", "epsc"], writes=["vrm"])
            P.op("vector", lambda h: h.reciprocal(rsm[:], vrm[:]), reads=["vrm"], writes=["rsm"])
            P.op("vector", (lambda hd=hd: (lambda h: h.scalar_tensor_tensor(out=knT[:, hd, :], in0=psQ_kv[:, 0:256], scalar=pv[:, PV["mkg"]:PV["mkg"] + 1], in1=rsm[:], op0=ALU.mult, op1=ALU.mult)))(),
                 reads=["psQB", "rsm", "pv"], writes=["knT"])
        for kt in range(2):
            for kc in range(8):
                P.op("tensor", (lambda kc=kc, kt=kt: (lambda h: h.matmul(psQ_kv[:], memT[:, kc, kt * 128:(kt + 1) * 128], wkvb[:, kc, 512:1024], start=(kc == 0), stop=(kc == 7))))(),
                     reads=["wkvb"] + mres, writes=["psQB"], inc=(kc == 7))
            P.op("vector", (lambda kt=kt: (lambda h: h.tensor_copy(vmem[:, kt, :], psQ_kv[:])))(), reads=["psQB"], writes=["vmem"])


    def emit_forget(self, st2):
        nc, P = self.nc, self.P
        hT, pv2 = self.hT, self.pv2
        winv = self.w_in.rearrange("(kc p) n -> p kc n", p=128)
        wf = st2.enter_context(nc.sbuf_tensor("wf", [128, 8, 8], BF16))
        psQ = st2.enter_context(nc.psum_tensor("psQf", [128, 512], F32))
        P.dma("gpsimd", lambda h: h.dma_start(out=wf[:], in_=winv[:, :, 2560:2568]), writes=["wf"])
        sb2 = lambda name, shape, dt: st2.enter_context(nc.sbuf_tensor(name, shape, dt))
        HW = 2048
        fe = sb2("fe", [8, HW], F32)
        fm0 = sb2("fm0", [8, HW], F32)
        fm = [fm0, fm0]
        fcar = sb2("fcar", [8, 1], F32)
        fr = fe
        pcs = [sb2("pc%d" % i, [8, HW], BF16) for i in range(7)]
        P.op("gpsimd", lambda h: h.memset(pcs[6][:], 1.0), writes=["pc6"])

        def block(tb):
            if True:
                tq = tb % 4
                t0 = tb * 512
                for kc in range(8):
                    P.op("tensor", (lambda kc=kc, t0=t0: (lambda h: h.matmul(psQ[0:8, :], wf[:, kc, :], hT[:, kc, t0:t0 + 512], start=(kc == 0), stop=(kc == 7))))(),
                         reads=["wf"] + ["hT%d" % (tb * 4 + j) for j in range(4)], writes=["psQf"], inc=(kc == 7))
                P.op("scalar", (lambda tq=tq: (lambda h: h.activation(out=fe[:, tq * 512:(tq + 1) * 512], in_=psQ[0:8, :], func=AF.Exp, scale=-1.0, bias=pv2[0:8, 14:15])))(),
                     reads=["psQf", "pv2f"], writes=["fe"])
        def half(hf):
            c0 = hf * HW
            P.op("scalar", lambda h: h.activation(out=fe[:], in_=fe[:], func=AF.Ln, bias=1.0), reads=["fe"], writes=["fe"])
            if hf == 0:
                P.op("vector", lambda h: h.tensor_tensor_scan(out=fm[0][:], data0=fe[:], data1=fe[:], initial=0.0, op0=ALU.add, op1=ALU.max), reads=["fe"], writes=["fm0"])
            else:
                P.op("vector", lambda h: h.tensor_copy(fcar[:], fm0[:, HW - 1:HW]), reads=["fm0"], writes=["fcar"])
                P.op("vector", lambda h: h.tensor_tensor_scan(out=fm0[:], data0=fe[:], data1=fe[:], initial=fcar[:, 0:1], op0=ALU.add, op1=ALU.max), reads=["fe", "fcar"], writes=["fm0"])
            fmh = fm[hf]
            fmn = "fm0"
            P.op("scalar", (lambda fmh=fmh: (lambda h: h.activation(out=pcs[0][:], in_=fmh[:], func=AF.Copy)))(), reads=[fmn], writes=["pc0"])
            P.op("vector", (lambda fmh=fmh: (lambda h: h.tensor_tensor(fr[:], fmh[:], pcs[0][:], op=ALU.subtract)))(), reads=[fmn, "pc0", "fe"], writes=["fe"])
            P.op("scalar", lambda h: h.activation(out=pcs[1][:], in_=fr[:], func=AF.Copy), reads=["fe"], writes=["pc1"])
            P.op("vector", lambda h: h.tensor_tensor(fr[:], fr[:], pcs[1][:], op=ALU.subtract), reads=["fe", "pc1"], writes=["fe"])
            P.op("scalar", lambda h: h.activation(out=pcs[2][:], in_=fr[:], func=AF.Copy), reads=["fe"], writes=["pc2"])
            for j in range(3):
                P.op("scalar", (lambda j=j: (lambda h: h.activation(out=pcs[3 + j][:], in_=pcs[j][:], func=AF.Copy, scale=-1.0)))(), reads=["pc%d" % j], writes=["pc%d" % (3 + j)])
            rowsrc = [3, 4, 5, 6, 6, 6, 6, 6, 6, 0, 1, 2]
            for row in range(12):
                src = rowsrc[row]
                P.dma("scalar", (lambda row=row, src=src, c0=c0: (lambda h: h.dma_start(out=self.cscr[:, row, c0:c0 + HW], in_=pcs[src][:])))(),
                      reads=["pc%d" % src], writes=["cscr_%d_%d" % (row, hf)], semres="pc%d" % src)
            if self.debug == "A3f":
                o = self.dbg_out("fm%d" % hf, [8, HW])
                P.dma("sync", (lambda o=o, fmh=fmh: (lambda h: h.dma_start(out=o[:, :], in_=fmh[:])))(), reads=[fmn], writes=["dbgf%d" % hf])
        return block, half


    def phaseA1(self, stA):
        nc, P = self.nc, self.P
        hT, cb, nhalf = self.hT, self.cb, self.nhalf
        with ExitStack() as st:
            xt = [st.enter_context(nc.sbuf_tensor("xt%d" % i, [128, D], F32)) for i in range(3)]
            hb = [st.enter_context(nc.sbuf_tensor("hb%d" % i, [128, D], BF16)) for i in range(2)]
            junk = st.enter_context(nc.sbuf_tensor("junkA", [128, D], BF16))
            stat = st.enter_context(nc.sbuf_tensor("statA", [128, 3 * NT], F32))
            gmixb = st.enter_context(nc.sbuf_tensor("gmixb", [128, D], F32))
            P.dma("sync", lambda h: h.dma_start(out=gmixb[:], in_=self.bvec[:, BV_GMIX:BV_GMIX + D].partition_broadcast(128)), writes=["gmixb"])
            pT = [st.enter_context(nc.psum_tensor("pTA%d" % i, [128, D], BF16)) for i in range(2)]
            fblock, fhalf = self.emit_forget(st)
            units = []
            for i in range(NT):
                def mk(i=i):
                    xb, b2 = i % 3, i % 2
                    xn, hn, pn = "xt%d" % xb, "hb%d" % b2, "pTA%d" % b2

                    def s0():
                        P.dma("sync", lambda h: h.dma_start(out=xt[xb][:], in_=self.x[i * 128:(i + 1) * 128, :]), writes=[xn])

                    def s1():
                        P.op("scalar", lambda h: h.activation(out=junk[:], in_=xt[xb][:], func=AF.Square, accum_out=stat[:, i:i + 1]), reads=[xn], writes=["junkA", "ssq%d" % i])
                        P.op("vector", lambda h: h.tensor_scalar(stat[:, NT + i:NT + i + 1], stat[:, i:i + 1], 1.0 / D, EPS, op0=ALU.mult, op1=ALU.add), reads=["ssq%d" % i], writes=["var%d" % i])
                        P.op("gpsimd", lambda h: h.tensor_tensor(stat[:, 2 * NT + i:2 * NT + i + 1], stat[:, NT + i:NT + i + 1], nhalf[:, 0:1], op=ALU.pow), reads=["var%d" % i, "nhalf"], writes=["rstd%d" % i])

                    def s2():
                        P.op("vector", lambda h: h.scalar_tensor_tensor(out=hb[b2][:], in0=xt[xb][:], scalar=stat[:, 2 * NT + i:2 * NT + i + 1], in1=gmixb[:], op0=ALU.mult, op1=ALU.mult),
                             reads=[xn, "rstd%d" % i, "gmixb"], writes=[hn])

                    def s3():
                        for kc in range(8):
                            P.op("tensor", (lambda kc=kc: (lambda h: h.transpose(pT[b2][:, kc * 128:(kc + 1) * 128], hb[b2][:, kc * 128:(kc + 1) * 128], cb[:, CB_ID:CB_ID + 128])))(),
                                 reads=[hn, "cb"], writes=[pn], inc=(kc == 7))

                    def s4():
                        if i % 2 == 0:
                            P.op("scalar", lambda h: h.activation(out=hT[:, :, i * 128:(i + 1) * 128], in_=pT[b2][:].rearrange("p (k t) -> p k t", k=8), func=AF.Copy), reads=[pn], writes=["hT%d" % i])
                        else:
                            P.op("vector", lambda h: h.tensor_copy(hT[:, :, i * 128:(i + 1) * 128], pT[b2][:].rearrange("p (k t) -> p k t", k=8)), reads=[pn], writes=["hT%d" % i])
                        if i == 5:
                            self.emit_kv(st)
                        if i % 4 == 3:
                            fblock(i // 4)
                        if i % 16 == 15:
                            fhalf(i // 16)
                    return [s0, s1, s2, s3, s4]
                units.append(mk())
            run_pipeline(units)
            if self.debug == "A1":
                o = self.dbg_out("hT", [128, 8 * S], BF16)
                P.dma("sync", lambda h: h.dma_start(out=o[:, :], in_=hT[:].rearrange("p k t -> p (k t)")), reads=["hT%d" % i for i in range(NT)], writes=["dbg"])

    def phaseA2(self):
        nc, P = self.nc, self.P
        hT, cb, pv, pv2 = self.hT, self.cb, self.pv, self.pv2
        winv = self.w_in.rearrange("(kc p) n -> p kc n", p=128)
        with ExitStack() as st:
            sb = lambda name, shape, dt: st.enter_context(nc.sbuf_tensor(name, shape, dt))
            lw = sb("lw", [128, 1024], F32)
            lwb = sb("lwb", [128, 1024], BF16)
            wu = [sb("wu%d" % i, [128, 8, 128], BF16) for i in range(2)]
            wg = [sb("wg%d" % i, [128, 8, 128], BF16) for i in range(2)]
            ub = [sb("ub%d" % i, [128, 515], F32) for i in range(2)]
            yb = sb("yb0", [128, S], BF16)
            aaF = sb("aaF", [128, S], F32)
            t1F = sb("t1F", [128, S], F32)
            omF = sb("omF", [128, S], F32)
            geF = sb("geF", [128, S], F32)
            names = ["xc", "tr", "ti", "gs", "g2"]
            T = {n: [sb(n + "%d" % i, [128, 512], F32) for i in range(2)] for n in names}
            xcb = [sb("xcb%d" % i, [128, 512], BF16) for i in range(2)]
            psU = [st.enter_context(nc.psum_tensor("psU%d" % i, [128, 512], F32)) for i in range(2)]
            psG = [st.enter_context(nc.psum_tensor("psG%d" % i, [128, 512], F32)) for i in range(2)]
            psR = [st.enter_context(nc.psum_tensor("psR%d" % i, [128, 512], F32)) for i in range(2)]
            psI = [st.enter_context(nc.psum_tensor("psI%d" % i, [128, 512], F32)) for i in range(2)]
            P.dma("sync", lambda h: h.dma_start(out=lw[:], in_=self.lruw[:, :]), writes=["lw"])
            P.op("vector", lambda h: h.tensor_copy(lwb[:], lw[:]), reads=["lw"], writes=["lwb"])
            def load_ct(ct):
                cp = ct % 2
                P.dma("gpsimd", (lambda ct=ct, cp=cp: (lambda h: h.dma_start(out=wu[cp][:], in_=winv[:, :, ct * 128:(ct + 1) * 128])))(), writes=["wu%d" % cp])
                P.dma("gpsimd", (lambda ct=ct, cp=cp: (lambda h: h.dma_start(out=wg[cp][:], in_=winv[:, :, 512 + ct * 128:512 + (ct + 1) * 128])))(), writes=["wg%d" % cp])
            load_ct(0)
            load_ct(1)
            allb = lambda n: [n + "%d" % t for t in range(NB)]
            units = []
            for ct in range(4):
                for tb in range(NB):
                    def mk(ct=ct, tb=tb):
                        it = ct * NB + tb
                        b = it % 2
                        cp = ct % 2
                        cwc = PV["convw"] + ct * 4
                        t0 = tb * 512
                        blk = slice(t0, t0 + 512)
                        hres = ["hT%d" % (tb * 4 + j) for j in range(4)]
                        R_ = lambda n: n + "%d" % b
                        xc, tr, ti, gs, g2 = (T[n][b] for n in ["xc", "tr", "ti", "gs", "g2"])
                        ur = [R_("ubm"), R_("ubh")]

                        def s0():
                            if tb == 0 and ct >= 1 and ct + 1 < 4:
                                load_ct(ct + 1)
                            if tb % 2 == 0:
                                self.precast(4 * ct + tb // 2, 4 * ct + tb // 2 + 1)
                            for kc in range(8):
                                P.op("tensor", (lambda kc=kc: (lambda h: h.matmul(psU[b][:], wu[cp][:, kc, :], hT[:, kc, t0:t0 + 512], start=(kc == 0), stop=(kc == 7))))(),
                                     reads=["wu%d" % cp] + hres, writes=[R_("psU")], inc=(kc == 7))
                            for kc in range(8):
                                P.op("tensor", (lambda kc=kc: (lambda h: h.matmul(psG[b][:], wg[cp][:, kc, :], hT[:, kc, t0:t0 + 512], start=(kc == 0), stop=(kc == 7))))(),
                                     reads=["wg%d" % cp] + hres, writes=[R_("psG")], inc=(kc == 7))

                        def s1():
                            P.op("scalar", lambda h: h.activation(out=ub[b][:, 3:515], in_=psU[b][:], func=AF.Copy), reads=[R_("psU")], writes=[R_("ubm")])
                            yield
                            if tb == 0:
                                P.op("gpsimd", lambda h: h.memset(ub[b][:, 0:3], 0.0), writes=[R_("ubh")])
                            else:
                                P.op("gpsimd", lambda h: h.tensor_copy(ub[b][:, 0:3], ub[1 - b][:, 512:515]), reads=["ubm%d" % (1 - b)], writes=[R_("ubh")])
                            P.op("scalar", lambda h: h.activation(out=gs[:], in_=psG[b][:], func=AF.Copy), reads=[R_("psG")], writes=[R_("gs")])
                            yield
                            P.op("scalar", lambda h: h.activation(out=g2[:], in_=psG[b][:], func=AF.Square), reads=[R_("psG")], writes=[R_("g2")])
                            yield
                            P.op("gpsimd", lambda h: h.tensor_scalar(g2[:], g2[:], 0.044715, 1.0, op0=ALU.mult, op1=ALU.add), reads=[R_("g2")], writes=[R_("g2")])
                            yield
                            P.op("vector", lambda h: h.tensor_scalar(xc[:], ub[b][:, 0:512], pv[:, cwc:cwc + 1], pv[:, PV["convb"] + ct:PV["convb"] + ct + 1], op0=ALU.mult, op1=ALU.add),
                                 reads=ur + ["pv"], writes=[R_("xc")])
                            for tap in range(1, 4):
                                P.op("vector", (lambda tap=tap: (lambda h: h.scalar_tensor_tensor(out=xc[:], in0=ub[b][:, tap:tap + 512], scalar=pv[:, cwc + tap:cwc + tap + 1], in1=xc[:], op0=ALU.mult, op1=ALU.add)))(),
                                     reads=ur + ["pv", R_("xc")], writes=[R_("xc")])
                            P.op("scalar", lambda h: h.activation(out=xcb[b][:], in_=xc[:], func=AF.Copy), reads=[R_("xc")], writes=[R_("xcb")])
                            yield
                            P.op("vector", lambda h: h.tensor_tensor(g2[:], g2[:], gs[:], op=ALU.mult), reads=[R_("g2"), R_("gs")], writes=[R_("g2")])
                            yield
                            P.op("scalar", lambda h: h.activation(out=g2[:], in_=g2[:], func=AF.Tanh, scale=0.7978845608028654), reads=[R_("g2")], writes=[R_("g2")])
                            yield
                            P.op("vector", lambda h: h.scalar_tensor_tensor(out=geF[:, blk], in0=g2[:], scalar=1.0, in1=gs[:], op0=ALU.add, op1=ALU.mult), reads=[R_("g2"), R_("gs")], writes=["geF%d" % tb])
                            yield

                        def s2():
                            P.op("tensor", lambda h: h.matmul(psR[b][:], lwb[:, ct * 256:ct * 256 + 128], xcb[b][:], start=True, stop=True), reads=["lwb", R_("xcb")], writes=[R_("psR")])
                            yield
                            P.op("tensor", lambda h: h.matmul(psI[b][:], lwb[:, ct * 256 + 128:ct * 256 + 256], xcb[b][:], start=True, stop=True), reads=["lwb", R_("xcb")], writes=[R_("psI")])
                            yield
                            P.op("scalar", lambda h: h.activation(out=tr[:], in_=psR[b][:], func=AF.Tanh, scale=0.5, bias=pv2[:, ct:ct + 1]), reads=[R_("psR"), "pv2a"], writes=[R_("tr")])
                            yield
                            P.op("scalar", lambda h: h.activation(out=ti[:], in_=psI[b][:], func=AF.Tanh, scale=0.5, bias=pv2[:, 4 + ct:5 + ct]), reads=[R_("psI"), "pv2b"], writes=[R_("ti")])
                            yield
                            P.op("scalar", lambda h: h.activation(out=aaF[:, blk], in_=tr[:], func=AF.Exp, scale=pv2[:, 8 + ct:9 + ct], bias=pv2[:, 8 + ct:9 + ct]), reads=[R_("tr"), "pv2c"], writes=["aaF%d" % tb])
                            yield
                            P.op("scalar", lambda h: h.activation(out=omF[:, blk], in_=aaF[:, blk], func=AF.Square), reads=["aaF%d" % tb], writes=["omF%d" % tb])
                            yield
                            P.op("gpsimd", lambda h: h.tensor_scalar(omF[:, blk], omF[:, blk], -1.0, 1.0, op0=ALU.mult, op1=ALU.add), reads=["omF%d" % tb], writes=["omF%d" % tb])
                            yield
                            P.op("vector", lambda h: h.scalar_tensor_tensor(out=t1F[:, blk], in0=ti[:], scalar=1.0, in1=xc[:], op0=ALU.add, op1=ALU.mult), reads=[R_("ti"), R_("xc")], writes=["t1F%d" % tb])
                            yield
                            if tb == NB - 1:
                                P.op("scalar", lambda h: h.activation(out=omF[:], in_=omF[:], func=AF.Sqrt), reads=allb("omF"), writes=allb("omF"))
                                P.op("vector", lambda h: h.scalar_tensor_tensor(out=omF[:], in0=t1F[:], scalar=0.5, in1=omF[:], op0=ALU.mult, op1=ALU.mult), reads=allb("t1F") + allb("omF"), writes=allb("omF"))
                                P.op("vector", lambda h: h.tensor_tensor_scan(out=t1F[:], data0=aaF[:], data1=omF[:], initial=0.0, op0=ALU.mult, op1=ALU.add), reads=allb("aaF") + allb("omF"), writes=allb("t1F"))
                                P.op("vector", lambda h: h.scalar_tensor_tensor(out=yb[:], in0=geF[:], scalar=pv2[:, 16 + ct:17 + ct], in1=t1F[:], op0=ALU.mult, op1=ALU.mult), reads=allb("geF") + allb("t1F") + ["pv2g"], writes=["yb0"])
                                P.dma("scalar", lambda h: h.dma_start(out=self.ymix[ct * 128:(ct + 1) * 128, :], in_=yb[:]), reads=["yb0"], writes=["ymix_l%d" % ct], semres="yb0")
                        def s2_full():
                            for _ in s2():
                                pass
                        return [s0, s1, s2_full if tb == NB - 1 else s2]
                    units.append(mk())
            run_pipeline(units)

    def phaseA3(self):
        nc, P = self.nc, self.P
        hT, cb, pv, pv2, nhalf = self.hT, self.cb, self.pv, self.pv2, self.nhalf
        winv = self.w_in.rearrange("(kc p) n -> p kc n", p=128)
        QC, KC, VC, FC = 1024, 1536, 2048, 2560
        with ExitStack() as st:
            sb = lambda name, shape, dt: st.enter_context(nc.sbuf_tensor(name, shape, dt))
            ps = lambda name, shape, dt=F32: st.enter_context(nc.psum_tensor(name, shape, dt))
            vtok = sb("vtok", [128, NT, 512], BF16)
            wv = sb("wv", [128, 8, 512], BF16)
            psS = [ps("psS%d" % i, [128, 2, 512]) for i in range(2)]
            psO = [ps("psO%d" % i, [128, 512]) for i in range(2)]
            psQ = ps("psQ", [128, 512])
            psN = ps("psN", [128, 512])
            P.dma("gpsimd", lambda h: h.dma_start(out=wv[:], in_=winv[:, :, VC:VC + 512]), writes=["wv"])
            for i in range(NT):
                pq = psS[i % 2]
                pn = "psS%d_0" % (i % 2)
                for kc in range(8):
                    P.op("tensor", (lambda kc=kc, i=i, pq=pq: (lambda h: h.matmul(pq[:, 0, :], hT[:, kc, i * 128:(i + 1) * 128], wv[:, kc, :], start=(kc == 0), stop=(kc == 7))))(),
                         reads=["wv", "hT%d" % i], writes=[pn], inc=(kc == 7))
                eng = "scalar" if i % 2 == 0 else "vector"
                if eng == "scalar":
                    P.op("scalar", (lambda i=i, pq=pq: (lambda h: h.activation(out=vtok[:, i, :], in_=pq[:, 0, :], func=AF.Copy)))(), reads=[pn], writes=["vtok%d" % i])
                else:
                    P.op("vector", (lambda i=i, pq=pq: (lambda h: h.tensor_copy(vtok[:, i, :], pq[:, 0, :])))(), reads=[pn], writes=["vtok%d" % i])
            sb = lambda name, shape, dt: st.enter_context(nc.sbuf_tensor(name, shape, dt))
            wqk = [sb("wqk%d" % i, [128, 8, 256], BF16) for i in range(2)]
            qa = [sb("qa%d" % i, [128, S], BF16) for i in range(2)]
            ka = [sb("ka%d" % i, [128, S], BF16) for i in range(2)]
            va = [sb("va%d" % i, [128, NT, 128], BF16) for i in range(2)]
            yf = [sb("yf0", [128, S], BF16)] * 2
            sq = [sb("sq%d" % i, [128, 512], BF16) for i in range(2)]
            vr = [sb("vr%d" % i, [128, 512], F32) for i in range(2)]
            rs = [sb("rs%d" % i, [128, 512], F32) for i in range(2)]
            pt = [sb("pt%d" % i, [128, 2, 512], BF16) for i in range(3)]
            rl = [sb("rl%d" % i, [64, 512], F32) for i in range(2)]
            for i in range(2):
                P.op("gpsimd", (lambda i=i: (lambda h: h.memset(va[i][:, :, 64:128], 1.0)))(), writes=["va1_%d" % i])
            nrm = 0
            pti = 0
            oi = 0
            for p in range(4):
                pp = p % 2
                P.dma("gpsimd", (lambda p=p, pp=pp: (lambda h: h.dma_start(out=wqk[pp][:, :, 0:128], in_=winv[:, :, QC + p * 128:QC + (p + 1) * 128])))(), writes=["wqk%d" % pp])
                P.dma("gpsimd", (lambda p=p, pp=pp: (lambda h: h.dma_start(out=wqk[pp][:, :, 128:256], in_=winv[:, :, KC + p * 128:KC + (p + 1) * 128])))(), writes=["wqk%d" % pp])
                for hp in range(2):
                    hd = 2 * p + hp
                    P.dma("sync", (lambda hp=hp, hd=hd: (lambda h: h.dma_start(out=qa[hp][64:70, :], in_=self.cscr[hd, 0:6, :])))(), writes=["qa%d_aug" % hp])
                    P.dma("sync", (lambda hp=hp, hd=hd: (lambda h: h.dma_start(out=ka[hp][64:70, :], in_=self.cscr[hd, 6:12, :])))(), writes=["ka%d_aug" % hp])
                    P.op("gpsimd", (lambda hp=hp, hd=hd: (lambda h: h.tensor_copy(va[hp][:, :, 0:64], vtok[:, :, hd * 64:(hd + 1) * 64])))(),
                         reads=["vtok%d" % i for i in range(NT)], writes=["va0_%d" % hp])
                if p == 1:
                    for r in range(NE * CAP // 256):
                        P.dma_bg("gpsimd", (lambda r=r: (lambda h: h.dma_start(out=self.xbuf[r * 256:(r + 1) * 256, :].rearrange("(p a) n -> p a n", a=2), in_=self.zt[:])))(), "xz")
                PQ4 = [(psQ[:], "psQ"), (psS[0][:, 0, :], "psS0_0"), (psS[0][:, 1, :], "psS0_1"), (psS[1][:, 0, :], "psS1_0")]
                PN2 = [(psN[:], "psN"), (psS[1][:, 1, :], "psS1_1")]
                units = []
                for tb in range(NB):
                    for which in range(2):
                        def mk(tb=tb, which=which, nrm=nrm, pp=pp):
                            t0 = tb * 512
                            hres = ["hT%d" % (tb * 4 + j) for j in range(4)]
                            nb = nrm % 2
                            pq, pqn = PQ4[nrm % 4]
                            pn_, pnn = PN2[nrm % 2]
                            dst = qa if which == 0 else ka
                            gcol = pv2[:, 12:13] if which == 0 else pv[:, PV["gk2"]:PV["gk2"] + 1]
                            gres = "pv2d" if which == 0 else "pv"

                            def s0():
                                for kc in range(8):
                                    P.op("tensor", (lambda kc=kc: (lambda h: h.matmul(pq, wqk[pp][:, kc, which * 128:(which + 1) * 128], hT[:, kc, t0:t0 + 512], start=(kc == 0), stop=(kc == 7))))(),
                                         reads=["wqk%d" % pp] + hres, writes=[pqn], inc=(kc == 7))

                            def s1():
                                P.op("scalar", lambda h: h.activation(out=sq[nb][:], in_=pq, func=AF.Square), reads=[pqn], writes=["sq%d" % nb])

                            def s1b():
                                P.op("tensor", lambda h: h.matmul(pn_, cb[:, CB_BO:CB_BO + 128], sq[nb][:], start=True, stop=True), reads=["cb", "sq%d" % nb], writes=[pnn])

                            def s2():
                                P.op("scalar", lambda h: h.activation(out=vr[nb][:], in_=pn_, func=AF.Ln, scale=1.0 / 64, bias=self.epsc[:, 0:1]), reads=[pnn, "epsc"], writes=["vr%d" % nb])
                                P.op("scalar", lambda h: h.activation(out=rs[nb][:], in_=vr[nb][:], func=AF.Exp, scale=-0.5), reads=["vr%d" % nb], writes=["rs%d" % nb])
                                for hp in range(2):
                                    P.op("vector", (lambda hp=hp: (lambda h: h.scalar_tensor_tensor(out=dst[hp][0:64, t0:t0 + 512], in0=pq[hp * 64:(hp + 1) * 64, :], scalar=gcol[hp * 64:(hp + 1) * 64, :], in1=rs[nb][hp * 64:(hp + 1) * 64, :], op0=ALU.mult, op1=ALU.mult)))(),
                                         reads=[pqn, "rs%d" % nb, gres], writes=[("qa%d_%d" if which == 0 else "ka%d_%d") % (hp, tb)])
                            return [s0, s1, s1b, s2]
                        units.append(mk())
                        nrm += 1
                run_pipeline(units)
                if self.debug == "A3q" and p == 0:
                    o = self.dbg_out("qa", [128, S], BF16)
                    o2 = self.dbg_out("ka", [128, S], BF16)
                    P.dma("sync", lambda h: h.dma_start(out=o[:, :], in_=qa[0][:]), reads=["qa0_%d" % t for t in range(NB)] + ["qa0_aug"], writes=["dbg"])
                    P.dma("sync", lambda h: h.dma_start(out=o2[:, :], in_=ka[0][:]), reads=["ka0_%d" % t for t in range(NB)] + ["ka0_aug"], writes=["dbg2"])
                units = []
                for hp in range(2):
                    for j in range(NB):
                        ob = oi % 2
                        oi += 1
                        nkt = 4 * j + 4
                        for g in range(0, nkt, 2):
                            def mk(hp=hp, j=j, g=g, ob=ob, nkt=nkt, pti=pti, pp=pp, p=p, gi=len(units)):
                                q0 = j * 512
                                sbi = pti % 2
                                ptb = pti % 3
                                qres = ["qa%d_%d" % (hp, j), "qa%d_aug" % hp]

                                cst = [max(0, 128 * (g + u - 4 * j)) for u in range(2)]
                                ce = min(cst)

                                def s0():
                                    if gi % 36 == 0:
                                        self.precast(16 + 4 * p + gi // 36, 16 + 4 * p + gi // 36 + 1)
                                    for u in range(2):
                                        i = g + u
                                        m = i - 4 * j
                                        c0 = cst[u]
                                        kres = ["ka%d_%d" % (hp, i // 4), "ka%d_aug" % hp]
                                        last = (m < 0)
                                        P.op("tensor", (lambda u=u, i=i, last=last, c0=c0: (lambda h: h.matmul(psS[sbi][:, u, c0:512], ka[hp][0:70, i * 128:(i + 1) * 128], qa[hp][0:70, q0 + c0:q0 + 512], start=True, stop=last)))(),
                                             reads=kres + qres, writes=["psS%d_%d" % (sbi, u)], inc=last)
                                        if m >= 0:
                                            P.op("tensor", (lambda u=u, c0=c0: (lambda h: h.matmul(psS[sbi][:, u, c0:c0 + 128], cb[:, CB_ID:CB_ID + 128], cb[:, CB_MASK:CB_MASK + 128], start=False, stop=True)))(),
                                                 reads=["cb"], writes=["psS%d_%d" % (sbi, u)], inc=True)

                                def s1():
                                    P.op("scalar", lambda h: h.activation(out=pt[ptb][:, :, ce:512], in_=psS[sbi][:, :, ce:512], func=AF.Exp), reads=["psS%d_0" % sbi, "psS%d_1" % sbi], writes=["pt%d" % ptb])

                                def s2():
                                    for u in range(2):
                                        i = g + u
                                        c0 = cst[u]
                                        P.op("tensor", (lambda u=u, i=i, c0=c0: (lambda h: h.matmul(psO[ob][:, c0:512], va[hp][:, i, :], pt[ptb][:, u, c0:512], start=(i == 0), stop=(i == nkt - 1))))(),
                                             reads=["va0_%d" % hp, "va1_%d" % hp, "pt%d" % ptb], writes=["psO%d" % ob], inc=(u == 1))
                                    if g + 2 >= nkt:
                                        P.op("vector", lambda h: h.reciprocal(rl[ob][:], psO[ob][64:128, :]), reads=["psO%d" % ob], writes=["rl%d" % ob])
                                        P.op("vector", lambda h: h.scalar_tensor_tensor(out=yf[pp][hp * 64:(hp + 1) * 64, q0:q0 + 512], in0=psO[ob][0:64, :], scalar=pv[0:64, PV["gfx"] + 2 * p + hp:PV["gfx"] + 2 * p + hp + 1], in1=rl[ob][:], op0=ALU.mult, op1=ALU.mult),
                                             reads=["psO%d" % ob, "rl%d" % ob, "pv"], writes=["yf0"])
                                return [s0, s1, s2]
                            units.append(mk())
                            pti += 1
                run_pipeline(units)
                P.dma("scalar", (lambda p=p, pp=pp: (lambda h: h.dma_start(out=self.ymix[512 + p * 128:512 + (p + 1) * 128, :], in_=yf[pp][:])))(), reads=["yf0"], writes=["ymix_f%d" % p], semres="yf0")


    def phaseB(self):
        nc, P = self.nc, self.P
        cb, cf, pv, pv2, nhalf = self.cb, self.cf, self.pv, self.pv2, self.nhalf
        gates, idxs = self.gates, self.idxs
        with ExitStack() as st:
            sb = lambda name, shape, dt: st.enter_context(nc.sbuf_tensor(name, shape, dt))
            ps = lambda name, shape, dt=F32: st.enter_context(nc.psum_tensor(name, shape, dt))
            knT, vmem = self.knT, self.vmem
            gbc = sb("gbc", [128, 1060], F32)
            wr32 = sb("wr32", [128, 8, 36], F32)
            cntrow = sb("cntrow", [1, 32], F32)
            P.dma("sync", lambda h: h.dma_start(out=gbc[:], in_=self.bvec[:, 0:1060].partition_broadcast(128)), writes=["gbc"])
            P.dma("sync", lambda h: h.dma_start(out=wr32[:], in_=self.wr.rearrange("(kc p) n -> p kc n", p=128)), writes=["wr32"])
            P.op("gpsimd", lambda h: h.memset(cntrow[:], 0.0), writes=["cntrow"])
            self.x1buf = nc.dram_tensor("x1buf", [S, D], F32).ap()
            sb = lambda name, shape, dt: st.enter_context(nc.sbuf_tensor(name, shape, dt))
            qnT = sb("qnT_all", [128, 4, S], BF16)
            statB = sb("statB", [128, NT, 8], F32)
            sX = ExitStack()
            xnT = sX.enter_context(nc.sbuf_tensor("xnT_all", [128, 8, S], BF16))
            ymv = self.ymix.rearrange("(cc p) t -> p cc t", p=128)
            with ExitStack() as s1:
                sb1 = lambda name, shape, dt: s1.enter_context(nc.sbuf_tensor(name, shape, dt))
                ps1 = lambda name, shape, dt=F32: s1.enter_context(nc.psum_tensor(name, shape, dt))
                woutb = sb1("woutb_s", [128, 8, D], BF16)
                yt = [sb1("yt%d" % i, [128, 8, 512], BF16) for i in range(2)]
                ysq = sb1("ysq", [128, 8, 512], BF16)
                xt = [sb1("xtB%d" % i, [128, D], F32) for i in range(3)]
                x1t = [sb1("x1t%d" % i, [128, D], F32) for i in range(4)]
                xnb = [sb1("xnb%d" % i, [128, D], BF16) for i in range(2)]
                junk_b1 = sb1("junkB1", [128, D], BF16)
                bkS = ps1("bkS", [128, 512])
                pW_b1 = [[ps1("pW%d%d" % (a_, b_), [128, 512]) for b_ in range(2)] for a_ in range(2)]
                pT_b1 = [ps1("pTB%d" % i, [128, D], BF16) for i in range(2)]
                gmxb = sb1("gmxb", [128, D], F32)
                P.dma("sync", lambda h: h.dma_start(out=gmxb[:], in_=self.bvec[:, BV_GMEMX:BV_GMEMX + D].partition_broadcast(128)), writes=["gmxb"])
                P.dma("gpsimd", lambda h: h.dma_start(out=woutb[:], in_=self.w_out.rearrange("(kc p) n -> p kc n", p=128)), writes=["woutb"])
                units = []
                for i in range(NT):
                    def mk(i=i):
                        tb, tt = i // 4, i % 4
                        yb = tb % 2
                        x3 = i % 3
                        b2 = i % 2
                        ts = slice(tt * 128, (tt + 1) * 128)
                        c0 = (i % 8) * 2
                        ytn, xn_, x1n = "yt%d" % yb, "xtB%d" % x3, "x1t%d" % (i % 4)

                        def s0():
                            if tt == 0:
                                P.dma("sync", lambda h: h.dma_start(out=yt[yb][:], in_=ymv[:, :, tb * 512:(tb + 1) * 512]), writes=[ytn])
                                P.op("vector", lambda h: h.tensor_tensor(ysq[:], yt[yb][:], yt[yb][:], op=ALU.mult), reads=[ytn], writes=["ysq"])
                            P.dma("sync", lambda h: h.dma_start(out=xt[x3][:], in_=self.x[i * 128:(i + 1) * 128, :]), writes=[xn_])

                        def s1_():
                            for grp in range(2):
                                for c4 in range(4):
                                    cc = grp * 4 + c4
                                    P.op("tensor", (lambda cc=cc, grp=grp, c4=c4: (lambda h: h.matmul(bkS[:, c0 + grp:c0 + grp + 1], ysq[:, cc, ts], self.gsq[:, cc:cc + 1], start=(c4 == 0), stop=(c4 == 3))))(),
                                         reads=["ysq", "gsq"], writes=["bkS"], inc=(c4 == 3))
                            for half in range(2):
                                hs_ = slice(half * 512, (half + 1) * 512)
                                for grp in range(2):
                                    for c4 in range(4):
                                        cc = grp * 4 + c4
                                        P.op("tensor", (lambda cc=cc, grp=grp, c4=c4, half=half, hs_=hs_: (lambda h: h.matmul(pW_b1[half][grp][:], yt[yb][:, cc, ts], woutb[:, cc, hs_], start=(c4 == 0), stop=(c4 == 3))))(),
                                             reads=[ytn, "woutb"], writes=["pW%d%d" % (half, grp)], inc=(c4 == 3))

                        def s2():
                            P.op("vector", lambda h: h.tensor_scalar(statB[:, i, 0:2], bkS[:, c0:c0 + 2], 1.0 / 512, EPS, op0=ALU.mult, op1=ALU.add), reads=["bkS"], writes=["sB01_%d" % i])
                            yield
                            P.op("gpsimd", lambda h: h.tensor_tensor(statB[:, i, 2:4], statB[:, i, 0:2], nhalf[:, 0:2], op=ALU.pow), reads=["sB01_%d" % i, "nhalf"], writes=["sB23_%d" % i])
                            yield
                            for half in range(2):
                                hs_ = slice(half * 512, (half + 1) * 512)
                                P.op("vector", (lambda half=half, hs_=hs_: (lambda h: h.scalar_tensor_tensor(out=x1t[i % 4][:, hs_], in0=pW_b1[half][0][:], scalar=statB[:, i, 2:3], in1=xt[x3][:, hs_], op0=ALU.mult, op1=ALU.add)))(),
                                     reads=["pW%d0" % half, "sB23_%d" % i, xn_], writes=[x1n])
                                P.op("vector", (lambda half=half, hs_=hs_: (lambda h: h.scalar_tensor_tensor(out=x1t[i % 4][:, hs_], in0=pW_b1[half][1][:], scalar=statB[:, i, 3:4], in1=x1t[i % 4][:, hs_], op0=ALU.mult, op1=ALU.add)))(),
                                     reads=["pW%d1" % half, "sB23_%d" % i, x1n], writes=[x1n])
                            P.dma("gpsimd", lambda h: h.dma_start(out=self.x1buf[i * 128:(i + 1) * 128, :], in_=x1t[i % 4][:]), reads=[x1n], writes=["x1buf%d" % i], semres=x1n)
                            P.op("scalar", lambda h: h.activation(out=junk_b1[:], in_=x1t[i % 4][:], func=AF.Square, accum_out=statB[:, i, 4:5]), reads=[x1n], writes=["junkB1", "sB4_%d" % i])
                            yield

                        def s2b():
                            P.op("vector", lambda h: h.tensor_scalar(statB[:, i, 5:6], statB[:, i, 4:5], 1.0 / D, EPS, op0=ALU.mult, op1=ALU.add), reads=["sB4_%d" % i], writes=["sB5_%d" % i])
                            yield
                            P.op("gpsimd", lambda h: h.tensor_tensor(statB[:, i, 6:7], statB[:, i, 5:6], nhalf[:, 0:1], op=ALU.pow), reads=["sB5_%d" % i, "nhalf"], writes=["sB6_%d" % i])
                            yield
                            P.op("vector", lambda h: h.scalar_tensor_tensor(out=xnb[b2][:], in0=x1t[i % 4][:], scalar=statB[:, i, 6:7], in1=gmxb[:], op0=ALU.mult, op1=ALU.mult), reads=[x1n, "sB6_%d" % i, "gmxb"], writes=["xnb%d" % b2])
                            yield

                        def s3():
                            for kc in range(8):
                                P.op("tensor", (lambda kc=kc: (lambda h: h.transpose(pT_b1[b2][:, kc * 128:(kc + 1) * 128], xnb[b2][:, kc * 128:(kc + 1) * 128], cb[:, CB_ID:CB_ID + 128])))(),
                                     reads=["xnb%d" % b2, "cb"], writes=["pTB%d" % b2], inc=(kc == 7))
                            if i % 2 == 0:
                                P.op("scalar", lambda h: h.activation(out=xnT[:, :, i * 128:(i + 1) * 128], in_=pT_b1[b2][:].rearrange("p (k t) -> p k t", k=8), func=AF.Copy), reads=["pTB%d" % b2], writes=["xnT%d" % i])
                            else:
                                P.op("vector", lambda h: h.tensor_copy(xnT[:, :, i * 128:(i + 1) * 128], pT_b1[b2][:].rearrange("p (k t) -> p k t", k=8)), reads=["pTB%d" % b2], writes=["xnT%d" % i])
                        return [s0, s1_, s2, s2b, s3]
                    units.append(mk())
                run_pipeline(units)
                if self.debug == "B":
                    self._o1 = self.dbg_out("x1", [S, D])
                    self._o2 = self.dbg_out("x2", [S, D])
                P.barrier()
            with ExitStack() as s2a:
                sb2 = lambda name, shape, dt: s2a.enter_context(nc.sbuf_tensor(name, shape, dt))
                ps2 = lambda name, shape, dt=F32: s2a.enter_context(nc.psum_tensor(name, shape, dt))
                wqb = sb2("wqb_s", [128, 8, 512], BF16)
                sq_2a = [sb2("sqB%d" % i, [128, 512], BF16) for i in range(2)]
                vr_2a = [sb2("vrB%d" % i, [128, 512], F32) for i in range(2)]
                rs_2a = [sb2("rsB%d" % i, [128, 512], F32) for i in range(2)]
                PQ_2a = [ps2("psQB%d" % i, [128, 512]) for i in range(4)]
                PN_2a = [ps2("psNB%d" % i, [128, 512]) for i in range(2)]
                P.dma("gpsimd", lambda h: h.dma_start(out=wqb[:], in_=self.mem_wq.rearrange("(kc p) n -> p kc n", p=128)), writes=["wqb"])

                units = []
                u = 0
                for tb in range(NB):
                    for hd in range(4):
                        def mk(tb=tb, hd=hd, u=u):
                            t0 = tb * 512
                            nb = u % 2
                            pq, pn_ = PQ_2a[u % 4], PN_2a[u % 2]
                            pqn, pnn = "psQB%d" % (u % 4), "psNB%d" % (u % 2)
                            xres = ["xnT%d" % (tb * 4 + t) for t in range(4)]

                            def s0():
                                for kc in range(8):
                                    P.op("tensor", (lambda kc=kc: (lambda h: h.matmul(pq[:], wqb[:, kc, hd * 128:(hd + 1) * 128], xnT[:, kc, t0:t0 + 512], start=(kc == 0), stop=(kc == 7))))(),
                                         reads=["wqb"] + xres, writes=[pqn], inc=(kc == 7))

                            def s1_():
                                P.op("scalar", lambda h: h.activation(out=sq_2a[nb][:], in_=pq[:], func=AF.Square), reads=[pqn], writes=["sqB%d" % nb])

                            def s1b():
                                P.op("tensor", lambda h: h.matmul(pn_[:], cb[:, CB_ONE:CB_ONE + 128], sq_2a[nb][:], start=True, stop=True), reads=["cb", "sqB%d" % nb], writes=[pnn])

                            def s2():
                                P.op("scalar", lambda h: h.activation(out=vr_2a[nb][:], in_=pn_[:], func=AF.Ln, scale=1.0 / 128, bias=self.epsc[:, 0:1]), reads=[pnn, "epsc"], writes=["vrB%d" % nb])
                                P.op("scalar", lambda h: h.activation(out=rs_2a[nb][:], in_=vr_2a[nb][:], func=AF.Exp, scale=-0.5), reads=["vrB%d" % nb], writes=["rsB%d" % nb])
                                P.op("vector", lambda h: h.scalar_tensor_tensor(out=qnT[:, hd, t0:t0 + 512], in0=pq[:], scalar=pv2[:, 13:14], in1=rs_2a[nb][:], op0=ALU.mult, op1=ALU.mult),
                                     reads=[pqn, "rsB%d" % nb, "pv2e"], writes=["qnT%d_%d" % (tb, hd)])
                            return [s0, s1_, s1b, s2]
                        units.append(mk())
                        u += 1
                run_pipeline(units)
                P.barrier()
            sX.close()
            onT = sb("onT_all", [128, 4, S], BF16)
            with ExitStack() as s2b:
                sb2 = lambda name, shape, dt: s2b.enter_context(nc.sbuf_tensor(name, shape, dt))
                ps2 = lambda name, shape, dt=F32: s2b.enter_context(nc.psum_tensor(name, shape, dt))
                pt = [sb2("ptB%d" % i, [128, 2, 512], BF16) for i in range(3)]
                rl = [sb2("rlB%d" % i, [128, 512], F32) for i in range(2)]
                psS = [ps2("psSB%d" % i, [128, 2, 512]) for i in range(2)]
                psO = [ps2("psOB%d" % i, [128, 512]) for i in range(2)]
                psL = [ps2("psLB%d" % i, [128, 512]) for i in range(2)]
                units = []
                u = 0
                for tb in range(NB):
                    for hd in range(4):
                        def mk(tb=tb, hd=hd, u=u):
                            t0 = tb * 512
                            b2, b3 = u % 2, u % 3

                            def s0():
                                for kt in range(2):
                                    P.op("tensor", (lambda kt=kt: (lambda h: h.matmul(psS[b2][:, kt, :], knT[:, hd, kt * 128:(kt + 1) * 128], qnT[:, hd, t0:t0 + 512], start=True, stop=True)))(),
                                         reads=["knT", "qnT%d_%d" % (tb, hd)], writes=["psSB%d" % b2], inc=(kt == 1))

                            def s1_():
                                P.op("scalar", lambda h: h.activation(out=pt[b3][:], in_=psS[b2][:], func=AF.Exp), reads=["psSB%d" % b2], writes=["ptB%d" % b3])

                            def s2():
                                for kt in range(2):
                                    P.op("tensor", (lambda kt=kt: (lambda h: h.matmul(psO[b2][:], vmem[:, kt, hd * 128:(hd + 1) * 128], pt[b3][:, kt, :], start=(kt == 0), stop=(kt == 1))))(),
                                         reads=["vmem", "ptB%d" % b3], writes=["psOB%d" % b2], inc=(kt == 1))
                                for kt in range(2):
                                    P.op("tensor", (lambda kt=kt: (lambda h: h.matmul(psL[b2][:], cb[:, CB_ONE:CB_ONE + 128], pt[b3][:, kt, :], start=(kt == 0), stop=(kt == 1))))(),
                                         reads=["cb", "ptB%d" % b3], writes=["psLB%d" % b2], inc=(kt == 1))

                            def s3():
                                P.op("scalar", lambda h: h.activation(out=rl[b2][:], in_=psL[b2][:], func=AF.Ln), reads=["psLB%d" % b2], writes=["rlB%d" % b2])
                                P.op("scalar", lambda h: h.activation(out=rl[b2][:], in_=rl[b2][:], func=AF.Exp, scale=-1.0), reads=["rlB%d" % b2], writes=["rlB%d" % b2])
                                P.op("vector", lambda h: h.tensor_tensor(onT[:, hd, t0:t0 + 512], psO[b2][:], rl[b2][:], op=ALU.mult), reads=["psOB%d" % b2, "rlB%d" % b2], writes=["onT%d_%d" % (tb, hd)])
                            return [s0, s1_, s2, s3]
                        units.append(mk())
                        u += 1
                run_pipeline(units)
                P.barrier()
            with ExitStack() as s3_:
                sb3 = lambda name, shape, dt: s3_.enter_context(nc.sbuf_tensor(name, shape, dt))
                ps3 = lambda name, shape, dt=F32: s3_.enter_context(nc.psum_tensor(name, shape, dt))
                wob = sb3("wob_s", [128, 4, D], BF16)
                x1r = [sb3("x1r%d" % i, [128, D], F32) for i in range(3)]
                x2t = [sb3("x2t%d" % i, [128, D], F32) for i in range(4)]
                xn2 = [sb3("xn2_%d" % i, [128, D], F32) for i in range(2)]
                xn2b = [sb3("xn2b%d" % i, [128, D], BF16) for i in range(4)]
                xn2T = [sb3("xn2T%d" % i, [128, 8, 128], F32) for i in range(2)]
                junk_b3 = sb3("junkB3", [128, D], BF16)
                SM = sb3("smB3", [128, 2, 32], F32)
                LG = sb3("lgB3", [128, 2, 36], F32)
                GOH = sb3("gohB3", [128, 2, 4], F32)
                ESEL = sb3("eselB3", [128, 2, 8], F32)
                MX8 = sb3("mx8B3", [128, 2, 8], F32)
                MK = sb3("mkB3", [128, 2, 8], F32)
                M2T = sb3("m2tB3", [128, 2, 8], F32)
                GEJ = sb3("gejB3", [128, 2, 4], F32)
                A1_ = sb3("A1B3", [128, 2, 32], F32)
                A2_ = sb3("A2B3", [128, 2, 32], F32)
                AA_ = sb3("AAB3", [128, 2, 32], F32)
                POS = sb3("posB3", [128, 2, 32], F32)
                J32 = sb3("j32B3", [128, 2, 32], F32)
                pW_b3 = [[ps3("pWo%d%d" % (a_, b_), [128, 512]) for b_ in range(2)] for a_ in range(2)]
                pX = ps3("pXB", [128, 2, 512])
                bk0 = ps3("bk0", [128, 512])
                bk1 = ps3("bk1", [128, 512])
                P.dma("gpsimd", lambda h: h.dma_start(out=wob[:], in_=self.mem_wo.rearrange("(kc p) n -> p kc n", p=128)), writes=["wob"])

                units = []
                for i in range(NT):
                    def mk(i=i):
                        tb, tt = i // 4, i % 4
                        x3, b2 = i % 3, i % 2
                        ts = slice(tb * 512 + tt * 128, tb * 512 + (tt + 1) * 128)
                        x1n, x2n = "x1r%d" % x3, "x2t%d" % (i % 4)
                        sm, lg, goh, esel, mx8, mk_, m2t, gej = SM[:, b2, :], LG[:, b2, :], GOH[:, b2, :], ESEL[:, b2, :], MX8[:, b2, :], MK[:, b2, :], M2T[:, b2, :], GEJ[:, b2, :]
                        A1, A2, AA, pos, j32 = A1_[:, b2, :], A2_[:, b2, :], AA_[:, b2, :], POS[:, b2, :], J32[:, b2, :]
                        N = lambda n: "%s_%d" % (n, b2)
                        V = lambda fn, reads, writes: P.op("vector", fn, reads=reads, writes=writes)

                        def s0():
                            P.dma("sync", lambda h: h.dma_start(out=x1r[x3][:], in_=self.x1buf[i * 128:(i + 1) * 128, :]), writes=[x1n])

                        def s1_():
                            for half in range(2):
                                hs_ = slice(half * 512, (half + 1) * 512)
                                for hd in range(4):
                                    P.op("tensor", (lambda hd=hd, half=half, hs_=hs_: (lambda h: h.matmul(pW_b3[b2][half][:], onT[:, hd, ts], wob[:, hd, hs_], start=(hd == 0), stop=(hd == 3))))(),
                                         reads=["onT%d_%d" % (tb, hd_) for hd_ in range(4)] + ["wob"], writes=["pWo%d%d" % (b2, half)], inc=(hd == 3))

                        def s2():
                            for half in range(2):
                                hs_ = slice(half * 512, (half + 1) * 512)
                                V((lambda half=half, hs_=hs_: (lambda h: h.tensor_tensor(x2t[i % 4][:, hs_], pW_b3[b2][half][:], x1r[x3][:, hs_], op=ALU.add)))(), ["pWo%d%d" % (b2, half), x1n], [x2n])
                            P.dma("scalar", lambda h: h.dma_start(out=self.x2buf[i * 128:(i + 1) * 128, :], in_=x2t[i % 4][:]), reads=[x2n], writes=["x2buf%d" % i], semres=x2n)
                            if self.debug == "B":
                                P.dma("sync", lambda h: h.dma_start(out=self._o2[i * 128:(i + 1) * 128, :], in_=x2t[i % 4][:]), reads=[x2n], writes=["dbg2"], semres=x2n)
                                P.dma("sync", lambda h: h.dma_start(out=self._o1[i * 128:(i + 1) * 128, :], in_=x1r[x3][:]), reads=[x1n], writes=["dbg1"], semres=x2n)
                            P.op("scalar", lambda h: h.activation(out=junk_b3[:], in_=x2t[i % 4][:], func=AF.Square, accum_out=sm[:, 8:9]), reads=[x2n], writes=["junkB3", N("sm8")])
                            V(lambda h: h.tensor_scalar(sm[:, 9:10], sm[:, 8:9], 1.0 / D, EPS, op0=ALU.mult, op1=ALU.add), [N("sm8")], [N("sm9")])
                            P.op("gpsimd", lambda h: h.tensor_tensor(sm[:, 10:11], sm[:, 9:10], nhalf[:, 0:1], op=ALU.pow), reads=[N("sm9"), "nhalf"], writes=[N("sm10")])
                            V(lambda h: h.scalar_tensor_tensor(out=xn2[b2][:], in0=x2t[i % 4][:], scalar=sm[:, 10:11], in1=gbc[:, 0:D], op0=ALU.mult, op1=ALU.mult), [x2n, N("sm10"), "gbc"], [N("xn2")])
                            P.op("scalar", lambda h: h.activation(out=xn2b[i % 4][:], in_=xn2[b2][:], func=AF.Copy), reads=[N("xn2")], writes=["xn2b%d" % (i % 4)])

                        def s3():
                            pXf = pX[:].rearrange("p a b -> p (a b)")
                            for kc in range(8):
                                P.op("tensor", (lambda kc=kc: (lambda h: h.transpose(pXf[:, kc * 128:(kc + 1) * 128], xn2[b2][:, kc * 128:(kc + 1) * 128], cf[:, CF_ID:CF_ID + 128])))(),
                                     reads=[N("xn2"), "cf"], writes=["pXB"], inc=(kc == 7))
                            P.op("scalar", lambda h: h.activation(out=xn2T[b2][:], in_=pXf.rearrange("p (k t) -> p k t", k=8), func=AF.Copy), reads=["pXB"], writes=[N("xn2T")])

                        def s4():
                            for kc in range(8):
                                P.op("tensor", (lambda kc=kc: (lambda h: h.matmul(bk0[:, 0:36], xn2T[b2][:, kc, :], wr32[:, kc, :], start=(kc == 0), stop=(kc == 7))))(),
                                     reads=[N("xn2T"), "wr32"], writes=["bk0"], inc=(kc == 7))
                            V(lambda h: h.tensor_tensor(lg, bk0[:, 0:36], gbc[:, D:D + 36], op=ALU.add), ["bk0", "gbc"], [N("lg")])
                            yield
                            V(lambda h: h.reduce_max(out=sm[:, 16:17], in_=lg[:, 0:4], axis=AX.X), [N("lg")], [N("sm16")])
                            yield
                            V(lambda h: h.tensor_scalar(goh, lg[:, 0:4], sm[:, 16:17], None, op0=ALU.is_equal), [N("lg"), N("sm16")], [N("goh")])
                            yield
                            V(lambda h: h.tensor_scalar(sm[:, 17:18], sm[:, 16:17], -1.0, None, op0=ALU.mult), [N("sm16")], [N("sm17")])
                            yield
                            P.op("scalar", lambda h: h.activation(out=gej, in_=lg[:, 0:4], func=AF.Exp, bias=sm[:, 17:18], accum_out=sm[:, 18:19]), reads=[N("lg"), N("sm17")], writes=[N("gej"), N("sm18")])
                            yield
                            V(lambda h: h.reciprocal(sm[:, 19:20], sm[:, 18:19]), [N("sm18")], [N("sm19")])
                            yield
                            V(lambda h: h.tensor_tensor(A1.rearrange("p (g e) -> p g e", g=4), lg[:, 4:36].rearrange("p (g e) -> p g e", g=4), goh.unsqueeze(2).to_broadcast([128, 4, 8]), op=ALU.mult), [N("lg"), N("goh")], [N("A1")])
                            yield
                            V(lambda h: h.tensor_reduce(out=esel, in_=A1.rearrange("p (g e) -> p e g", g=4), axis=AX.X, op=ALU.add), [N("A1")], [N("esel")])
                            yield
                            V(lambda h: h.max(out=mx8, in_=esel), [N("esel")], [N("mx8")])
                            yield
                            V(lambda h: h.tensor_scalar(mk_, esel, mx8[:, 0:1], None, op0=ALU.is_equal), [N("esel"), N("mx8")], [N("mk0")])
                            yield
                            V(lambda h: h.tensor_scalar(m2t, esel, mx8[:, 1:2], None, op0=ALU.is_equal), [N("esel"), N("mx8")], [N("mk1")])
                            yield
                            V(lambda h: h.tensor_tensor(sm[:, 20:21], mx8[:, 1:2], mx8[:, 0:1], op=ALU.subtract), [N("mx8")], [N("sm20")])
                            yield
                            P.op("scalar", lambda h: h.activation(out=sm[:, 21:22], in_=sm[:, 20:21], func=AF.Exp), reads=[N("sm20")], writes=[N("sm21")])
                            yield
                            V(lambda h: h.tensor_scalar(sm[:, 22:23], sm[:, 21:22], 1.0, None, op0=ALU.add), [N("sm21")], [N("sm22")])
                            yield
                            V(lambda h: h.reciprocal(sm[:, 23:24], sm[:, 22:23]), [N("sm22")], [N("sm23")])
                            yield
                            V(lambda h: h.tensor_tensor(gates[:, i, 0:1], sm[:, 19:20], sm[:, 23:24], op=ALU.mult), [N("sm19"), N("sm23")], ["gate%d" % i])
                            yield
                            V(lambda h: h.tensor_tensor(gates[:, i, 1:2], gates[:, i, 0:1], sm[:, 21:22], op=ALU.mult), ["gate%d" % i, N("sm21")], ["gate%d" % i])
                            yield
                            V(lambda h: h.tensor_tensor(A1.rearrange("p (g e) -> p g e", g=4), goh.unsqueeze(2).to_broadcast([128, 4, 8]), mk_.unsqueeze(1).to_broadcast([128, 4, 8]), op=ALU.mult), [N("mk0"), N("goh"), N("esel")], [N("A1")])
                            yield
                            V(lambda h: h.tensor_tensor(A2.rearrange("p (g e) -> p g e", g=4), goh.unsqueeze(2).to_broadcast([128, 4, 8]), m2t.unsqueeze(1).to_broadcast([128, 4, 8]), op=ALU.mult), [N("mk1"), N("goh")], [N("A2")])
                            yield
                            V(lambda h: h.tensor_tensor(AA, A1, A2, op=ALU.add), [N("A1"), N("A2")], [N("AA")])
                            yield

                        def s5():
                            P.op("tensor", lambda h: h.matmul(bk1[:, 64:96], cf[:, CF_US:CF_US + 128], AA, start=True, stop=False), reads=["cf", N("AA")], writes=["bk1"], inc=False)
                            yield
                            P.op("tensor", lambda h: h.matmul(bk1[:, 64:96], cf[0:1, CF_ONE:CF_ONE + 128], cntrow[0:1, :], start=False, stop=True), reads=["cf", "cntrow"], writes=["bk1"])
                            yield
                            V(lambda h: h.tensor_tensor(pos, bk1[:, 64:96], cf[:, CF_EB:CF_EB + 32], op=ALU.add), ["bk1", "cf"], [N("pos")])
                            yield
                            P.op("tensor", lambda h: h.matmul(bk1[0:1, 128:160], cf[:, CF_ONE:CF_ONE + 1], AA, start=True, stop=True), reads=["cf", N("AA")], writes=["bk1"])
                            yield
                            V(lambda h: h.tensor_tensor(cntrow[:], cntrow[:], bk1[0:1, 128:160], op=ALU.add), ["bk1", "cntrow"], ["cntrow"])
                            yield
                            V(lambda h: h.scalar_tensor_tensor(out=j32, in0=pos, scalar=1.0, in1=A1, op0=ALU.mult, op1=ALU.mult, accum_out=sm[:, 24:25]), [N("pos"), N("A1")], [N("j32"), N("sm24")])
                            yield
                            V(lambda h: h.scalar_tensor_tensor(out=j32, in0=pos, scalar=1.0, in1=A2, op0=ALU.mult, op1=ALU.mult, accum_out=sm[:, 25:26]), [N("pos"), N("A2"), N("j32")], [N("j32"), N("sm25")])
                            yield
                            V(lambda h: h.tensor_scalar(idxs[:, i, 0:2], sm[:, 24:26], float(NE * CAP - 1), None, op0=ALU.min), [N("sm24"), N("sm25")], ["idx%d" % i])
                            yield
                            for k2 in range(2):
                                P.dma("gpsimd", (lambda k2=k2: (lambda h: h.indirect_dma_start(out=self.xbuf, out_offset=bass.IndirectOffsetOnAxis(ap=idxs[:, i, k2:k2 + 1], axis=0), in_=xn2b[i % 4][:], in_offset=None)))(),
                                      reads=["xn2b%d" % (i % 4), "idx%d" % i, "bg:xz"], writes=["xbuf_s%d_%d" % (i, k2)], semres="xn2b%d" % (i % 4))
                        return [s0, s1_, s2, s3, s4, s5]
                    units.append(mk())
                run_pipeline(units)
                P.op("vector", lambda h: h.tensor_copy(self.cnti[:], cntrow[:]), reads=["cntrow"], writes=["cnti"])
                if self.debug == "B":
                    og = self.dbg_out("gates", [128, NT * 2])
                    oi = self.dbg_out("idxs", [128, NT * 2], I32)
                    P.dma("sync", lambda h: h.dma_start(out=og[:, :], in_=gates[:].rearrange("p a b -> p (a b)")), reads=["gate%d" % i for i in range(NT)], writes=["dbg3"])
                    P.dma("sync", lambda h: h.dma_start(out=oi[:, :], in_=idxs[:].rearrange("p a b -> p (a b)")), reads=["idx%d" % i for i in range(NT)], writes=["dbg4"])
                P.barrier()

    def phaseC(self):
        nc, P = self.nc, self.P
        cb = self.cb
        with ExitStack() as st:
            sb = lambda name, shape, dt: st.enter_context(nc.sbuf_tensor(name, shape, dt))
            ps = lambda name, shape, dt=F32: st.enter_context(nc.psum_tensor(name, shape, dt))
            NW, ND = 3, 4
            wgb = [sb("wgb%d" % i, [128, 8, 512], BF16) for i in range(NW)]
            wub = [sb("wub%d" % i, [128, 8, 512], BF16) for i in range(NW)]
            wdb = [sb("wdb%d" % i, [128, 4, D], BF16) for i in range(ND)]
            xrow = [sb("xrow%d" % i, [128, NSB, D], BF16) for i in range(2)]
            XT = [sb("XT%d" % i, [128, 8, CAP], BF16) for i in range(2)]
            th = [sb("thC%d" % i, [128, CAP], F32) for i in range(2)]
            t1 = [sb("t1C%d" % i, [128, CAP], F32) for i in range(2)]
            HT = [sb("HT%d" % i, [128, 4, CAP], BF16) for i in range(2)]
            yo = [sb("yo%d" % i, [128, D], BF16) for i in range(8)]
            pT = [ps("pTC%d" % i, [128, D], BF16) for i in range(2)]
            psG = [ps("psGC%d" % i, [128, 512]) for i in range(2)]
            psU = [ps("psUC%d" % i, [128, 512]) for i in range(2)]
            psY = [ps("psYC%d" % i, [128, 512]) for i in range(2)]
            preg, cnti = self.preg, self.cnti
            semT = P.sems["E_tensor"]
            Ncell = [CAP]

            def skip_wrap(e, thr):
                def wrap(h, clos, nincs):
                    h.reg_load(preg, cnti[0:1, e:e + 1])
                    with h.If_lt(preg, thr + 1):
                        for c_ in clos:
                            for k_, v_ in getattr(c_, "waits", ()):
                                h.wait_ge(P.sems[k_], v_)
                        h.matmul(psY[0][:, 0:1], cb[:, CB_ID:CB_ID + 128], cb[:, CB_ONE:CB_ONE + 1], start=True, stop=True).then_inc(semT, nincs)
                    with h.Else():
                        for c_ in clos:
                            c_(h)
                return wrap

            def nvar_wrap(e):
                def wrap(h, clos, nincs):
                    h.reg_load(preg, cnti[0:1, e:e + 1])
                    with h.If_lt(preg, 257):
                        Ncell[0] = 256
                        for c_ in clos:
                            c_(h)
                    with h.Else():
                        with h.If_lt(preg, 385):
                            Ncell[0] = 384
                            for c_ in clos:
                                c_(h)
                        with h.Else():
                            Ncell[0] = CAP
                            for c_ in clos:
                                c_(h)
                    Ncell[0] = CAP
                return wrap
            zl = sb("zlC", [128, 128], BF16)
            P.op("vector", lambda h: h.memset(zl[:], 0.0), writes=["zlC"])
            for nm, bank in [("psGC0", psG[0]), ("psGC1", psG[1]), ("psUC0", psU[0]), ("psUC1", psU[1]), ("psYC0", psY[0]), ("psYC1", psY[1])]:
                P.op("tensor", (lambda bank=bank: (lambda h: h.matmul(bank[:], zl[:], cb[:, CB_MASK:CB_MASK + 512], start=True, stop=True)))(), reads=["zlC", "cb"], writes=[nm])
            units = []
            for e in range(NE):
                def mk(e=e):
                    b = e % 2
                    w3 = e % NW
                    w4 = e % ND

                    def s0():
                        P.dma("sync", lambda h: h.dma_start(out=xrow[b][:], in_=self.xbuf[e * CAP:(e + 1) * CAP, :].rearrange("(s p) n -> p s n", p=128)), writes=["xrow%d" % b])
                        P.dma("gpsimd", lambda h: h.dma_start(out=wgb[w3][:], in_=self.wg16[e].rearrange("(kc p) n -> p kc n", p=128)), reads=["bg:w16_%d" % e], writes=["wgb%d" % w3])
                        P.dma("gpsimd", lambda h: h.dma_start(out=wub[w3][:], in_=self.wu16[e].rearrange("(kc p) n -> p kc n", p=128)), reads=["bg:w16_%d" % e], writes=["wub%d" % w3])
                        P.dma("gpsimd", lambda h: h.dma_start(out=wdb[w4][:], in_=self.wd16[e].rearrange("(kc p) n -> p kc n", p=128)), reads=["bg:w16_%d" % e], writes=["wdb%d" % w4])

                    def s1():
                        for sbk in range(NSB):
                            xb = sbk % 2
                            for kc in range(8):
                                P.op("tensor", (lambda kc=kc, sbk=sbk, xb=xb: (lambda h: h.transpose(pT[xb][:, kc * 128:(kc + 1) * 128], xrow[b][:, sbk, kc * 128:(kc + 1) * 128], cb[:, CB_ID:CB_ID + 128])))(),
                                     reads=["xrow%d" % b, "cb"], writes=["pTC%d" % xb], inc=(kc == 7))
                            if sbk % 2 == 0:
                                P.op("scalar", (lambda sbk=sbk, xb=xb: (lambda h: h.activation(out=XT[b][:, :, sbk * 128:(sbk + 1) * 128], in_=pT[xb][:].rearrange("p (k t) -> p k t", k=8), func=AF.Copy)))(),
                                     reads=["pTC%d" % xb], writes=["XT%d_%d" % (b, sbk)])
                            else:
                                P.op("vector", (lambda sbk=sbk, xb=xb: (lambda h: h.tensor_copy(XT[b][:, :, sbk * 128:(sbk + 1) * 128], pT[xb][:].rearrange("p (k t) -> p k t", k=8))))(),
                                     reads=["pTC%d" % xb], writes=["XT%d_%d" % (b, sbk)])

                    def s2():
                        xres = ["XT%d_%d" % (b, k) for k in range(NSB)]
                        P.begin_region("tensor")
                        for mt in range(4):
                            mb = mt % 2
                            for kc in range(8):
                                P.op("tensor", (lambda kc=kc, mt=mt, mb=mb: (lambda h: h.matmul(psG[mb][:, 0:Ncell[0]], wgb[w3][:, kc, mt * 128:(mt + 1) * 128], XT[b][:, kc, 0:Ncell[0]], start=(kc == 0), stop=(kc == 7))))(),
                                     reads=["wgb%d" % w3, "cnti"] + xres, writes=["psGC%d" % mb], inc=(kc == 7))
                            for kc in range(8):
                                P.op("tensor", (lambda kc=kc, mt=mt: (lambda h: h.matmul(psU[mt % 2][:, 0:Ncell[0]], wub[w3][:, kc, mt * 128:(mt + 1) * 128], XT[b][:, kc, 0:Ncell[0]], start=(kc == 0), stop=(kc == 7))))(),
                                     reads=["wub%d" % w3, "cnti"] + xres, writes=["psUC%d" % (mt % 2)], inc=(kc == 7))
                            P.op("scalar", (lambda mb=mb: (lambda h: h.activation(out=th[mb][:], in_=psG[mb][:, 0:CAP], func=AF.Tanh, scale=0.5)))(), reads=["psGC%d" % mb], writes=["thC%d" % mb])
                            P.op("vector", (lambda mb=mb: (lambda h: h.scalar_tensor_tensor(out=t1[mb][:], in0=th[mb][:], scalar=1.0, in1=psG[mb][:, 0:CAP], op0=ALU.add, op1=ALU.mult)))(), reads=["thC%d" % mb, "psGC%d" % mb], writes=["t1C%d" % mb])
                            P.op("vector", (lambda mb=mb, mt=mt: (lambda h: h.scalar_tensor_tensor(out=HT[b][:, mt, :], in0=t1[mb][:], scalar=0.5, in1=psU[mb][:, 0:CAP], op0=ALU.mult, op1=ALU.mult)))(), reads=["t1C%d" % mb, "psUC%d" % mb], writes=["HT%d_%d" % (b, mt)])
                        P.end_region(nvar_wrap(e))

                    def s3():
                        hres = ["HT%d_%d" % (b, k) for k in range(4)]
                        for sbk in range(NSB):
                            ob = (e * NSB + sbk) % 8
                            r0 = e * CAP + sbk * 128
                            if sbk >= 2:
                                P.begin_region("tensor")
                            for half in range(2):
                                for mt in range(4):
                                    P.op("tensor", (lambda mt=mt, sbk=sbk, half=half: (lambda h: h.matmul(psY[half][:], HT[b][:, mt, sbk * 128:(sbk + 1) * 128], wdb[w4][:, mt, half * 512:(half + 1) * 512], start=(mt == 0), stop=(mt == 3))))(),
                                         reads=hres + ["wdb%d" % w4, "cnti"], writes=["psYC%d" % half], inc=(mt == 3))
                            if sbk >= 2:
                                P.end_region(skip_wrap(e, 128 * sbk))
                            for half in range(2):
                                if half == 0:
                                    P.op("scalar", (lambda ob=ob: (lambda h: h.activation(out=yo[ob][:, 0:512], in_=psY[0][:], func=AF.Copy)))(), reads=["psYC0"], writes=["yo%d" % ob])
                                else:
                                    P.op("vector", (lambda ob=ob: (lambda h: h.tensor_copy(yo[ob][:, 512:1024], psY[1][:])))(), reads=["psYC1"], writes=["yo%d" % ob])
                            P.dma("scalar", (lambda ob=ob, r0=r0: (lambda h: h.dma_start(out=self.ybuf[r0:r0 + 128, :], in_=yo[ob][:])))(), reads=["yo%d" % ob], writes=["ybuf%d" % (r0 // 128)], semres="yo%d" % ob)
                    return [s0, s1, s2, s3]
                units.append(mk())
            run_pipeline(units)
            self.phaseD(st)

    def phaseD(self, st):
        nc, P = self.nc, self.P
        gates, idxs = self.gates, self.idxs
        if True:
            sb = lambda name, shape, dt: st.enter_context(nc.sbuf_tensor(name, shape, dt))
            yall = ["ybuf%d" % k for k in range(NE * NSB)]
            y1 = [sb("y1D%d" % i, [128, D], BF16) for i in range(3)]
            y2 = [sb("y2D%d" % i, [128, D], BF16) for i in range(3)]
            x2 = [sb("x2D%d" % i, [128, D], F32) for i in range(3)]
            oo = [sb("ooD%d" % i, [128, D], F32) for i in range(3)]
            for i in range(NT):
                b = i % 3
                P.dma("gpsimd", (lambda i=i, b=b: (lambda h: h.indirect_dma_start(out=y1[b][:], out_offset=None, in_=self.ybuf, in_offset=bass.IndirectOffsetOnAxis(ap=idxs[:, i, 0:1], axis=0))))(), reads=yall, writes=["y1D%d" % b])
                P.dma("gpsimd", (lambda i=i, b=b: (lambda h: h.indirect_dma_start(out=y2[b][:], out_offset=None, in_=self.ybuf, in_offset=bass.IndirectOffsetOnAxis(ap=idxs[:, i, 1:2], axis=0))))(), reads=yall, writes=["y2D%d" % b])
                P.dma("sync", (lambda i=i, b=b: (lambda h: h.dma_start(out=x2[b][:], in_=self.x2buf[i * 128:(i + 1) * 128, :])))(), writes=["x2D%d" % b])
                P.op("vector", (lambda i=i, b=b: (lambda h: h.scalar_tensor_tensor(out=oo[b][:], in0=y1[b][:], scalar=gates[:, i, 0:1], in1=x2[b][:], op0=ALU.mult, op1=ALU.add)))(), reads=["y1D%d" % b, "x2D%d" % b], writes=["ooD%d" % b])
                P.op("vector", (lambda i=i, b=b: (lambda h: h.scalar_tensor_tensor(out=oo[b][:], in0=y2[b][:], scalar=gates[:, i, 1:2], in1=oo[b][:], op0=ALU.mult, op1=ALU.add)))(), reads=["y2D%d" % b, "ooD%d" % b], writes=["ooD%d" % b])
                P.dma("scalar", (lambda i=i, b=b: (lambda h: h.dma_start(out=self.out[i * 128:(i + 1) * 128, :], in_=oo[b][:])))(), reads=["ooD%d" % b], writes=["out%d" % i], semres="ooD%d" % b)


def host_shared(inputs):
    f = lambda k: np.ascontiguousarray(np.asarray(inputs[k], dtype=np.float32)[0])
    pv = np.zeros((128, NPV), np.float32)
    t8 = lambda v: v.reshape(8, 128).T
    pv[:, PV["gmix"]:PV["gmix"] + 8] = t8(f("norm_mix_g"))
    pv[:, PV["gout"]:PV["gout"] + 8] = t8(np.concatenate([f("lru_out_g"), f("fox_out_g")]))
    pv[:, PV["gmemx"]:PV["gmemx"] + 8] = t8(f("norm_mem_x_g"))
    pv[:, PV["gmem"]:PV["gmem"] + 8] = t8(f("norm_mem_g"))
    pv[:, PV["convw"]:PV["convw"] + 16] = f("conv_w").reshape(4, 4, 128).transpose(2, 1, 0).reshape(128, 16)
    for n, k in [("convb", "conv_b"), ("ba", "lru_ba"), ("bx", "lru_bx"), ("apar", "lru_a_param")]:
        pv[:, PV[n]:PV[n] + 4] = f(k).reshape(4, 128).T
    pv[:, PV["gq2"]] = np.tile(f("fox_q_g"), 2)
    pv[:, PV["gk2"]] = np.tile(f("fox_k_g"), 2)
    pv[:, PV["mqg"]] = f("mem_q_g")
    pv[:, PV["mkg"]] = f("mem_k_g")
    pv[0:8, PV["bf"]] = f("b_forget")
    pv[0:64, PV["gfx"]:PV["gfx"] + 8] = f("fox_out_g").reshape(8, 64).T
    bvec = np.concatenate([f("norm_ffn_g"), f("router_group_b"), f("router_expert_b"), f("norm_mix_g"), f("norm_mem_x_g"), f("norm_mem_g")])[None, :]
    wr = np.concatenate([f("router_group_w"), f("router_expert_w")], axis=1)
    wa, wx = f("lru_wa"), f("lru_wx")
    lruw = np.zeros((128, 4, 2, 128), np.float32)
    for ct in range(4):
        for hb in range(2):
            lruw[hb * 64:(hb + 1) * 64, ct, 0, hb * 64:(hb + 1) * 64] = wa[2 * ct + hb]
            lruw[hb * 64:(hb + 1) * 64, ct, 1, hb * 64:(hb + 1) * 64] = wx[2 * ct + hb]
    cbf = np.zeros((128, NCB), np.float32)
    cbf[:, CB_ID:CB_ID + 128] = np.eye(128)
    cbf[0:64, CB_BO:CB_BO + 64] = 1.0
    cbf[64:128, CB_BO + 64:CB_BO + 128] = 1.0
    cbf[:, CB_ONE:CB_ONE + 128] = 1.0
    sp = np.arange(128)[:, None]
    tq = np.arange(512)[None, :]
    for m in range(4):
        cbf[:, CB_MASK + m * 512:CB_MASK + (m + 1) * 512] = np.where(m * 128 + sp > tq, -30000.0, 0.0)
    cf = np.zeros((128, NCF), np.float32)
    cf[:, CF_ID:CF_ID + 128] = np.eye(128)
    cf[:, CF_US:CF_US + 128] = (np.arange(128)[:, None] < np.arange(128)[None, :]).astype(np.float32)
    cf[:, CF_ONE:CF_ONE + 128] = 1.0
    cf[:, CF_EB:CF_EB + 32] = (np.arange(32) * CAP)[None, :]
    return {
        "w_in": f("w_in"), "w_out": f("w_out"), "mem_wq": f("mem_wq"), "mem_wkv": f("mem_wkv"), "mem_wo": f("mem_wo"),
        "wr": np.ascontiguousarray(wr), "wgate": f("exp_w_gate"), "wup": f("exp_w_up"), "wdown": f("exp_w_down"),
        "lruw": lruw.reshape(128, 1024), "pvec": pv, "bvec": np.ascontiguousarray(bvec),
        "cbf": cbf.astype(ml_dtypes.bfloat16), "cf32": cf,
    }


def kernel(**inputs):
    nc = Builder().build()
    shared = host_shared(inputs)
    x = np.asarray(inputs["x"], dtype=np.float32)
    mem = np.asarray(inputs["mem"], dtype=np.float32)
    in_maps = []
    for c in range(8):
        m = dict(shared)
        m["x"] = np.ascontiguousarray(x[c])
        m["mem"] = np.ascontiguousarray(mem[c])
        in_maps.append(m)
    res = run_bass_kernel_spmd(nc, in_maps, core_ids=list(range(8)))
    return np.stack([np.asarray(r["out"], dtype=np.float32) for r in res.results], axis=0)
```

```python
from contextlib import ExitStack
import numpy as np
import ml_dtypes
import concourse.bass as bass
import concourse.mybir as mybir
from concourse.bass_utils import run_bass_kernel_spmd

F32 = mybir.dt.float32
BF16 = mybir.dt.bfloat16
I32 = mybir.dt.int32
AF = mybir.ActivationFunctionType
ALU = mybir.AluOpType
AX = mybir.AxisListType

ENGS = ["tensor", "vector", "scalar", "gpsimd", "sync"]

S = 4096
D = 1024
NT = 32
NB = 8
NE = 32
CAP = 512
NSB = CAP // 128
EPS = 1e-6
IN_COLS = 2568


class Res:
    __slots__ = ("name", "w", "r", "dsem")

    def __init__(self, name):
        self.name = name
        self.w = None
        self.r = []
        self.dsem = {}


class Prog:
    def __init__(self, nc, stack):
        self.nc = nc
        self.stack = stack
        self.q = {e: [] for e in ENGS}
        self.sems = {}
        self.cnt = {}
        self.waited = {e: {} for e in ENGS}
        self.res = {}
        self.nsem = 0
        self.free_dsems = {"sw": [], "hw": []}
        self.bgsem = {}
        self.bgkeys = set()
        for e in ENGS:
            self._mksem("E_" + e)

    def _mksem(self, key):
        s = self.stack.enter_context(self.nc.semaphore("s%d" % self.nsem))
        self.nsem += 1
        self.sems[key] = s
        self.cnt[key] = 0
        return s

    def R(self, name):
        r = self.res.get(name)
        if r is None:
            r = Res(name)
            self.res[name] = r
        return r

    def _deps(self, eng, reads, writes):
        need = {}

        def add(ev):
            if ev is None:
                return
            k, v = ev
            if need.get(k, 0) < v:
                need[k] = v
        for r in reads:
            if r.startswith("bg:"):
                k_ = self.bgsem[r[3:]]
                add((k_, self.cnt[k_]))
            else:
                add(self.R(r).w)
        for w in writes:
            rw = self.R(w)
            add(rw.w)
            for ev in rw.r:
                add(ev)
        out = []
        own = "E_" + eng
        for k, v in need.items():
            if k == own and v > self.cnt[own]:
                continue
            if self.waited[eng].get(k, 0) >= v:
                continue
            self.waited[eng][k] = v
            out.append((k, v))
        return out

    def _commit(self, ev, reads, writes):
        for w in writes:
            rw = self.R(w)
            rw.w = ev
            rw.r = []
        for r in reads:
            if r in writes or r.startswith("bg:"):
                continue
            rr = self.R(r)
            rr.r = [e for e in rr.r if e[0] != ev[0]]
            rr.r.append(ev)

    def op(self, eng, fn, reads=(), writes=(), inc=True):
        reads = list(reads)
        writes = list(writes)
        waits = self._deps(eng, reads, writes)
        key = "E_" + eng
        if inc:
            self.cnt[key] += 1
            ev = (key, self.cnt[key])
        else:
            ev = (key, self.cnt[key] + 1)
        sems = self.sems
        sem = sems[key]

        def run(h, waits=waits, fn=fn, inc=inc, sem=sem):
            for k, v in waits:
                h.wait_ge(sems[k], v)
            ins = fn(h)
            if inc:
                ins.then_inc(sem, 1)
        run.waits = waits
        self.q[eng].append(run)
        self._commit(ev, reads, writes)
        return ev

    def dma(self, eng, fn, reads=(), writes=(), semres=None):
        reads = list(reads)
        writes = list(writes)
        waits = self._deps(eng, reads, writes)
        if semres is None:
            semres = writes[0] if writes else reads[0]
        rr = self.R(semres)
        cls = "sw" if eng == "gpsimd" else "hw"
        if cls not in rr.dsem:
            if self.free_dsems[cls]:
                rr.dsem[cls] = self.free_dsems[cls].pop()
            else:
                rr.dsem[cls] = "D%s%d" % (cls, self.nsem)
                self._mksem(rr.dsem[cls])
        key = rr.dsem[cls]
        self.cnt[key] += 16
        ev = (key, self.cnt[key])
        sems = self.sems
        sem = sems[key]

        def run(h, waits=waits, fn=fn, sem=sem):
            for k, v in waits:
                h.wait_ge(sems[k], v)
            fn(h).then_inc(sem, 16)
        self.q[eng].append(run)
        self._commit(ev, reads, writes)
        return ev

    def dma_bg(self, eng, fn, name, reads=()):
        key = self.bgsem.get(name)
        if key is None:
            key = "B_" + name
            self._mksem(key)
            self.bgsem[name] = key
            self.bgkeys.add(key)
        waits = self._deps(eng, list(reads), [])
        self.cnt[key] += 16
        sem = self.sems[key]
        sems = self.sems

        def run(h, fn=fn, sem=sem, waits=waits):
            for k, v in waits:
                h.wait_ge(sems[k], v)
            fn(h).then_inc(sem, 16)
        self.q[eng].append(run)

    def begin_region(self, eng):
        self._rg = (eng, len(self.q[eng]), dict(self.waited[eng]), self.cnt["E_" + eng])

    def end_region(self, wrap):
        eng, start, waited, c0 = self._rg
        clos = self.q[eng][start:]
        del self.q[eng][start:]
        nincs = self.cnt["E_" + eng] - c0
        self.q[eng].append(lambda h, clos=clos, nincs=nincs: wrap(h, clos, nincs))
        self.waited[eng] = waited

    def barrier(self):
        for e in ENGS:
            waits = []
            for k, v in self.cnt.items():
                if v == 0 or k in self.bgkeys or self.waited[e].get(k, 0) >= v:
                    continue
                self.waited[e][k] = v
                waits.append((k, v))
            sems = self.sems

            def run(h, waits=waits):
                for k, v in waits:
                    h.wait_ge(sems[k], v)
            self.q[e].append(run)
        for r in self.res.values():
            for cls, k in r.dsem.items():
                self.free_dsems[cls].append(k)
        for cls in self.free_dsems:
            self.free_dsems[cls] = sorted(set(self.free_dsems[cls]))
        self.res = {}

    def emit(self):
        nc = self.nc
        with nc.Block() as block:
            for e in ENGS:
                lst = self.q[e]
                if not lst:
                    continue

                def body(h, lst=lst):
                    for f in lst:
                        f(h)
                getattr(block, e)(body)


def run_pipeline(units):
    n = len(units)
    ns = max(len(u) for u in units)
    for t in range(n + ns - 1):
        gens = []

        def flush():
            while gens:
                for g_ in list(gens):
                    try:
                        next(g_)
                    except StopIteration:
                        gens.remove(g_)
        for s_ in reversed(range(ns)):
            u = t - s_
            if 0 <= u < n and s_ < len(units[u]):
                r = units[u][s_]()
                if r is not None:
                    gens.append(r)
                else:
                    pass
            if gens and (s_ == 0 or not _is_gen_next(units, t, s_ - 1, n)):
                flush()
        flush()


def _is_gen_next(units, t, s_, n):
    u = t - s_
    if not (0 <= u < n and s_ < len(units[u])):
        return True
    import inspect
    return inspect.isgeneratorfunction(units[u][s_])


PV = {}
_c = 0
for _n, _w in [("gmix", 8), ("gout", 8), ("gmemx", 8), ("gmem", 8), ("convw", 16), ("convb", 4), ("ba", 4),
               ("bx", 4), ("apar", 4), ("gq2", 1), ("gk2", 1), ("mqg", 1), ("mkg", 1), ("bf", 1), ("gfx", 8)]:
    PV[_n] = _c
    _c += _w
NPV = _c
CB_ID, CB_BO, CB_ONE, CB_MASK = 0, 128, 256, 384
NCB = 384 + 2048
CF_ID, CF_US, CF_ONE, CF_EB = 0, 128, 256, 384
NCF = 384 + 32
NBV = 1024 + 36 + 3 * 1024
BV_GMIX, BV_GMEMX, BV_GMEM = 1060, 2084, 3108


class Builder:
    def __init__(self, debug=None):
        self.debug = debug
        self.nc = bass.Bass("TRN2", target_bir_lowering=False)
        nc = self.nc
        di = lambda n, s, d=F32: nc.dram_tensor(n, s, d, kind="ExternalInput").ap()
        self.x = di("x", [S, D])
        self.mem = di("mem", [256, D])
        self.w_in = di("w_in", [D, IN_COLS])
        self.w_out = di("w_out", [D, D])
        self.mem_wq = di("mem_wq", [D, 512])
        self.mem_wkv = di("mem_wkv", [D, 1024])
        self.mem_wo = di("mem_wo", [512, D])
        self.wr = di("wr", [D, 36])
        self.wgate = di("wgate", [NE, D, 512])
        self.wup = di("wup", [NE, D, 512])
        self.wdown = di("wdown", [NE, 512, D])
        self.lruw = di("lruw", [128, 4 * 2 * 128])
        self.pvec = di("pvec", [128, NPV])
        self.bvec = di("bvec", [1, NBV])
        self.cbf = di("cbf", [128, NCB], BF16)
        self.cf32 = di("cf32", [128, NCF])
        self.out = nc.dram_tensor("out", [S, D], F32, kind="ExternalOutput").ap()
        dt = lambda n, s, d: nc.dram_tensor(n, s, d).ap()
        self.ymix = dt("ymix", [D, S], BF16)
        self.cscr = dt("cscr", [8, 12, S], BF16)
        self.x2buf = dt("x2buf", [S, D], F32)
        self.xbuf = dt("xbuf", [NE * CAP, D], BF16)
        self.ybuf = dt("ybuf", [NE * CAP, D], BF16)
        self.wg16 = dt("wg16", [NE, D, 512], BF16)
        self.wu16 = dt("wu16", [NE, D, 512], BF16)
        self.wd16 = dt("wd16", [NE, 512, D], BF16)
        if debug:
            self.dbg = {}

    def dbg_out(self, name, shape, dtype=F32):
        t = self.nc.dram_tensor("dbg_" + name, shape, dtype, kind="ExternalOutput").ap()
        self.dbg[name] = t
        return t

    def build(self, upto=99):
        nc = self.nc
        with ExitStack() as top:
            P = Prog(nc, top)
            self.P = P
            sbt = lambda st, name, shape, dt: st.enter_context(nc.sbuf_tensor(name, shape, dt))
            pst = lambda st, name, shape, dt=F32: st.enter_context(nc.psum_tensor(name, shape, dt))
            pv = sbt(top, "pv", [128, NPV], F32)
            pv2 = sbt(top, "pv2", [128, 24], F32)
            cb = sbt(top, "cb", [128, NCB], BF16)
            cf = sbt(top, "cf", [128, NCF], F32)
            nhalf = sbt(top, "nhalf", [128, 512], F32)
            phalf = sbt(top, "phalf", [128, 512], F32)
            self.pv, self.pv2, self.cb, self.cf, self.nhalf, self.phalf = pv, pv2, cb, cf, nhalf, phalf
            P.dma("sync", lambda h: h.dma_start(out=pv[:], in_=self.pvec[:, :]), writes=["pv"])
            P.dma("sync", lambda h: h.dma_start(out=cb[:], in_=self.cbf[:, :]), writes=["cb"])
            P.dma("sync", lambda h: h.dma_start(out=cf[:], in_=self.cf32[:, :]), writes=["cf"])
            self.epsc = sbt(top, "epsc", [128, 1], F32)
            P.op("gpsimd", lambda h: h.memset(self.epsc[:], EPS), writes=["epsc"])
            P.op("gpsimd", lambda h: h.memset(nhalf[:], -0.5), writes=["nhalf"])
            zt = sbt(top, "zt", [128, 2, D], BF16)
            P.op("gpsimd", lambda h: h.memset(zt[:], 0.0), writes=["zt"])
            self.zt = zt
            P.op("gpsimd", lambda h: h.memset(phalf[:], 0.5), writes=["phalf"])
            c = PV
            P.op("vector", lambda h: h.tensor_scalar(pv2[:, 0:4], pv[:, c["ba"]:c["ba"] + 4], 0.5, None, op0=ALU.mult), reads=["pv"], writes=["pv2a"])
            P.op("vector", lambda h: h.tensor_scalar(pv2[:, 4:8], pv[:, c["bx"]:c["bx"] + 4], 0.5, None, op0=ALU.mult), reads=["pv"], writes=["pv2b"])
            P.op("scalar", lambda h: h.activation(out=pv2[:, 8:12], in_=pv[:, c["apar"]:c["apar"] + 4], func=AF.Exp, scale=-1.0), reads=["pv"], writes=["pv2c"])
            P.op("scalar", lambda h: h.activation(out=pv2[:, 8:12], in_=pv2[:, 8:12], func=AF.Ln, bias=1.0), reads=["pv2c"], writes=["pv2c"])
            P.op("vector", lambda h: h.tensor_scalar(pv2[:, 8:12], pv2[:, 8:12], -4.0, None, op0=ALU.mult), reads=["pv2c"], writes=["pv2c"])
            P.op("vector", lambda h: h.tensor_scalar(pv2[:, 12:13], pv[:, c["gq2"]:c["gq2"] + 1], 0.125, None, op0=ALU.mult), reads=["pv"], writes=["pv2d"])
            P.op("vector", lambda h: h.tensor_scalar(pv2[:, 13:14], pv[:, c["mqg"]:c["mqg"] + 1], 128.0 ** -0.5, None, op0=ALU.mult), reads=["pv"], writes=["pv2e"])
            P.op("vector", lambda h: h.tensor_scalar(pv2[:, 14:15], pv[:, c["bf"]:c["bf"] + 1], -1.0, None, op0=ALU.mult), reads=["pv"], writes=["pv2f"])
            self.PVR = ["pv", "pv2a", "pv2b", "pv2c", "pv2d", "pv2e", "pv2f", "cb", "cf", "nhalf", "phalf"]

            self.knT = sbt(top, "knT", [128, 4, 256], BF16)
            self.vmem = sbt(top, "vmem", [128, 2, 512], BF16)
            self.cnti = sbt(top, "cnti", [1, 32], I32)
            self.preg = top.enter_context(nc.tensor.register("cntreg"))
            self.gates = sbt(top, "gates", [128, NT, 2], F32)
            self.idxs = sbt(top, "idxs", [128, NT, 2], I32)
            self.gsq = sbt(top, "gsq", [128, 8], BF16)
            gtmp = sbt(top, "gtmp", [128, 8], F32)
            P.op("vector", lambda h: h.tensor_scalar(pv2[:, 16:20], pv[:, c["gout"]:c["gout"] + 4], 0.5, None, op0=ALU.mult), reads=["pv"], writes=["pv2g"])
            P.op("vector", lambda h: h.tensor_tensor(gtmp[:], pv[:, c["gout"]:c["gout"] + 8], pv[:, c["gout"]:c["gout"] + 8], op=ALU.mult), reads=["pv"], writes=["gtmp"])
            P.op("vector", lambda h: h.reciprocal(gtmp[:], gtmp[:]), reads=["gtmp"], writes=["gtmp"])
            P.op("vector", lambda h: h.tensor_copy(self.gsq[:], gtmp[:]), reads=["gtmp"], writes=["gsq"])
            P.barrier()
            if upto >= 1:
                with ExitStack() as stA:
                    hT = sbt(stA, "hT", [128, 8, S], BF16)
                    self.hT = hT
                    self.phaseA1(stA)
                    P.barrier()
                    if upto >= 2:
                        self.phaseA2()
                        P.barrier()
                    if upto >= 3:
                        self.phaseA3()
                        P.barrier()
            if upto >= 4:
                self.phaseB()
                P.barrier()
            if upto >= 5:
                self.phaseC()
            P.barrier()
            if self.debug in ("A2", "A3"):
                o = self.dbg_out("ymix", [D, S], BF16)
                for r_ in range(8):
                    P.dma("sync", (lambda r_=r_: (lambda h: h.dma_start(out=o[r_ * 128:(r_ + 1) * 128, :], in_=self.ymix[r_ * 128:(r_ + 1) * 128, :])))(), writes=["dbg%d" % r_])
                P.barrier()
            P.emit()
        return nc

    def precast(self, e0, e1):
        P = self.P
        for e in range(e0, e1):
            P.dma_bg("gpsimd", (lambda e=e: (lambda h: h.dma_start(out=self.wg16[e], in_=self.wgate[e])))(), "w16_%d" % e)
            P.dma_bg("gpsimd", (lambda e=e: (lambda h: h.dma_start(out=self.wu16[e], in_=self.wup[e])))(), "w16_%d" % e)
            P.dma_bg("gpsimd", (lambda e=e: (lambda h: h.dma_start(out=self.wd16[e], in_=self.wdown[e])))(), "w16_%d" % e)

    def emit_kv(self, st2):
        nc, P = self.nc, self.P
        cb, pv, nhalf, knT, vmem = self.cb, self.pv, self.nhalf, self.knT, self.vmem
        sb2 = lambda name, shape, dt: st2.enter_context(nc.sbuf_tensor(name, shape, dt))
        pT_kv = st2.enter_context(nc.psum_tensor("pTB", [128, D], BF16))
        psQ_kv = st2.enter_context(nc.psum_tensor("psQB", [128, 512], F32))
        psN_kv = st2.enter_context(nc.psum_tensor("psNB", [128, 512], F32))
        wkvb = sb2("wkvb_s", [128, 8, D], BF16)
        mt_ = [sb2("memt%d" % i, [128, D], F32) for i in range(2)]
        mb_ = [sb2("memb%d" % i, [128, D], BF16) for i in range(2)]
        memT = sb2("memT", [128, 8, 256], BF16)
        junk_kv = sb2("junkM", [128, D], BF16)
        mstat = sb2("mstat", [128, 6], F32)
        sqm = sb2("sqm", [128, 256], BF16)
        vrm = sb2("vrm", [128, 256], F32)
        rsm = sb2("rsm", [128, 256], F32)
        P.dma("gpsimd", lambda h: h.dma_start(out=wkvb[:], in_=self.mem_wkv.rearrange("(kc p) n -> p kc n", p=128)), writes=["wkvb"])
        gmb = sb2("gmb", [128, D], F32)
        P.dma("sync", lambda h: h.dma_start(out=gmb[:], in_=self.bvec[:, BV_GMEM:BV_GMEM + D].partition_broadcast(128)), writes=["gmb"])
        for i in range(2):
            P.dma("sync", (lambda i=i: (lambda h: h.dma_start(out=mt_[i][:], in_=self.mem[i * 128:(i + 1) * 128, :])))(), writes=["memt%d" % i])
            P.op("scalar", (lambda i=i: (lambda h: h.activation(out=junk_kv[:], in_=mt_[i][:], func=AF.Square, accum_out=mstat[:, i:i + 1])))(), reads=["memt%d" % i], writes=["junkM", "ms%d" % i])
            P.op("vector", (lambda i=i: (lambda h: h.tensor_scalar(mstat[:, 2 + i:3 + i], mstat[:, i:i + 1], 1.0 / D, EPS, op0=ALU.mult, op1=ALU.add)))(), reads=["ms%d" % i], writes=["mv%d" % i])
            P.op("gpsimd", (lambda i=i: (lambda h: h.tensor_tensor(mstat[:, 4 + i:5 + i], mstat[:, 2 + i:3 + i], nhalf[:, 0:1], op=ALU.pow)))(), reads=["mv%d" % i, "nhalf"], writes=["mr%d" % i])
            P.op("vector", (lambda i=i: (lambda h: h.scalar_tensor_tensor(out=mb_[i][:], in0=mt_[i][:], scalar=mstat[:, 4 + i:5 + i], in1=gmb[:], op0=ALU.mult, op1=ALU.mult)))(), reads=["memt%d" % i, "mr%d" % i, "gmb"], writes=["memb%d" % i])
            for kc in range(8):
                P.op("tensor", (lambda kc=kc, i=i: (lambda h: h.transpose(pT_kv[:, kc * 128:(kc + 1) * 128], mb_[i][:, kc * 128:(kc + 1) * 128], cb[:, CB_ID:CB_ID + 128])))(),
                     reads=["memb%d" % i, "cb"], writes=["pTB"], inc=(kc == 7))
            P.op("vector", (lambda i=i: (lambda h: h.tensor_copy(memT[:, :, i * 128:(i + 1) * 128], pT_kv[:].rearrange("p (k t) -> p k t", k=8))))(), reads=["pTB"], writes=["memT%d" % i])
        mres = ["memT0", "memT1"]
        for hd in range(4):
            for kc in range(8):
                P.op("tensor", (lambda kc=kc, hd=hd: (lambda h: h.matmul(psQ_kv[:, 0:256], wkvb[:, kc, hd * 128:(hd + 1) * 128], memT[:, kc, :], start=(kc == 0), stop=(kc == 7))))(),
                     reads=["wkvb"] + mres, writes=["psQB"], inc=(kc == 7))
            P.op("scalar", lambda h: h.activation(out=sqm[:], in_=psQ_kv[:, 0:256], func=AF.Square), reads=["psQB"], writes=["sqm"])
            P.op("tensor", lambda h: h.matmul(psN_kv[:, 0:256], cb[:, CB_ONE:CB_ONE + 128], sqm[:], start=True, stop=True), reads=["cb", "sqm"], writes=["psNB"])
            P.op("scalar", lambda h: h.activation(out=vrm[:], in_=psN_kv[:, 0:256], func=AF.Sqrt, scale=1.0 / 128, bias=self.epsc[:, 0:1]), reads=["psNB", "epsc"], writes=["vrm"])
            P.op("vector", lambda h: h.reciprocal(rsm[:], vrm[:]), reads=["vrm"], writes=["rsm"])
            P.op("vector", (lambda hd=hd: (lambda h: h.scalar_tensor_tensor(out=knT[:, hd, :], in0=psQ_kv[:, 0:256], scalar=pv[:, PV["mkg"]:PV["mkg"] + 1], in1=rsm[:], op0=ALU.mult, op1=ALU.mult)))(),
                 reads=["psQB", "rsm", "pv"], writes=["knT"])
        for kt in range(2):
            for kc in range(8):
                P.op("tensor", (lambda kc=kc, kt=kt: (lambda h: h.matmul(psQ_kv[:], memT[:, kc, kt * 128:(kt + 1) * 128], wkvb[:, kc, 512:1024], start=(kc == 0), stop=(kc == 7))))(),
                     reads=["wkvb"] + mres, writes=["psQB"], inc=(kc == 7))
            P.op("vector", (lambda kt=kt: (lambda h: h.tensor_copy(vmem[:, kt, :], psQ_kv[:])))(), reads=["psQB"], writes=["vmem"])


    def emit_forget(self, st2):
        nc, P = self.nc, self.P
        hT, pv2 = self.hT, self.pv2
        winv = self.w_in.rearrange("(kc p) n -> p kc n", p=128)
        wf = st2.enter_context(nc.sbuf_tensor("wf", [128, 8, 8], BF16))
        psQ = st2.enter_context(nc.psum_tensor("psQf", [128, 512], F32))
        P.dma("gpsimd", lambda h: h.dma_start(out=wf[:], in_=winv[:, :, 2560:2568]), writes=["wf"])
        sb2 = lambda name, shape, dt: st2.enter_context(nc.sbuf_tensor(name, shape, dt))
        HW = 2048
        fe = sb2("fe", [8, HW], F32)
        fm0 = sb2("fm0", [8, HW], F32)
        fm = [fm0, fm0]
        fcar = sb2("fcar", [8, 1], F32)
        fr = fe
        pcs = [sb2("pc%d" % i, [8, HW], BF16) for i in range(7)]
        P.op("gpsimd", lambda h: h.memset(pcs[6][:], 1.0), writes=["pc6"])

        def block(tb):
            if True:
                tq = tb % 4
                t0 = tb * 512
                for kc in range(8):
                    P.op("tensor", (lambda kc=kc, t0=t0: (lambda h: h.matmul(psQ[0:8, :], wf[:, kc, :], hT[:, kc, t0:t0 + 512], start=(kc == 0), stop=(kc == 7))))(),
                         reads=["wf"] + ["hT%d" % (tb * 4 + j) for j in range(4)], writes=["psQf"], inc=(kc == 7))
                P.op("scalar", (lambda tq=tq: (lambda h: h.activation(out=fe[:, tq * 512:(tq + 1) * 512], in_=psQ[0:8, :], func=AF.Exp, scale=-1.0, bias=pv2[0:8, 14:15])))(),
                     reads=["psQf", "pv2f"], writes=["fe"])
        def half(hf):
            c0 = hf * HW
            P.op("scalar", lambda h: h.activation(out=fe[:], in_=fe[:], func=AF.Ln, bias=1.0), reads=["fe"], writes=["fe"])
            if hf == 0:
                P.op("vector", lambda h: h.tensor_tensor_scan(out=fm[0][:], data0=fe[:], data1=fe[:], initial=0.0, op0=ALU.add, op1=ALU.max), reads=["fe"], writes=["fm0"])
            else:
                P.op("vector", lambda h: h.tensor_copy(fcar[:], fm0[:, HW - 1:HW]), reads=["fm0"], writes=["fcar"])
                P.op("vector", lambda h: h.tensor_tensor_scan(out=fm0[:], data0=fe[:], data1=fe[:], initial=fcar[:, 0:1], op0=ALU.add, op1=ALU.max), reads=["fe", "fcar"], writes=["fm0"])
            fmh = fm[hf]
            fmn = "fm0"
            P.op("scalar", (lambda fmh=fmh: (lambda h: h.activation(out=pcs[0][:], in_=fmh[:], func=AF.Copy)))(), reads=[fmn], writes=["pc0"])
            P.op("vector", (lambda fmh=fmh: (lambda h: h.tensor_tensor(fr[:], fmh[:], pcs[0][:], op=ALU.subtract)))(), reads=[fmn, "pc0", "fe"], writes=["fe"])
            P.op("scalar", lambda h: h.activation(out=pcs[1][:], in_=fr[:], func=AF.Copy), reads=["fe"], writes=["pc1"])
            P.op("vector", lambda h: h.tensor_tensor(fr[:], fr[:], pcs[1][:], op=ALU.subtract), reads=["fe", "pc1"], writes=["fe"])
            P.op("scalar", lambda h: h.activation(out=pcs[2][:], in_=fr[:], func=AF.Copy), reads=["fe"], writes=["pc2"])
            for j in range(3):
                P.op("scalar", (lambda j=j: (lambda h: h.activation(out=pcs[3 + j][:], in_=pcs[j][:], func=AF.Copy, scale=-1.0)))(), reads=["pc%d" % j], writes=["pc%d" % (3 + j)])
            rowsrc = [3, 4, 5, 6, 6, 6, 6, 6, 6, 0, 1, 2]
            for row in range(12):
                src = rowsrc[row]
                P.dma("scalar", (lambda row=row, src=src, c0=c0: (lambda h: h.dma_start(out=self.cscr[:, row, c0:c0 + HW], in_=pcs[src][:])))(),
                      reads=["pc%d" % src], writes=["cscr_%d_%d" % (row, hf)], semres="pc%d" % src)
            if self.debug == "A3f":
                o = self.dbg_out("fm%d" % hf, [8, HW])
                P.dma("sync", (lambda o=o, fmh=fmh: (lambda h: h.dma_start(out=o[:, :], in_=fmh[:])))(), reads=[fmn], writes=["dbgf%d" % hf])
        return block, half


    def phaseA1(self, stA):
        nc, P = self.nc, self.P
        hT, cb, nhalf = self.hT, self.cb, self.nhalf
        with ExitStack() as st:
            xt = [st.enter_context(nc.sbuf_tensor("xt%d" % i, [128, D], F32)) for i in range(3)]
            hb = [st.enter_context(nc.sbuf_tensor("hb%d" % i, [128, D], BF16)) for i in range(2)]
            junk = st.enter_context(nc.sbuf_tensor("junkA", [128, D], BF16))
            stat = st.enter_context(nc.sbuf_tensor("statA", [128, 3 * NT], F32))
            gmixb = st.enter_context(nc.sbuf_tensor("gmixb", [128, D], F32))
            P.dma("sync", lambda h: h.dma_start(out=gmixb[:], in_=self.bvec[:, BV_GMIX:BV_GMIX + D].partition_broadcast(128)), writes=["gmixb"])
            pT = [st.enter_context(nc.psum_tensor("pTA%d" % i, [128, D], BF16)) for i in range(2)]
            fblock, fhalf = self.emit_forget(st)
            units = []
            for i in range(NT):
                def mk(i=i):
                    xb, b2 = i % 3, i % 2
                    xn, hn, pn = "xt%d" % xb, "hb%d" % b2, "pTA%d" % b2

                    def s0():
                        P.dma("sync", lambda h: h.dma_start(out=xt[xb][:], in_=self.x[i * 128:(i + 1) * 128, :]), writes=[xn])

                    def s1():
                        P.op("scalar", lambda h: h.activation(out=junk[:], in_=xt[xb][:], func=AF.Square, accum_out=stat[:, i:i + 1]), reads=[xn], writes=["junkA", "ssq%d" % i])
                        P.op("vector", lambda h: h.tensor_scalar(stat[:, NT + i:NT + i + 1], stat[:, i:i + 1], 1.0 / D, EPS, op0=ALU.mult, op1=ALU.add), reads=["ssq%d" % i], writes=["var%d" % i])
                        P.op("gpsimd", lambda h: h.tensor_tensor(stat[:, 2 * NT + i:2 * NT + i + 1], stat[:, NT + i:NT + i + 1], nhalf[:, 0:1], op=ALU.pow), reads=["var%d" % i, "nhalf"], writes=["rstd%d" % i])

                    def s2():
                        P.op("vector", lambda h: h.scalar_tensor_tensor(out=hb[b2][:], in0=xt[xb][:], scalar=stat[:, 2 * NT + i:2 * NT + i + 1], in1=gmixb[:], op0=ALU.mult, op1=ALU.mult),
                             reads=[xn, "rstd%d" % i, "gmixb"], writes=[hn])

                    def s3():
                        for kc in range(8):
                            P.op("tensor", (lambda kc=kc: (lambda h: h.transpose(pT[b2][:, kc * 128:(kc + 1) * 128], hb[b2][:, kc * 128:(kc + 1) * 128], cb[:, CB_ID:CB_ID + 128])))(),
                                 reads=[hn, "cb"], writes=[pn], inc=(kc == 7))

                    def s4():
                        if i % 2 == 0:
                            P.op("scalar", lambda h: h.activation(out=hT[:, :, i * 128:(i + 1) * 128], in_=pT[b2][:].rearrange("p (k t) -> p k t", k=8), func=AF.Copy), reads=[pn], writes=["hT%d" % i])
                        else:
                            P.op("vector", lambda h: h.tensor_copy(hT[:, :, i * 128:(i + 1) * 128], pT[b2][:].rearrange("p (k t) -> p k t", k=8)), reads=[pn], writes=["hT%d" % i])
                        if i == 5:
                            self.emit_kv(st)
                        if i % 4 == 3:
                            fblock(i // 4)
                        if i % 16 == 15:
                            fhalf(i // 16)
                    return [s0, s1, s2, s3, s4]
                units.append(mk())
            run_pipeline(units)
            if self.debug == "A1":
                o = self.dbg_out("hT", [128, 8 * S], BF16)
                P.dma("sync", lambda h: h.dma_start(out=o[:, :], in_=hT[:].rearrange("p k t -> p (k t)")), reads=["hT%d" % i for i in range(NT)], writes=["dbg"])

    def phaseA2(self):
        nc, P = self.nc, self.P
        hT, cb, pv, pv2 = self.hT, self.cb, self.pv, self.pv2
        winv = self.w_in.rearrange("(kc p) n -> p kc n", p=128)
        with ExitStack() as st:
            sb = lambda name, shape, dt: st.enter_context(nc.sbuf_tensor(name, shape, dt))
            lw = sb("lw", [128, 1024], F32)
            lwb = sb("lwb", [128, 1024], BF16)
            wu = [sb("wu%d" % i, [128, 8, 128], BF16) for i in range(2)]
            wg = [sb("wg%d" % i, [128, 8, 128], BF16) for i in range(2)]
            ub = [sb("ub%d" % i, [128, 515], F32) for i in range(2)]
            yb = sb("yb0", [128, S], BF16)
            aaF = sb("aaF", [128, S], F32)
            t1F = sb("t1F", [128, S], F32)
            omF = sb("omF", [128, S], F32)
            geF = sb("geF", [128, S], F32)
            names = ["xc", "tr", "ti", "gs", "g2"]
            T = {n: [sb(n + "%d" % i, [128, 512], F32) for i in range(2)] for n in names}
            xcb = [sb("xcb%d" % i, [128, 512], BF16) for i in range(2)]
            psU = [st.enter_context(nc.psum_tensor("psU%d" % i, [128, 512], F32)) for i in range(2)]
            psG = [st.enter_context(nc.psum_tensor("psG%d" % i, [128, 512], F32)) for i in range(2)]
            psR = [st.enter_context(nc.psum_tensor("psR%d" % i, [128, 512], F32)) for i in range(2)]
            psI = [st.enter_context(nc.psum_tensor("psI%d" % i, [128, 512], F32)) for i in range(2)]
            P.dma("sync", lambda h: h.dma_start(out=lw[:], in_=self.lruw[:, :]), writes=["lw"])
            P.op("vector", lambda h: h.tensor_copy(lwb[:], lw[:]), reads=["lw"], writes=["lwb"])
            def load_ct(ct):
                cp = ct % 2
                P.dma("gpsimd", (lambda ct=ct, cp=cp: (lambda h: h.dma_start(out=wu[cp][:], in_=winv[:, :, ct * 128:(ct + 1) * 128])))(), writes=["wu%d" % cp])
                P.dma("gpsimd", (lambda ct=ct, cp=cp: (lambda h: h.dma_start(out=wg[cp][:], in_=winv[:, :, 512 + ct * 128:512 + (ct + 1) * 128])))(), writes=["wg%d" % cp])
            load_ct(0)
            load_ct(1)
            allb = lambda n: [n + "%d" % t for t in range(NB)]
            units = []
            for ct in range(4):
                for tb in range(NB):
                    def mk(ct=ct, tb=tb):
                        it = ct * NB + tb
                        b = it % 2
                        cp = ct % 2
                        cwc = PV["convw"] + ct * 4
                        t0 = tb * 512
                        blk = slice(t0, t0 + 512)
                        hres = ["hT%d" % (tb * 4 + j) for j in range(4)]
                        R_ = lambda n: n + "%d" % b
                        xc, tr, ti, gs, g2 = (T[n][b] for n in ["xc", "tr", "ti", "gs", "g2"])
                        ur = [R_("ubm"), R_("ubh")]

                        def s0():
                            if tb == 0 and ct >= 1 and ct + 1 < 4:
                                load_ct(ct + 1)
                            if tb % 2 == 0:
                                self.precast(4 * ct + tb // 2, 4 * ct + tb // 2 + 1)
                            for kc in range(8):
                                P.op("tensor", (lambda kc=kc: (lambda h: h.matmul(psU[b][:], wu[cp][:, kc, :], hT[:, kc, t0:t0 + 512], start=(kc == 0), stop=(kc == 7))))(),
                                     reads=["wu%d" % cp] + hres, writes=[R_("psU")], inc=(kc == 7))
                            for kc in range(8):
                                P.op("tensor", (lambda kc=kc: (lambda h: h.matmul(psG[b][:], wg[cp][:, kc, :], hT[:, kc, t0:t0 + 512], start=(kc == 0), stop=(kc == 7))))(),
                                     reads=["wg%d" % cp] + hres, writes=[R_("psG")], inc=(kc == 7))

                        def s1():
                            P.op("scalar", lambda h: h.activation(out=ub[b][:, 3:515], in_=psU[b][:], func=AF.Copy), reads=[R_("psU")], writes=[R_("ubm")])
                            yield
                            if tb == 0:
                                P.op("gpsimd", lambda h: h.memset(ub[b][:, 0:3], 0.0), writes=[R_("ubh")])
                            else:
                                P.op("gpsimd", lambda h: h.tensor_copy(ub[b][:, 0:3], ub[1 - b][:, 512:515]), reads=["ubm%d" % (1 - b)], writes=[R_("ubh")])
                            P.op("scalar", lambda h: h.activation(out=gs[:], in_=psG[b][:], func=AF.Copy), reads=[R_("psG")], writes=[R_("gs")])
                            yield
                            P.op("scalar", lambda h: h.activation(out=g2[:], in_=psG[b][:], func=AF.Square), reads=[R_("psG")], writes=[R_("g2")])
                            yield
                            P.op("gpsimd", lambda h: h.tensor_scalar(g2[:], g2[:], 0.044715, 1.0, op0=ALU.mult, op1=ALU.add), reads=[R_("g2")], writes=[R_("g2")])
                            yield
                            P.op("vector", lambda h: h.tensor_scalar(xc[:], ub[b][:, 0:512], pv[:, cwc:cwc + 1], pv[:, PV["convb"] + ct:PV["convb"] + ct + 1], op0=ALU.mult, op1=ALU.add),
                                 reads=ur + ["pv"], writes=[R_("xc")])
                            for tap in range(1, 4):
                                P.op("vector", (lambda tap=tap: (lambda h: h.scalar_tensor_tensor(out=xc[:], in0=ub[b][:, tap:tap + 512], scalar=pv[:, cwc + tap:cwc + tap + 1], in1=xc[:], op0=ALU.mult, op1=ALU.add)))(),
                                     reads=ur + ["pv", R_("xc")], writes=[R_("xc")])
                            P.op("scalar", lambda h: h.activation(out=xcb[b][:], in_=xc[:], func=AF.Copy), reads=[R_("xc")], writes=[R_("xcb")])
                            yield
                            P.op("vector", lambda h: h.tensor_tensor(g2[:], g2[:], gs[:], op=ALU.mult), reads=[R_("g2"), R_("gs")], writes=[R_("g2")])
                            yield
                            P.op("scalar", lambda h: h.activation(out=g2[:], in_=g2[:], func=AF.Tanh, scale=0.7978845608028654), reads=[R_("g2")], writes=[R_("g2")])
                            yield
                            P.op("vector", lambda h: h.scalar_tensor_tensor(out=geF[:, blk], in0=g2[:], scalar=1.0, in1=gs[:], op0=ALU.add, op1=ALU.mult), reads=[R_("g2"), R_("gs")], writes=["geF%d" % tb])
                            yield

                        def s2():
                            P.op("tensor", lambda h: h.matmul(psR[b][:], lwb[:, ct * 256:ct * 256 + 128], xcb[b][:], start=True, stop=True), reads=["lwb", R_("xcb")], writes=[R_("psR")])
                            yield
                            P.op("tensor", lambda h: h.matmul(psI[b][:], lwb[:, ct * 256 + 128:ct * 256 + 256], xcb[b][:], start=True, stop=True), reads=["lwb", R_("xcb")], writes=[R_("psI")])
                            yield
                            P.op("scalar", lambda h: h.activation(out=tr[:], in_=psR[b][:], func=AF.Tanh, scale=0.5, bias=pv2[:, ct:ct + 1]), reads=[R_("psR"), "pv2a"], writes=[R_("tr")])
                            yield
                            P.op("scalar", lambda h: h.activation(out=ti[:], in_=psI[b][:], func=AF.Tanh, scale=0.5, bias=pv2[:, 4 + ct:5 + ct]), reads=[R_("psI"), "pv2b"], writes=[R_("ti")])
                            yield
                            P.op("scalar", lambda h: h.activation(out=aaF[:, blk], in_=tr[:], func=AF.Exp, scale=pv2[:, 8 + ct:9 + ct], bias=pv2[:, 8 + ct:9 + ct]), reads=[R_("tr"), "pv2c"], writes=["aaF%d" % tb])
                            yield
                            P.op("scalar", lambda h: h.activation(out=omF[:, blk], in_=aaF[:, blk], func=AF.Square), reads=["aaF%d" % tb], writes=["omF%d" % tb])
                            yield
                            P.op("gpsimd", lambda h: h.tensor_scalar(omF[:, blk], omF[:, blk], -1.0, 1.0, op0=ALU.mult, op1=ALU.add), reads=["omF%d" % tb], writes=["omF%d" % tb])
                            yield
                            P.op("vector", lambda h: h.scalar_tensor_tensor(out=t1F[:, blk], in0=ti[:], scalar=1.0, in1=xc[:], op0=ALU.add, op1=ALU.mult), reads=[R_("ti"), R_("xc")], writes=["t1F%d" % tb])
                            yield
                            if tb == NB - 1:
                                P.op("scalar", lambda h: h.activation(out=omF[:], in_=omF[:], func=AF.Sqrt), reads=allb("omF"), writes=allb("omF"))
                                P.op("vector", lambda h: h.scalar_tensor_tensor(out=omF[:], in0=t1F[:], scalar=0.5, in1=omF[:], op0=ALU.mult, op1=ALU.mult), reads=allb("t1F") + allb("omF"), writes=allb("omF"))
                                P.op("vector", lambda h: h.tensor_tensor_scan(out=t1F[:], data0=aaF[:], data1=omF[:], initial=0.0, op0=ALU.mult, op1=ALU.add), reads=allb("aaF") + allb("omF"), writes=allb("t1F"))
                                P.op("vector", lambda h: h.scalar_tensor_tensor(out=yb[:], in0=geF[:], scalar=pv2[:, 16 + ct:17 + ct], in1=t1F[:], op0=ALU.mult, op1=ALU.mult), reads=allb("geF") + allb("t1F") + ["pv2g"], writes=["yb0"])
                                P.dma("scalar", lambda h: h.dma_start(out=self.ymix[ct * 128:(ct + 1) * 128, :], in_=yb[:]), reads=["yb0"], writes=["ymix_l%d" % ct], semres="yb0")
                        def s2_full():
                            for _ in s2():
                                pass
                        return [s0, s1, s2_full if tb == NB - 1 else s2]
                    units.append(mk())
            run_pipeline(units)

    def phaseA3(self):
        nc, P = self.nc, self.P
        hT, cb, pv, pv2, nhalf = self.hT, self.cb, self.pv, self.pv2, self.nhalf
        winv = self.w_in.rearrange("(kc p) n -> p kc n", p=128)
        QC, KC, VC, FC = 1024, 1536, 2048, 2560
        with ExitStack() as st:
            sb = lambda name, shape, dt: st.enter_context(nc.sbuf_tensor(name, shape, dt))
            ps = lambda name, shape, dt=F32: st.enter_context(nc.psum_tensor(name, shape, dt))
            vtok = sb("vtok", [128, NT, 512], BF16)
            wv = sb("wv", [128, 8, 512], BF16)
            psS = [ps("psS%d" % i, [128, 2, 512]) for i in range(2)]
            psO = [ps("psO%d" % i, [128, 512]) for i in range(2)]
            psQ = ps("psQ", [128, 512])
            psN = ps("psN", [128, 512])
            P.dma("gpsimd", lambda h: h.dma_start(out=wv[:], in_=winv[:, :, VC:VC + 512]), writes=["wv"])
            for i in range(NT):
                pq = psS[i % 2]
                pn = "psS%d_0" % (i % 2)
                for kc in range(8):
                    P.op("tensor", (lambda kc=kc, i=i, pq=pq: (lambda h: h.matmul(pq[:, 0, :], hT[:, kc, i * 128:(i + 1) * 128], wv[:, kc, :], start=(kc == 0), stop=(kc == 7))))(),
                         reads=["wv", "hT%d" % i], writes=[pn], inc=(kc == 7))
                eng = "scalar" if i % 2 == 0 else "vector"
                if eng == "scalar":
                    P.op("scalar", (lambda i=i, pq=pq: (lambda h: h.activation(out=vtok[:, i, :], in_=pq[:, 0, :], func=AF.Copy)))(), reads=[pn], writes=["vtok%d" % i])
                else:
                    P.op("vector", (lambda i=i, pq=pq: (lambda h: h.tensor_copy(vtok[:, i, :], pq[:, 0, :])))(), reads=[pn], writes=["vtok%d" % i])
            sb = lambda name, shape, dt: st.enter_context(nc.sbuf_tensor(name, shape, dt))
            wqk = [sb("wqk%d" % i, [128, 8, 256], BF16) for i in range(2)]
            qa = [sb("qa%d" % i, [128, S], BF16) for i in range(2)]
            ka = [sb("ka%d" % i, [128, S], BF16) for i in range(2)]
            va = [sb("va%d" % i, [128, NT, 128], BF16) for i in range(2)]
            yf = [sb("yf0", [128, S], BF16)] * 2
            sq = [sb("sq%d" % i, [128, 512], BF16) for i in range(2)]
            vr = [sb("vr%d" % i, [128, 512], F32) for i in range(2)]
            rs = [sb("rs%d" % i, [128, 512], F32) for i in range(2)]
            pt = [sb("pt%d" % i, [128, 2, 512], BF16) for i in range(3)]
            rl = [sb("rl%d" % i, [64, 512], F32) for i in range(2)]
            for i in range(2):
                P.op("gpsimd", (lambda i=i: (lambda h: h.memset(va[i][:, :, 64:128], 1.0)))(), writes=["va1_%d" % i])
            nrm = 0
            pti = 0
            oi = 0
            for p in range(4):
                pp = p % 2
                P.dma("gpsimd", (lambda p=p, pp=pp: (lambda h: h.dma_start(out=wqk[pp][:, :, 0:128], in_=winv[:, :, QC + p * 128:QC + (p + 1) * 128])))(), writes=["wqk%d" % pp])
                P.dma("gpsimd", (lambda p=p, pp=pp: (lambda h: h.dma_start(out=wqk[pp][:, :, 128:256], in_=winv[:, :, KC + p * 128:KC + (p + 1) * 128])))(), writes=["wqk%d" % pp])
                for hp in range(2):
                    hd = 2 * p + hp
                    P.dma("sync", (lambda hp=hp, hd=hd: (lambda h: h.dma_start(out=qa[hp][64:70, :], in_=self.cscr[hd, 0:6, :])))(), writes=["qa%d_aug" % hp])
                    P.dma("sync", (lambda hp=hp, hd=hd: (lambda h: h.dma_start(out=ka[hp][64:70, :], in_=self.cscr[hd, 6:12, :])))(), writes=["ka%d_aug" % hp])
                    P.op("gpsimd", (lambda hp=hp, hd=hd: (lambda h: h.tensor_copy(va[hp][:, :, 0:64], vtok[:, :, hd * 64:(hd + 1) * 64])))(),
                         reads=["vtok%d" % i for i in range(NT)], writes=["va0_%d" % hp])
                if p == 1:
                    for r in range(NE * CAP // 256):
                        P.dma_bg("gpsimd", (lambda r=r: (lambda h: h.dma_start(out=self.xbuf[r * 256:(r + 1) * 256, :].rearrange("(p a) n -> p a n", a=2), in_=self.zt[:])))(), "xz")
                PQ4 = [(psQ[:], "psQ"), (psS[0][:, 0, :], "psS0_0"), (psS[0][:, 1, :], "psS0_1"), (psS[1][:, 0, :], "psS1_0")]
                PN2 = [(psN[:], "psN"), (psS[1][:, 1, :], "psS1_1")]
                units = []
                for tb in range(NB):
                    for which in range(2):
                        def mk(tb=tb, which=which, nrm=nrm, pp=pp):
                            t0 = tb * 512
                            hres = ["hT%d" % (tb * 4 + j) for j in range(4)]
                            nb = nrm % 2
                            pq, pqn = PQ4[nrm % 4]
                            pn_, pnn = PN2[nrm % 2]
                            dst = qa if which == 0 else ka
                            gcol = pv2[:, 12:13] if which == 0 else pv[:, PV["gk2"]:PV["gk2"] + 1]
                            gres = "pv2d" if which == 0 else "pv"

                            def s0():
                                for kc in range(8):
                                    P.op("tensor", (lambda kc=kc: (lambda h: h.matmul(pq, wqk[pp][:, kc, which * 128:(which + 1) * 128], hT[:, kc, t0:t0 + 512], start=(kc == 0), stop=(kc == 7))))(),
                                         reads=["wqk%d" % pp] + hres, writes=[pqn], inc=(kc == 7))

                            def s1():
                                P.op("scalar", lambda h: h.activation(out=sq[nb][:], in_=pq, func=AF.Square), reads=[pqn], writes=["sq%d" % nb])

                            def s1b():
                                P.op("tensor", lambda h: h.matmul(pn_, cb[:, CB_BO:CB_BO + 128], sq[nb][:], start=True, stop=True), reads=["cb", "sq%d" % nb], writes=[pnn])

                            def s2():
                                P.op("scalar", lambda h: h.activation(out=vr[nb][:], in_=pn_, func=AF.Ln, scale=1.0 / 64, bias=self.epsc[:, 0:1]), reads=[pnn, "epsc"], writes=["vr%d" % nb])
                                P.op("scalar", lambda h: h.activation(out=rs[nb][:], in_=vr[nb][:], func=AF.Exp, scale=-0.5), reads=["vr%d" % nb], writes=["rs%d" % nb])
                                for hp in range(2):
                                    P.op("vector", (lambda hp=hp: (lambda h: h.scalar_tensor_tensor(out=dst[hp][0:64, t0:t0 + 512], in0=pq[hp * 64:(hp + 1) * 64, :], scalar=gcol[hp * 64:(hp + 1) * 64, :], in1=rs[nb][hp * 64:(hp + 1) * 64, :], op0=ALU.mult, op1=ALU.mult)))(),
                                         reads=[pqn, "rs%d" % nb, gres], writes=[("qa%d_%d" if which == 0 else "ka%d_%d") % (hp, tb)])
                            return [s0, s1, s1b, s2]
                        units.append(mk())
                        nrm += 1
                run_pipeline(units)
                if self.debug == "A3q" and p == 0:
                    o = self.dbg_out("qa", [128, S], BF16)
                    o2 = self.dbg_out("ka", [128, S], BF16)
                    P.dma("sync", lambda h: h.dma_start(out=o[:, :], in_=qa[0][:]), reads=["qa0_%d" % t for t in range(NB)] + ["qa0_aug"], writes=["dbg"])
                    P.dma("sync", lambda h: h.dma_start(out=o2[:, :], in_=ka[0][:]), reads=["ka0_%d" % t for t in range(NB)] + ["ka0_aug"], writes=["dbg2"])
                units = []
                for hp in range(2):
                    for j in range(NB):
                        ob = oi % 2
                        oi += 1
                        nkt = 4 * j + 4
                        for g in range(0, nkt, 2):
                            def mk(hp=hp, j=j, g=g, ob=ob, nkt=nkt, pti=pti, pp=pp, p=p, gi=len(units)):
                                q0 = j * 512
                                sbi = pti % 2
                                ptb = pti % 3
                                qres = ["qa%d_%d" % (hp, j), "qa%d_aug" % hp]

                                cst = [max(0, 128 * (g + u - 4 * j)) for u in range(2)]
                                ce = min(cst)

                                def s0():
                                    if gi % 36 == 0:
                                        self.precast(16 + 4 * p + gi // 36, 16 + 4 * p + gi // 36 + 1)
                                    for u in range(2):
                                        i = g + u
                                        m = i - 4 * j
                                        c0 = cst[u]
                                        kres = ["ka%d_%d" % (hp, i // 4), "ka%d_aug" % hp]
                                        last = (m < 0)
                                        P.op("tensor", (lambda u=u, i=i, last=last, c0=c0: (lambda h: h.matmul(psS[sbi][:, u, c0:512], ka[hp][0:70, i * 128:(i + 1) * 128], qa[hp][0:70, q0 + c0:q0 + 512], start=True, stop=last)))(),
                                             reads=kres + qres, writes=["psS%d_%d" % (sbi, u)], inc=last)
                                        if m >= 0:
                                            P.op("tensor", (lambda u=u, c0=c0: (lambda h: h.matmul(psS[sbi][:, u, c0:c0 + 128], cb[:, CB_ID:CB_ID + 128], cb[:, CB_MASK:CB_MASK + 128], start=False, stop=True)))(),
                                                 reads=["cb"], writes=["psS%d_%d" % (sbi, u)], inc=True)

                                def s1():
                                    P.op("scalar", lambda h: h.activation(out=pt[ptb][:, :, ce:512], in_=psS[sbi][:, :, ce:512], func=AF.Exp), reads=["psS%d_0" % sbi, "psS%d_1" % sbi], writes=["pt%d" % ptb])

                                def s2():
                                    for u in range(2):
                                        i = g + u
                                        c0 = cst[u]
                                        P.op("tensor", (lambda u=u, i=i, c0=c0: (lambda h: h.matmul(psO[ob][:, c0:512], va[hp][:, i, :], pt[ptb][:, u, c0:512], start=(i == 0), stop=(i == nkt - 1))))(),
                                             reads=["va0_%d" % hp, "va1_%d" % hp, "pt%d" % ptb], writes=["psO%d" % ob], inc=(u == 1))
                                    if g + 2 >= nkt:
                                        P.op("vector", lambda h: h.reciprocal(rl[ob][:], psO[ob][64:128, :]), reads=["psO%d" % ob], writes=["rl%d" % ob])
                                        P.op("vector", lambda h: h.scalar_tensor_tensor(out=yf[pp][hp * 64:(hp + 1) * 64, q0:q0 + 512], in0=psO[ob][0:64, :], scalar=pv[0:64, PV["gfx"] + 2 * p + hp:PV["gfx"] + 2 * p + hp + 1], in1=rl[ob][:], op0=ALU.mult, op1=ALU.mult),
                                             reads=["psO%d" % ob, "rl%d" % ob, "pv"], writes=["yf0"])
                                return [s0, s1, s2]
                            units.append(mk())
                            pti += 1
                run_pipeline(units)
                P.dma("scalar", (lambda p=p, pp=pp: (lambda h: h.dma_start(out=self.ymix[512 + p * 128:512 + (p + 1) * 128, :], in_=yf[pp][:])))(), reads=["yf0"], writes=["ymix_f%d" % p], semres="yf0")


    def phaseB(self):
        nc, P = self.nc, self.P
        cb, cf, pv, pv2, nhalf = self.cb, self.cf, self.pv, self.pv2, self.nhalf
        gates, idxs = self.gates, self.idxs
        with ExitStack() as st:
            sb = lambda name, shape, dt: st.enter_context(nc.sbuf_tensor(name, shape, dt))
            ps = lambda name, shape, dt=F32: st.enter_context(nc.psum_tensor(name, shape, dt))
            knT, vmem = self.knT, self.vmem
            gbc = sb("gbc", [128, 1060], F32)
            wr32 = sb("wr32", [128, 8, 36], F32)
            cntrow = sb("cntrow", [1, 32], F32)
            P.dma("sync", lambda h: h.dma_start(out=gbc[:], in_=self.bvec[:, 0:1060].partition_broadcast(128)), writes=["gbc"])
            P.dma("sync", lambda h: h.dma_start(out=wr32[:], in_=self.wr.rearrange("(kc p) n -> p kc n", p=128)), writes=["wr32"])
            P.op("gpsimd", lambda h: h.memset(cntrow[:], 0.0), writes=["cntrow"])
            self.x1buf = nc.dram_tensor("x1buf", [S, D], F32).ap()
            sb = lambda name, shape, dt: st.enter_context(nc.sbuf_tensor(name, shape, dt))
            qnT = sb("qnT_all", [128, 4, S], BF16)
            statB = sb("statB", [128, NT, 8], F32)
            sX = ExitStack()
            xnT = sX.enter_context(nc.sbuf_tensor("xnT_all", [128, 8, S], BF16))
            ymv = self.ymix.rearrange("(cc p) t -> p cc t", p=128)
            with ExitStack() as s1:
                sb1 = lambda name, shape, dt: s1.enter_context(nc.sbuf_tensor(name, shape, dt))
                ps1 = lambda name, shape, dt=F32: s1.enter_context(nc.psum_tensor(name, shape, dt))
                woutb = sb1("woutb_s", [128, 8, D], BF16)
                yt = [sb1("yt%d" % i, [128, 8, 512], BF16) for i in range(2)]
                ysq = sb1("ysq", [128, 8, 512], BF16)
                xt = [sb1("xtB%d" % i, [128, D], F32) for i in range(3)]
                x1t = [sb1("x1t%d" % i, [128, D], F32) for i in range(4)]
                xnb = [sb1("xnb%d" % i, [128, D], BF16) for i in range(2)]
                junk_b1 = sb1("junkB1", [128, D], BF16)
                bkS = ps1("bkS", [128, 512])
                pW_b1 = [[ps1("pW%d%d" % (a_, b_), [128, 512]) for b_ in range(2)] for a_ in range(2)]
                pT_b1 = [ps1("pTB%d" % i, [128, D], BF16) for i in range(2)]
                gmxb = sb1("gmxb", [128, D], F32)
                P.dma("sync", lambda h: h.dma_start(out=gmxb[:], in_=self.bvec[:, BV_GMEMX:BV_GMEMX + D].partition_broadcast(128)), writes=["gmxb"])
                P.dma("gpsimd", lambda h: h.dma_start(out=woutb[:], in_=self.w_out.rearrange("(kc p) n -> p kc n", p=128)), writes=["woutb"])
                units = []
                for i in range(NT):
                    def mk(i=i):
                        tb, tt = i // 4, i % 4
                        yb = tb % 2
                        x3 = i % 3
                        b2 = i % 2
                        ts = slice(tt * 128, (tt + 1) * 128)
                        c0 = (i % 8) * 2
                        ytn, xn_, x1n = "yt%d" % yb, "xtB%d" % x3, "x1t%d" % (i % 4)

                        def s0():
                            if tt == 0:
                                P.dma("sync", lambda h: h.dma_start(out=yt[yb][:], in_=ymv[:, :, tb * 512:(tb + 1) * 512]), writes=[ytn])
                                P.op("vector", lambda h: h.tensor_tensor(ysq[:], yt[yb][:], yt[yb][:], op=ALU.mult), reads=[ytn], writes=["ysq"])
                            P.dma("sync", lambda h: h.dma_start(out=xt[x3][:], in_=self.x[i * 128:(i + 1) * 128, :]), writes=[xn_])

                        def s1_():
                            for grp in range(2):
                                for c4 in range(4):
                                    cc = grp * 4 + c4
                                    P.op("tensor", (lambda cc=cc, grp=grp, c4=c4: (lambda h: h.matmul(bkS[:, c0 + grp:c0 + grp + 1], ysq[:, cc, ts], self.gsq[:, cc:cc + 1], start=(c4 == 0), stop=(c4 == 3))))(),
                                         reads=["ysq", "gsq"], writes=["bkS"], inc=(c4 == 3))
                            for half in range(2):
                                hs_ = slice(half * 512, (half + 1) * 512)
                                for grp in range(2):
                                    for c4 in range(4):
                                        cc = grp * 4 + c4
                                        P.op("tensor", (lambda cc=cc, grp=grp, c4=c4, half=half, hs_=hs_: (lambda h: h.matmul(pW_b1[half][grp][:], yt[yb][:, cc, ts], woutb[:, cc, hs_], start=(c4 == 0), stop=(c4 == 3))))(),
                                             reads=[ytn, "woutb"], writes=["pW%d%d" % (half, grp)], inc=(c4 == 3))

                        def s2():
                            P.op("vector", lambda h: h.tensor_scalar(statB[:, i, 0:2], bkS[:, c0:c0 + 2], 1.0 / 512, EPS, op0=ALU.mult, op1=ALU.add), reads=["bkS"], writes=["sB01_%d" % i])
                            yield
                            P.op("gpsimd", lambda h: h.tensor_tensor(statB[:, i, 2:4], statB[:, i, 0:2], nhalf[:, 0:2], op=ALU.pow), reads=["sB01_%d" % i, "nhalf"], writes=["sB23_%d" % i])
                            yield
                            for half in range(2):
                                hs_ = slice(half * 512, (half + 1) * 512)
                                P.op("vector", (lambda half=half, hs_=hs_: (lambda h: h.scalar_tensor_tensor(out=x1t[i % 4][:, hs_], in0=pW_b1[half][0][:], scalar=statB[:, i, 2:3], in1=xt[x3][:, hs_], op0=ALU.mult, op1=ALU.add)))(),
                                     reads=["pW%d0" % half, "sB23_%d" % i, xn_], writes=[x1n])
                                P.op("vector", (lambda half=half, hs_=hs_: (lambda h: h.scalar_tensor_tensor(out=x1t[i % 4][:, hs_], in0=pW_b1[half][1][:], scalar=statB[:, i, 3:4], in1=x1t[i % 4][:, hs_], op0=ALU.mult, op1=ALU.add)))(),
                                     reads=["pW%d1" % half, "sB23_%d" % i, x1n], writes=[x1n])
                            P.dma("gpsimd", lambda h: h.dma_start(out=self.x1buf[i * 128:(i + 1) * 128, :], in_=x1t[i % 4][:]), reads=[x1n], writes=["x1buf%d" % i], semres=x1n)
                            P.op("scalar", lambda h: h.activation(out=junk_b1[:], in_=x1t[i % 4][:], func=AF.Square, accum_out=statB[:, i, 4:5]), reads=[x1n], writes=["junkB1", "sB4_%d" % i])
                            yield

                        def s2b():
                            P.op("vector", lambda h: h.tensor_scalar(statB[:, i, 5:6], statB[:, i, 4:5], 1.0 / D, EPS, op0=ALU.mult, op1=ALU.add), reads=["sB4_%d" % i], writes=["sB5_%d" % i])
                            yield
                            P.op("gpsimd", lambda h: h.tensor_tensor(statB[:, i, 6:7], statB[:, i, 5:6], nhalf[:, 0:1], op=ALU.pow), reads=["sB5_%d" % i, "nhalf"], writes=["sB6_%d" % i])
                            yield
                            P.op("vector", lambda h: h.scalar_tensor_tensor(out=xnb[b2][:], in0=x1t[i % 4][:], scalar=statB[:, i, 6:7], in1=gmxb[:], op0=ALU.mult, op1=ALU.mult), reads=[x1n, "sB6_%d" % i, "gmxb"], writes=["xnb%d" % b2])
                            yield

                        def s3():
                            for kc in range(8):
                                P.op("tensor", (lambda kc=kc: (lambda h: h.transpose(pT_b1[b2][:, kc * 128:(kc + 1) * 128], xnb[b2][:, kc * 128:(kc + 1) * 128], cb[:, CB_ID:CB_ID + 128])))(),
                                     reads=["xnb%d" % b2, "cb"], writes=["pTB%d" % b2], inc=(kc == 7))
                            if i % 2 == 0:
                                P.op("scalar", lambda h: h.activation(out=xnT[:, :, i * 128:(i + 1) * 128], in_=pT_b1[b2][:].rearrange("p (k t) -> p k t", k=8), func=AF.Copy), reads=["pTB%d" % b2], writes=["xnT%d" % i])
                            else:
                                P.op("vector", lambda h: h.tensor_copy(xnT[:, :, i * 128:(i + 1) * 128], pT_b1[b2][:].rearrange("p (k t) -> p k t", k=8)), reads=["pTB%d" % b2], writes=["xnT%d" % i])
                        return [s0, s1_, s2, s2b, s3]
                    units.append(mk())
                run_pipeline(units)
                if self.debug == "B":
                    self._o1 = self.dbg_out("x1", [S, D])
                    self._o2 = self.dbg_out("x2", [S, D])
                P.barrier()
            with ExitStack() as s2a:
                sb2 = lambda name, shape, dt: s2a.enter_context(nc.sbuf_tensor(name, shape, dt))
                ps2 = lambda name, shape, dt=F32: s2a.enter_context(nc.psum_tensor(name, shape, dt))
                wqb = sb2("wqb_s", [128, 8, 512], BF16)
                sq_2a = [sb2("sqB%d" % i, [128, 512], BF16) for i in range(2)]
                vr_2a = [sb2("vrB%d" % i, [128, 512], F32) for i in range(2)]
                rs_2a = [sb2("rsB%d" % i, [128, 512], F32) for i in range(2)]
                PQ_2a = [ps2("psQB%d" % i, [128, 512]) for i in range(4)]
                PN_2a = [ps2("psNB%d" % i, [128, 512]) for i in range(2)]
                P.dma("gpsimd", lambda h: h.dma_start(out=wqb[:], in_=self.mem_wq.rearrange("(kc p) n -> p kc n", p=128)), writes=["wqb"])

                units = []
                u = 0
                for tb in range(NB):
                    for hd in range(4):
                        def mk(tb=tb, hd=hd, u=u):
                            t0 = tb * 512
                            nb = u % 2
                            pq, pn_ = PQ_2a[u % 4], PN_2a[u % 2]
                            pqn, pnn = "psQB%d" % (u % 4), "psNB%d" % (u % 2)
                            xres = ["xnT%d" % (tb * 4 + t) for t in range(4)]

                            def s0():
                                for kc in range(8):
                                    P.op("tensor", (lambda kc=kc: (lambda h: h.matmul(pq[:], wqb[:, kc, hd * 128:(hd + 1) * 128], xnT[:, kc, t0:t0 + 512], start=(kc == 0), stop=(kc == 7))))(),
                                         reads=["wqb"] + xres, writes=[pqn], inc=(kc == 7))

                            def s1_():
                                P.op("scalar", lambda h: h.activation(out=sq_2a[nb][:], in_=pq[:], func=AF.Square), reads=[pqn], writes=["sqB%d" % nb])

                            def s1b():
                                P.op("tensor", lambda h: h.matmul(pn_[:], cb[:, CB_ONE:CB_ONE + 128], sq_2a[nb][:], start=True, stop=True), reads=["cb", "sqB%d" % nb], writes=[pnn])

                            def s2():
                                P.op("scalar", lambda h: h.activation(out=vr_2a[nb][:], in_=pn_[:], func=AF.Ln, scale=1.0 / 128, bias=self.epsc[:, 0:1]), reads=[pnn, "epsc"], writes=["vrB%d" % nb])
                                P.op("scalar", lambda h: h.activation(out=rs_2a[nb][:], in_=vr_2a[nb][:], func=AF.Exp, scale=-0.5), reads=["vrB%d" % nb], writes=["rsB%d" % nb])
                                P.op("vector", lambda h: h.scalar_tensor_tensor(out=qnT[:, hd, t0:t0 + 512], in0=pq[:], scalar=pv2[:, 13:14], in1=rs_2a[nb][:], op0=ALU.mult, op1=ALU.mult),
                                     reads=[pqn, "rsB%d" % nb, "pv2e"], writes=["qnT%d_%d" % (tb, hd)])
                            return [s0, s1_, s1b, s2]
                        units.append(mk())
                        u += 1
                run_pipeline(units)
                P.barrier()
            sX.close()
            onT = sb("onT_all", [128, 4, S], BF16)
            with ExitStack() as s2b:
                sb2 = lambda name, shape, dt: s2b.enter_context(nc.sbuf_tensor(name, shape, dt))
                ps2 = lambda name, shape, dt=F32: s2b.enter_context(nc.psum_tensor(name, shape, dt))
                pt = [sb2("ptB%d" % i, [128, 2, 512], BF16) for i in range(3)]
                rl = [sb2("rlB%d" % i, [128, 512], F32) for i in range(2)]
                psS = [ps2("psSB%d" % i, [128, 2, 512]) for i in range(2)]
                psO = [ps2("psOB%d" % i, [128, 512]) for i in range(2)]
                psL = [ps2("psLB%d" % i, [128, 512]) for i in range(2)]
                units = []
                u = 0
                for tb in range(NB):
                    for hd in range(4):
                        def mk(tb=tb, hd=hd, u=u):
                            t0 = tb * 512
                            b2, b3 = u % 2, u % 3

                            def s0():
                                for kt in range(2):
                                    P.op("tensor", (lambda kt=kt: (lambda h: h.matmul(psS[b2][:, kt, :], knT[:, hd, kt * 128:(kt + 1) * 128], qnT[:, hd, t0:t0 + 512], start=True, stop=True)))(),
                                         reads=["knT", "qnT%d_%d" % (tb, hd)], writes=["psSB%d" % b2], inc=(kt == 1))

                            def s1_():
                                P.op("scalar", lambda h: h.activation(out=pt[b3][:], in_=psS[b2][:], func=AF.Exp), reads=["psSB%d" % b2], writes=["ptB%d" % b3])

                            def s2():
                                for kt in range(2):
                                    P.op("tensor", (lambda kt=kt: (lambda h: h.matmul(psO[b2][:], vmem[:, kt, hd * 128:(hd + 1) * 128], pt[b3][:, kt, :], start=(kt == 0), stop=(kt == 1))))(),
                                         reads=["vmem", "ptB%d" % b3], writes=["psOB%d" % b2], inc=(kt == 1))
                                for kt in range(2):
                                    P.op("tensor", (lambda kt=kt: (lambda h: h.matmul(psL[b2][:], cb[:, CB_ONE:CB_ONE + 128], pt[b3][:, kt, :], start=(kt == 0), stop=(kt == 1))))(),
                                         reads=["cb", "ptB%d" % b3], writes=["psLB%d" % b2], inc=(kt == 1))

                            def s3():
                                P.op("scalar", lambda h: h.activation(out=rl[b2][:], in_=psL[b2][:], func=AF.Ln), reads=["psLB%d" % b2], writes=["rlB%d" % b2])
                                P.op("scalar", lambda h: h.activation(out=rl[b2][:], in_=rl[b2][:], func=AF.Exp, scale=-1.0), reads=["rlB%d" % b2], writes=["rlB%d" % b2])
                                P.op("vector", lambda h: h.tensor_tensor(onT[:, hd, t0:t0 + 512], psO[b2][:], rl[b2][:], op=ALU.mult), reads=["psOB%d" % b2, "rlB%d" % b2], writes=["onT%d_%d" % (tb, hd)])
                            return [s0, s1_, s2, s3]
                        units.append(mk())
                        u += 1
                run_pipeline(units)
                P.barrier()
            with ExitStack() as s3_:
                sb3 = lambda name, shape, dt: s3_.enter_context(nc.sbuf_tensor(name, shape, dt))
                ps3 = lambda name, shape, dt=F32: s3_.enter_context(nc.psum_tensor(name, shape, dt))
                wob = sb3("wob_s", [128, 4, D], BF16)
                x1r = [sb3("x1r%d" % i, [128, D], F32) for i in range(3)]
                x2t = [sb3("x2t%d" % i, [128, D], F32) for i in range(4)]
                xn2 = [sb3("xn2_%d" % i, [128, D], F32) for i in range(2)]
                xn2b = [sb3("xn2b%d" % i, [128, D], BF16) for i in range(4)]
                xn2T = [sb3("xn2T%d" % i, [128, 8, 128], F32) for i in range(2)]
                junk_b3 = sb3("junkB3", [128, D], BF16)
                SM = sb3("smB3", [128, 2, 32], F32)
                LG = sb3("lgB3", [128, 2, 36], F32)
                GOH = sb3("gohB3", [128, 2, 4], F32)
                ESEL = sb3("eselB3", [128, 2, 8], F32)
                MX8 = sb3("mx8B3", [128, 2, 8], F32)
                MK = sb3("mkB3", [128, 2, 8], F32)
                M2T = sb3("m2tB3", [128, 2, 8], F32)
                GEJ = sb3("gejB3", [128, 2, 4], F32)
                A1_ = sb3("A1B3", [128, 2, 32], F32)
                A2_ = sb3("A2B3", [128, 2, 32], F32)
                AA_ = sb3("AAB3", [128, 2, 32], F32)
                POS = sb3("posB3", [128, 2, 32], F32)
                J32 = sb3("j32B3", [128, 2, 32], F32)
                pW_b3 = [[ps3("pWo%d%d" % (a_, b_), [128, 512]) for b_ in range(2)] for a_ in range(2)]
                pX = ps3("pXB", [128, 2, 512])
                bk0 = ps3("bk0", [128, 512])
                bk1 = ps3("bk1", [128, 512])
                P.dma("gpsimd", lambda h: h.dma_start(out=wob[:], in_=self.mem_wo.rearrange("(kc p) n -> p kc n", p=128)), writes=["wob"])

                units = []
                for i in range(NT):
                    def mk(i=i):
                        tb, tt = i // 4, i % 4
                        x3, b2 = i % 3, i % 2
                        ts = slice(tb * 512 + tt * 128, tb * 512 + (tt + 1) * 128)
                        x1n, x2n = "x1r%d" % x3, "x2t%d" % (i % 4)
                        sm, lg, goh, esel, mx8, mk_, m2t, gej = SM[:, b2, :], LG[:, b2, :], GOH[:, b2, :], ESEL[:, b2, :], MX8[:, b2, :], MK[:, b2, :], M2T[:, b2, :], GEJ[:, b2, :]
                        A1, A2, AA, pos, j32 = A1_[:, b2, :], A2_[:, b2, :], AA_[:, b2, :], POS[:, b2, :], J32[:, b2, :]
                        N = lambda n: "%s_%d" % (n, b2)
                        V = lambda fn, reads, writes: P.op("vector", fn, reads=reads, writes=writes)

                        def s0():
                            P.dma("sync", lambda h: h.dma_start(out=x1r[x3][:], in_=self.x1buf[i * 128:(i + 1) * 128, :]), writes=[x1n])

                        def s1_():
                            for half in range(2):
                                hs_ = slice(half * 512, (half + 1) * 512)
                                for hd in range(4):
                                    P.op("tensor", (lambda hd=hd, half=half, hs_=hs_: (lambda h: h.matmul(pW_b3[b2][half][:], onT[:, hd, ts], wob[:, hd, hs_], start=(hd == 0), stop=(hd == 3))))(),
                                         reads=["onT%d_%d" % (tb, hd_) for hd_ in range(4)] + ["wob"], writes=["pWo%d%d" % (b2, half)], inc=(hd == 3))

                        def s2():
                            for half in range(2):
                                hs_ = slice(half * 512, (half + 1) * 512)
                                V((lambda half=half, hs_=hs_: (lambda h: h.tensor_tensor(x2t[i % 4][:, hs_], pW_b3[b2][half][:], x1r[x3][:, hs_], op=ALU.add)))(), ["pWo%d%d" % (b2, half), x1n], [x2n])
                            P.dma("scalar", lambda h: h.dma_start(out=self.x2buf[i * 128:(i + 1) * 128, :], in_=x2t[i % 4][:]), reads=[x2n], writes=["x2buf%d" % i], semres=x2n)
                            if self.debug == "B":
                                P.dma("sync", lambda h: h.dma_start(out=self._o2[i * 128:(i + 1) * 128, :], in_=x2t[i % 4][:]), reads=[x2n], writes=["dbg2"], semres=x2n)
                                P.dma("sync", lambda h: h.dma_start(out=self._o1[i * 128:(i + 1) * 128, :], in_=x1r[x3][:]), reads=[x1n], writes=["dbg1"], semres=x2n)
                            P.op("scalar", lambda h: h.activation(out=junk_b3[:], in_=x2t[i % 4][:], func=AF.Square, accum_out=sm[:, 8:9]), reads=[x2n], writes=["junkB3", N("sm8")])
                            V(lambda h: h.tensor_scalar(sm[:, 9:10], sm[:, 8:9], 1.0 / D, EPS, op0=ALU.mult, op1=ALU.add), [N("sm8")], [N("sm9")])
                            P.op("gpsimd", lambda h: h.tensor_tensor(sm[:, 10:11], sm[:, 9:10], nhalf[:, 0:1], op=ALU.pow), reads=[N("sm9"), "nhalf"], writes=[N("sm10")])
                            V(lambda h: h.scalar_tensor_tensor(out=xn2[b2][:], in0=x2t[i % 4][:], scalar=sm[:, 10:11], in1=gbc[:, 0:D], op0=ALU.mult, op1=ALU.mult), [x2n, N("sm10"), "gbc"], [N("xn2")])
                            P.op("scalar", lambda h: h.activation(out=xn2b[i % 4][:], in_=xn2[b2][:], func=AF.Copy), reads=[N("xn2")], writes=["xn2b%d" % (i % 4)])

                        def s3():
                            pXf = pX[:].rearrange("p a b -> p (a b)")
                            for kc in range(8):
                                P.op("tensor", (lambda kc=kc: (lambda h: h.transpose(pXf[:, kc * 128:(kc + 1) * 128], xn2[b2][:, kc * 128:(kc + 1) * 128], cf[:, CF_ID:CF_ID + 128])))(),
                                     reads=[N("xn2"), "cf"], writes=["pXB"], inc=(kc == 7))
                            P.op("scalar", lambda h: h.activation(out=xn2T[b2][:], in_=pXf.rearrange("p (k t) -> p k t", k=8), func=AF.Copy), reads=["pXB"], writes=[N("xn2T")])

                        def s4():
                            for kc in range(8):
                                P.op("tensor", (lambda kc=kc: (lambda h: h.matmul(bk0[:, 0:36], xn2T[b2][:, kc, :], wr32[:, kc, :], start=(kc == 0), stop=(kc == 7))))(),
                                     reads=[N("xn2T"), "wr32"], writes=["bk0"], inc=(kc == 7))
                            V(lambda h: h.tensor_tensor(lg, bk0[:, 0:36], gbc[:, D:D + 36], op=ALU.add), ["bk0", "gbc"], [N("lg")])
                            yield
                            V(lambda h: h.reduce_max(out=sm[:, 16:17], in_=lg[:, 0:4], axis=AX.X), [N("lg")], [N("sm16")])
                            yield
                            V(lambda h: h.tensor_scalar(goh, lg[:, 0:4], sm[:, 16:17], None, op0=ALU.is_equal), [N("lg"), N("sm16")], [N("goh")])
                            yield
                            V(lambda h: h.tensor_scalar(sm[:, 17:18], sm[:, 16:17], -1.0, None, op0=ALU.mult), [N("sm16")], [N("sm17")])
                            yield
                            P.op("scalar", lambda h: h.activation(out=gej, in_=lg[:, 0:4], func=AF.Exp, bias=sm[:, 17:18], accum_out=sm[:, 18:19]), reads=[N("lg"), N("sm17")], writes=[N("gej"), N("sm18")])
                            yield
                            V(lambda h: h.reciprocal(sm[:, 19:20], sm[:, 18:19]), [N("sm18")], [N("sm19")])
                            yield
                            V(lambda h: h.tensor_tensor(A1.rearrange("p (g e) -> p g e", g=4), lg[:, 4:36].rearrange("p (g e) -> p g e", g=4), goh.unsqueeze(2).to_broadcast([128, 4, 8]), op=ALU.mult), [N("lg"), N("goh")], [N("A1")])
                            yield
                            V(lambda h: h.tensor_reduce(out=esel, in_=A1.rearrange("p (g e) -> p e g", g=4), axis=AX.X, op=ALU.add), [N("A1")], [N("esel")])
                            yield
                            V(lambda h: h.max(out=mx8, in_=esel), [N("esel")], [N("mx8")])
                            yield
                            V(lambda h: h.tensor_scalar(mk_, esel, mx8[:, 0:1], None, op0=ALU.is_equal), [N("esel"), N("mx8")], [N("mk0")])
                            yield
                            V(lambda h: h.tensor_scalar(m2t, esel, mx8[:, 1:2], None, op0=ALU.is_equal), [N("esel"), N("mx8")], [N("mk1")])
                            yield
                            V(lambda h: h.tensor_tensor(sm[:, 20:21], mx8[:, 1:2], mx8[:, 0:1], op=ALU.subtract), [N("mx8")], [N("sm20")])
                            yield
                            P.op("scalar", lambda h: h.activation(out=sm[:, 21:22], in_=sm[:, 20:21], func=AF.Exp), reads=[N("sm20")], writes=[N("sm21")])
                            yield
                            V(lambda h: h.tensor_scalar(sm[:, 22:23], sm[:, 21:22], 1.0, None, op0=ALU.add), [N("sm21")], [N("sm22")])
                            yield
                            V(lambda h: h.reciprocal(sm[:, 23:24], sm[:, 22:23]), [N("sm22")], [N("sm23")])
                            yield
                            V(lambda h: h.tensor_tensor(gates[:, i, 0:1], sm[:, 19:20], sm[:, 23:24], op=ALU.mult), [N("sm19"), N("sm23")], ["gate%d" % i])
                            yield
                            V(lambda h: h.tensor_tensor(gates[:, i, 1:2], gates[:, i, 0:1], sm[:, 21:22], op=ALU.mult), ["gate%d" % i, N("sm21")], ["gate%d" % i])
                            yield
                            V(lambda h: h.tensor_tensor(A1.rearrange("p (g e) -> p g e", g=4), goh.unsqueeze(2).to_broadcast([128, 4, 8]), mk_.unsqueeze(1).to_broadcast([128, 4, 8]), op=ALU.mult), [N("mk0"), N("goh"), N("esel")], [N("A1")])
                            yield
                            V(lambda h: h.tensor_tensor(A2.rearrange("p (g e) -> p g e", g=4), goh.unsqueeze(2).to_broadcast([128, 4, 8]), m2t.unsqueeze(1).to_broadcast([128, 4, 8]), op=ALU.mult), [N("mk1"), N("goh")], [N("A2")])
                            yield
                            V(lambda h: h.tensor_tensor(AA, A1, A2, op=ALU.add), [N("A1"), N("A2")], [N("AA")])
                            yield

                        def s5():
                            P.op("tensor", lambda h: h.matmul(bk1[:, 64:96], cf[:, CF_US:CF_US + 128], AA, start=True, stop=False), reads=["cf", N("AA")], writes=["bk1"], inc=False)
                            yield
                            P.op("tensor", lambda h: h.matmul(bk1[:, 64:96], cf[0:1, CF_ONE:CF_ONE + 128], cntrow[0:1, :], start=False, stop=True), reads=["cf", "cntrow"], writes=["bk1"])
                            yield
                            V(lambda h: h.tensor_tensor(pos, bk1[:, 64:96], cf[:, CF_EB:CF_EB + 32], op=ALU.add), ["bk1", "cf"], [N("pos")])
                            yield
                            P.op("tensor", lambda h: h.matmul(bk1[0:1, 128:160], cf[:, CF_ONE:CF_ONE + 1], AA, start=True, stop=True), reads=["cf", N("AA")], writes=["bk1"])
                            yield
                            V(lambda h: h.tensor_tensor(cntrow[:], cntrow[:], bk1[0:1, 128:160], op=ALU.add), ["bk1", "cntrow"], ["cntrow"])
                            yield
                            V(lambda h: h.scalar_tensor_tensor(out=j32, in0=pos, scalar=1.0, in1=A1, op0=ALU.mult, op1=ALU.mult, accum_out=sm[:, 24:25]), [N("pos"), N("A1")], [N("j32"), N("sm24")])
                            yield
                            V(lambda h: h.scalar_tensor_tensor(out=j32, in0=pos, scalar=1.0, in1=A2, op0=ALU.mult, op1=ALU.mult, accum_out=sm[:, 25:26]), [N("pos"), N("A2"), N("j32")], [N("j32"), N("sm25")])
                            yield
                            V(lambda h: h.tensor_scalar(idxs[:, i, 0:2], sm[:, 24:26], float(NE * CAP - 1), None, op0=ALU.min), [N("sm24"), N("sm25")], ["idx%d" % i])
                            yield
                            for k2 in range(2):
                                P.dma("gpsimd", (lambda k2=k2: (lambda h: h.indirect_dma_start(out=self.xbuf, out_offset=bass.IndirectOffsetOnAxis(ap=idxs[:, i, k2:k2 + 1], axis=0), in_=xn2b[i % 4][:], in_offset=None)))(),
                                      reads=["xn2b%d" % (i % 4), "idx%d" % i, "bg:xz"], writes=["xbuf_s%d_%d" % (i, k2)], semres="xn2b%d" % (i % 4))
                        return [s0, s1_, s2, s3, s4, s5]
                    units.append(mk())
                run_pipeline(units)
                P.op("vector", lambda h: h.tensor_copy(self.cnti[:], cntrow[:]), reads=["cntrow"], writes=["cnti"])
                if self.debug == "B":
                    og = self.dbg_out("gates", [128, NT * 2])
                    oi = self.dbg_out("idxs", [128, NT * 2], I32)
                    P.dma("sync", lambda h: h.dma_start(out=og[:, :], in_=gates[:].rearrange("p a b -> p (a b)")), reads=["gate%d" % i for i in range(NT)], writes=["dbg3"])
                    P.dma("sync", lambda h: h.dma_start(out=oi[:, :], in_=idxs[:].rearrange("p a b -> p (a b)")), reads=["idx%d" % i for i in range(NT)], writes=["dbg4"])
                P.barrier()

    def phaseC(self):
        nc, P = self.nc, self.P
        cb = self.cb
        with ExitStack() as st:
            sb = lambda name, shape, dt: st.enter_context(nc.sbuf_tensor(name, shape, dt))
            ps = lambda name, shape, dt=F32: st.enter_context(nc.psum_tensor(name, shape, dt))
            NW, ND = 3, 4
            wgb = [sb("wgb%d" % i, [128, 8, 512], BF16) for i in range(NW)]
            wub = [sb("wub%d" % i, [128, 8, 512], BF16) for i in range(NW)]
            wdb = [sb("wdb%d" % i, [128, 4, D], BF16) for i in range(ND)]
            xrow = [sb("xrow%d" % i, [128, NSB, D], BF16) for i in range(2)]
            XT = [sb("XT%d" % i, [128, 8, CAP], BF16) for i in range(2)]
            th = [sb("thC%d" % i, [128, CAP], F32) for i in range(2)]
            t1 = [sb("t1C%d" % i, [128, CAP], F32) for i in range(2)]
            HT = [sb("HT%d" % i, [128, 4, CAP], BF16) for i in range(2)]
            yo = [sb("yo%d" % i, [128, D], BF16) for i in range(8)]
            pT = [ps("pTC%d" % i, [128, D], BF16) for i in range(2)]
            psG = [ps("psGC%d" % i, [128, 512]) for i in range(2)]
            psU = [ps("psUC%d" % i, [128, 512]) for i in range(2)]
            psY = [ps("psYC%d" % i, [128, 512]) for i in range(2)]
            preg, cnti = self.preg, self.cnti
            semT = P.sems["E_tensor"]
            Ncell = [CAP]

            def skip_wrap(e, thr):
                def wrap(h, clos, nincs):
                    h.reg_load(preg, cnti[0:1, e:e + 1])
                    with h.If_lt(preg, thr + 1):
                        for c_ in clos:
                            for k_, v_ in getattr(c_, "waits", ()):
                                h.wait_ge(P.sems[k_], v_)
                        h.matmul(psY[0][:, 0:1], cb[:, CB_ID:CB_ID + 128], cb[:, CB_ONE:CB_ONE + 1], start=True, stop=True).then_inc(semT, nincs)
                    with h.Else():
                        for c_ in clos:
                            c_(h)
                return wrap

            def nvar_wrap(e):
                def wrap(h, clos, nincs):
                    h.reg_load(preg, cnti[0:1, e:e + 1])
                    with h.If_lt(preg, 257):
                        Ncell[0] = 256
                        for c_ in clos:
                            c_(h)
                    with h.Else():
                        with h.If_lt(preg, 385):
                            Ncell[0] = 384
                            for c_ in clos:
                                c_(h)
                        with h.Else():
                            Ncell[0] = CAP
                            for c_ in clos:
                                c_(h)
                    Ncell[0] = CAP
                return wrap
            zl = sb("zlC", [128, 128], BF16)
            P.op("vector", lambda h: h.memset(zl[:], 0.0), writes=["zlC"])
            for nm, bank in [("psGC0", psG[0]), ("psGC1", psG[1]), ("psUC0", psU[0]), ("psUC1", psU[1]), ("psYC0", psY[0]), ("psYC1", psY[1])]:
                P.op("tensor", (lambda bank=bank: (lambda h: h.matmul(bank[:], zl[:], cb[:, CB_MASK:CB_MASK + 512], start=True, stop=True)))(), reads=["zlC", "cb"], writes=[nm])
            units = []
            for e in range(NE):
                def mk(e=e):
                    b = e % 2
                    w3 = e % NW
                    w4 = e % ND

                    def s0():
                        P.dma("sync", lambda h: h.dma_start(out=xrow[b][:], in_=self.xbuf[e * CAP:(e + 1) * CAP, :].rearrange("(s p) n -> p s n", p=128)), writes=["xrow%d" % b])
                        P.dma("gpsimd", lambda h: h.dma_start(out=wgb[w3][:], in_=self.wg16[e].rearrange("(kc p) n -> p kc n", p=128)), reads=["bg:w16_%d" % e], writes=["wgb%d" % w3])
                        P.dma("gpsimd", lambda h: h.dma_start(out=wub[w3][:], in_=self.wu16[e].rearrange("(kc p) n -> p kc n", p=128)), reads=["bg:w16_%d" % e], writes=["wub%d" % w3])
                        P.dma("gpsimd", lambda h: h.dma_start(out=wdb[w4][:], in_=self.wd16[e].rearrange("(kc p) n -> p kc n", p=128)), reads=["bg:w16_%d" % e], writes=["wdb%d" % w4])

                    def s1():
                        for sbk in range(NSB):
                            xb = sbk % 2
                            for kc in range(8):
                                P.op("tensor", (lambda kc=kc, sbk=sbk, xb=xb: (lambda h: h.transpose(pT[xb][:, kc * 128:(kc + 1) * 128], xrow[b][:, sbk, kc * 128:(kc + 1) * 128], cb[:, CB_ID:CB_ID + 128])))(),
                                     reads=["xrow%d" % b, "cb"], writes=["pTC%d" % xb], inc=(kc == 7))
                            if sbk % 2 == 0:
                                P.op("scalar", (lambda sbk=sbk, xb=xb: (lambda h: h.activation(out=XT[b][:, :, sbk * 128:(sbk + 1) * 128], in_=pT[xb][:].rearrange("p (k t) -> p k t", k=8), func=AF.Copy)))(),
                                     reads=["pTC%d" % xb], writes=["XT%d_%d" % (b, sbk)])
                            else:
                                P.op("vector", (lambda sbk=sbk, xb=xb: (lambda h: h.tensor_copy(XT[b][:, :, sbk * 128:(sbk + 1) * 128], pT[xb][:].rearrange("p (k t) -> p k t", k=8))))(),
                                     reads=["pTC%d" % xb], writes=["XT%d_%d" % (b, sbk)])

                    def s2():
                        xres = ["XT%d_%d" % (b, k) for k in range(NSB)]
                        P.begin_region("tensor")
                        for mt in range(4):
                            mb = mt % 2
                            for kc in range(8):
                                P.op("tensor", (lambda kc=kc, mt=mt, mb=mb: (lambda h: h.matmul(psG[mb][:, 0:Ncell[0]], wgb[w3][:, kc, mt * 128:(mt + 1) * 128], XT[b][:, kc, 0:Ncell[0]], start=(kc == 0), stop=(kc == 7))))(),
                                     reads=["wgb%d" % w3, "cnti"] + xres, writes=["psGC%d" % mb], inc=(kc == 7))
                            for kc in range(8):
                                P.op("tensor", (lambda kc=kc, mt=mt: (lambda h: h.matmul(psU[mt % 2][:, 0:Ncell[0]], wub[w3][:, kc, mt * 128:(mt + 1) * 128], XT[b][:, kc, 0:Ncell[0]], start=(kc == 0), stop=(kc == 7))))(),
                                     reads=["wub%d" % w3, "cnti"] + xres, writes=["psUC%d" % (mt % 2)], inc=(kc == 7))
                            P.op("scalar", (lambda mb=mb: (lambda h: h.activation(out=th[mb][:], in_=psG[mb][:, 0:CAP], func=AF.Tanh, scale=0.5)))(), reads=["psGC%d" % mb], writes=["thC%d" % mb])
                            P.op("vector", (lambda mb=mb: (lambda h: h.scalar_tensor_tensor(out=t1[mb][:], in0=th[mb][:], scalar=1.0, in1=psG[mb][:, 0:CAP], op0=ALU.add, op1=ALU.mult)))(), reads=["thC%d" % mb, "psGC%d" % mb], writes=["t1C%d" % mb])
                            P.op("vector", (lambda mb=mb, mt=mt: (lambda h: h.scalar_tensor_tensor(out=HT[b][:, mt, :], in0=t1[mb][:], scalar=0.5, in1=psU[mb][:, 0:CAP], op0=ALU.mult, op1=ALU.mult)))(), reads=["t1C%d" % mb, "psUC%d" % mb], writes=["HT%d_%d" % (b, mt)])
                        P.end_region(nvar_wrap(e))

                    def s3():
                        hres = ["HT%d_%d" % (b, k) for k in range(4)]
                        for sbk in range(NSB):
                            ob = (e * NSB + sbk) % 8
                            r0 = e * CAP + sbk * 128
                            if sbk >= 2:
                                P.begin_region("tensor")
                            for half in range(2):
                                for mt in range(4):
                                    P.op("tensor", (lambda mt=mt, sbk=sbk, half=half: (lambda h: h.matmul(psY[half][:], HT[b][:, mt, sbk * 128:(sbk + 1) * 128], wdb[w4][:, mt, half * 512:(half + 1) * 512], start=(mt == 0), stop=(mt == 3))))(),
                                         reads=hres + ["wdb%d" % w4, "cnti"], writes=["psYC%d" % half], inc=(mt == 3))
                            if sbk >= 2:
                                P.end_region(skip_wrap(e, 128 * sbk))
                            for half in range(2):
                                if half == 0:
                                    P.op("scalar", (lambda ob=ob: (lambda h: h.activation(out=yo[ob][:, 0:512], in_=psY[0][:], func=AF.Copy)))(), reads=["psYC0"], writes=["yo%d" % ob])
                                else:
                                    P.op("vector", (lambda ob=ob: (lambda h: h.tensor_copy(yo[ob][:, 512:1024], psY[1][:])))(), reads=["psYC1"], writes=["yo%d" % ob])
                            P.dma("scalar", (lambda ob=ob, r0=r0: (lambda h: h.dma_start(out=self.ybuf[r0:r0 + 128, :], in_=yo[ob][:])))(), reads=["yo%d" % ob], writes=["ybuf%d" % (r0 // 128)], semres="yo%d" % ob)
                    return [s0, s1, s2, s3]
                units.append(mk())
            run_pipeline(units)
            self.phaseD(st)

    def phaseD(self, st):
        nc, P = self.nc, self.P
        gates, idxs = self.gates, self.idxs
        if True:
            sb = lambda name, shape, dt: st.enter_context(nc.sbuf_tensor(name, shape, dt))
            yall = ["ybuf%d" % k for k in range(NE * NSB)]
            y1 = [sb("y1D%d" % i, [128, D], BF16) for i in range(3)]
            y2 = [sb("y2D%d" % i, [128, D], BF16) for i in range(3)]
            x2 = [sb("x2D%d" % i, [128, D], F32) for i in range(3)]
            oo = [sb("ooD%d" % i, [128, D], F32) for i in range(3)]
            for i in range(NT):
                b = i % 3
                P.dma("gpsimd", (lambda i=i, b=b: (lambda h: h.indirect_dma_start(out=y1[b][:], out_offset=None, in_=self.ybuf, in_offset=bass.IndirectOffsetOnAxis(ap=idxs[:, i, 0:1], axis=0))))(), reads=yall, writes=["y1D%d" % b])
                P.dma("gpsimd", (lambda i=i, b=b: (lambda h: h.indirect_dma_start(out=y2[b][:], out_offset=None, in_=self.ybuf, in_offset=bass.IndirectOffsetOnAxis(ap=idxs[:, i, 1:2], axis=0))))(), reads=yall, writes=["y2D%d" % b])
                P.dma("sync", (lambda i=i, b=b: (lambda h: h.dma_start(out=x2[b][:], in_=self.x2buf[i * 128:(i + 1) * 128, :])))(), writes=["x2D%d" % b])
                P.op("vector", (lambda i=i, b=b: (lambda h: h.scalar_tensor_tensor(out=oo[b][:], in0=y1[b][:], scalar=gates[:, i, 0:1], in1=x2[b][:], op0=ALU.mult, op1=ALU.add)))(), reads=["y1D%d" % b, "x2D%d" % b], writes=["ooD%d" % b])
                P.op("vector", (lambda i=i, b=b: (lambda h: h.scalar_tensor_tensor(out=oo[b][:], in0=y2[b][:], scalar=gates[:, i, 1:2], in1=oo[b][:], op0=ALU.mult, op1=ALU.add)))(), reads=["y2D%d" % b, "ooD%d" % b], writes=["ooD%d" % b])
                P.dma("scalar", (lambda i=i, b=b: (lambda h: h.dma_start(out=self.out[i * 128:(i + 1) * 128, :], in_=oo[b][:])))(), reads=["ooD%d" % b], writes=["out%d" % i], semres="ooD%d" % b)


def host_shared(inputs):
    f = lambda k: np.ascontiguousarray(np.asarray(inputs[k], dtype=np.float32)[0])
    pv = np.zeros((128, NPV), np.float32)
    t8 = lambda v: v.reshape(8, 128).T
    pv[:, PV["gmix"]:PV["gmix"] + 8] = t8(f("norm_mix_g"))
    pv[:, PV["gout"]:PV["gout"] + 8] = t8(np.concatenate([f("lru_out_g"), f("fox_out_g")]))
    pv[:, PV["gmemx"]:PV["gmemx"] + 8] = t8(f("norm_mem_x_g"))
    pv[:, PV["gmem"]:PV["gmem"] + 8] = t8(f("norm_mem_g"))
    pv[:, PV["convw"]:PV["convw"] + 16] = f("conv_w").reshape(4, 4, 128).transpose(2, 1, 0).reshape(128, 16)
    for n, k in [("convb", "conv_b"), ("ba", "lru_ba"), ("bx", "lru_bx"), ("apar", "lru_a_param")]:
        pv[:, PV[n]:PV[n] + 4] = f(k).reshape(4, 128).T
    pv[:, PV["gq2"]] = np.tile(f("fox_q_g"), 2)
    pv[:, PV["gk2"]] = np.tile(f("fox_k_g"), 2)
    pv[:, PV["mqg"]] = f("mem_q_g")
    pv[:, PV["mkg"]] = f("mem_k_g")
    pv[0:8, PV["bf"]] = f("b_forget")
    pv[0:64, PV["gfx"]:PV["gfx"] + 8] = f("fox_out_g").reshape(8, 64).T
    bvec = np.concatenate([f("norm_ffn_g"), f("router_group_b"), f("router_expert_b"), f("norm_mix_g"), f("norm_mem_x_g"), f("norm_mem_g")])[None, :]
    wr = np.concatenate([f("router_group_w"), f("router_expert_w")], axis=1)
    wa, wx = f("lru_wa"), f("lru_wx")
    lruw = np.zeros((128, 4, 2, 128), np.float32)
    for ct in range(4):
        for hb in range(2):
            lruw[hb * 64:(hb + 1) * 64, ct, 0, hb * 64:(hb + 1) * 64] = wa[2 * ct + hb]
            lruw[hb * 64:(hb + 1) * 64, ct, 1, hb * 64:(hb + 1) * 64] = wx[2 * ct + hb]
    cbf = np.zeros((128, NCB), np.float32)
    cbf[:, CB_ID:CB_ID + 128] = np.eye(128)
    cbf[0:64, CB_BO:CB_BO + 64] = 1.0
    cbf[64:128, CB_BO + 64:CB_BO + 128] = 1.0
    cbf[:, CB_ONE:CB_ONE + 128] = 1.0
    sp = np.arange(128)[:, None]
    tq = np.arange(512)[None, :]
    for m in range(4):
        cbf[:, CB_MASK + m * 512:CB_MASK + (m + 1) * 512] = np.where(m * 128 + sp > tq, -30000.0, 0.0)
    cf = np.zeros((128, NCF), np.float32)
    cf[:, CF_ID:CF_ID + 128] = np.eye(128)
    cf[:, CF_US:CF_US + 128] = (np.arange(128)[:, None] < np.arange(128)[None, :]).astype(np.float32)
    cf[:, CF_ONE:CF_ONE + 128] = 1.0
    cf[:, CF_EB:CF_EB + 32] = (np.arange(32) * CAP)[None, :]
    return {
        "w_in": f("w_in"), "w_out": f("w_out"), "mem_wq": f("mem_wq"), "mem_wkv": f("mem_wkv"), "mem_wo": f("mem_wo"),
        "wr": np.ascontiguousarray(wr), "wgate": f("exp_w_gate"), "wup": f("exp_w_up"), "wdown": f("exp_w_down"),
        "lruw": lruw.reshape(128, 1024), "pvec": pv, "bvec": np.ascontiguousarray(bvec),
        "cbf": cbf.astype(ml_dtypes.bfloat16), "cf32": cf,
    }


def kernel(**inputs):
    nc = Builder().build()
    shared = host_shared(inputs)
    x = np.asarray(inputs["x"], dtype=np.float32)
    mem = np.asarray(inputs["mem"], dtype=np.float32)
    in_maps = []
    for c in range(8):
        m = dict(shared)
        m["x"] = np.ascontiguousarray(x[c])
        m["mem"] = np.ascontiguousarray(mem[c])
        in_maps.append(m)
    res = run_bass_kernel_spmd(nc, in_maps, core_ids=list(range(8)))
    return np.stack([np.asarray(r["out"], dtype=np.float32) for r in res.results], axis=0)
```
